# Optimizing a Trainium2 kernel written in Bass

```python
import math
import jax
import jax.numpy as jnp
from jax import lax
import numpy as np

D_MODEL = 1024
BATCH = 4
SEQ = 8192
DEPTH = 4

GRID_W = 64
CTX_LEN = 256
N_MIXERS = 2
N_HEADS = 8
HEAD_DIM = 64
V_HEAD_DIM = 2 * HEAD_DIM
Q_BLOCK = 128
ROPE_BASE = 10000.0
ROPE_AXIS_DIM = HEAD_DIM // 2
D_RNN = D_MODEL
N_LRU_BLOCKS = 8
LRU_BLOCK = D_RNN // N_LRU_BLOCKS
CONV_W = 4
CONV_PAD_L = CONV_W // 2
CONV_PAD_R = CONV_W - 1 - CONV_PAD_L
RG_C = 8.0
D_FF = 2816
N_EXPERTS = 8
TOP_K = 2
D_FF_EXPERT = 3584
EPS = 1e-6
N_EVEN = (DEPTH + 1) // 2
N_ODD = DEPTH // 2

kernel_name = 'hybrid_diffattn_rglru_moe_dit'


def rmsnorm(x, g):
    xf = x.astype(jnp.float32)
    xf = xf * lax.rsqrt(jnp.mean(xf * xf, axis=-1, keepdims=True) + EPS)
    return xf.astype(x.dtype) * g


def modulate(x, g, shift, scale):
    return rmsnorm(x, g) * (1.0 + scale) + shift


def axial_rope_tables(n_tokens):
    rows = n_tokens // GRID_W
    row = jnp.repeat(jnp.arange(rows, dtype=jnp.float32), GRID_W)
    col = jnp.tile(jnp.arange(GRID_W, dtype=jnp.float32), rows)
    inv_freq = 1.0 / (ROPE_BASE ** (jnp.arange(0, ROPE_AXIS_DIM, 2, dtype=jnp.float32) / ROPE_AXIS_DIM))
    ang = jnp.stack([row[:, None] * inv_freq, col[:, None] * inv_freq], axis=1)
    ang = jnp.broadcast_to(ang[:, :, None, :], (n_tokens, 2, 2, ROPE_AXIS_DIM // 2))
    ang = ang.reshape(n_tokens, HEAD_DIM)
    return jnp.cos(ang), jnp.sin(ang)


def apply_axial_rope(x, cos, sin):
    xs = x.reshape(*x.shape[:-1], 2, 2, ROPE_AXIS_DIM // 2)
    rot = jnp.stack([-xs[..., 1, :], xs[..., 0, :]], axis=-2).reshape(x.shape)
    cos = cos[None, :, None, None, :].astype(x.dtype)
    sin = sin[None, :, None, None, :].astype(x.dtype)
    return x * cos + rot * sin


def diff_softmax_mix(q, k, v, lam):
    s = jnp.einsum('bqhpd,bkhpd->pbhqk', q, k).astype(jnp.float32)
    p = jax.nn.softmax(s, axis=-1)
    w = (p[0] - lam * p[1]).astype(v.dtype)
    return jnp.einsum('bhqk,bkhe->bqhe', w, v)


def diff_attention(h_lat, h_ctx, w_qkv, w_o, lam_vec, subln_g, lambda_init, cos, sin, with_ctx_out):
    bsz, n_lat, _ = h_lat.shape
    n_ctx = h_ctx.shape[1]
    scale = HEAD_DIM ** -0.5
    q_l, k_l, v_l = jnp.split(h_lat @ w_qkv, 3, axis=-1)
    q_l = apply_axial_rope(q_l.reshape(bsz, n_lat, N_HEADS, 2, HEAD_DIM) * scale, cos, sin)
    k_l = apply_axial_rope(k_l.reshape(bsz, n_lat, N_HEADS, 2, HEAD_DIM), cos, sin)
    v_l = v_l.reshape(bsz, n_lat, N_HEADS, V_HEAD_DIM)
    k_c, v_c = jnp.split(h_ctx @ w_qkv[:, D_MODEL:], 2, axis=-1)
    k_c = k_c.reshape(bsz, n_ctx, N_HEADS, 2, HEAD_DIM)
    v_c = v_c.reshape(bsz, n_ctx, N_HEADS, V_HEAD_DIM)
    lv = lam_vec.astype(jnp.float32)
    lam = jnp.exp(jnp.sum(lv[0] * lv[1])) - jnp.exp(jnp.sum(lv[2] * lv[3])) + lambda_init
    k_all = jnp.concatenate([k_l, k_c], axis=1)
    v_all = jnp.concatenate([v_l, v_c], axis=1)
    n_blocks = n_lat // Q_BLOCK
    q_blocks = q_l.reshape(bsz, n_blocks, Q_BLOCK, N_HEADS, 2, HEAD_DIM).swapaxes(0, 1)
    o_l = lax.map(lambda qb: diff_softmax_mix(qb, k_all, v_all, lam), q_blocks)
    o_l = o_l.swapaxes(0, 1).reshape(bsz, n_lat, N_HEADS, V_HEAD_DIM)

    def head_out(o):
        o = rmsnorm(o, subln_g) * (1.0 - lambda_init)
        return o.reshape(bsz, o.shape[1], D_MODEL) @ w_o

    y_l = head_out(o_l)
    if with_ctx_out:
        q_c = (h_ctx @ w_qkv[:, :D_MODEL]).reshape(bsz, n_ctx, N_HEADS, 2, HEAD_DIM) * scale
        y_c = head_out(diff_softmax_mix(q_c, k_c, v_c, lam))
    else:
        y_c = None
    return y_l, y_c


def depthwise_conv(u, w, b):
    y = lax.conv_general_dilated(u, w[:, None, :], window_strides=(1,),
                                 padding=[(CONV_PAD_L, CONV_PAD_R)],
                                 dimension_numbers=('NWC', 'WIO', 'NWC'),
                                 feature_group_count=u.shape[-1])
    return y + b


def rglru_coeffs(u, w_gates, b_gates, a_param):
    bsz, n, _ = u.shape
    ub = u.reshape(bsz, n, N_LRU_BLOCKS, LRU_BLOCK)
    g = jnp.einsum('bnkc,gkcd->gbnkd', ub, w_gates).reshape(2, bsz, n, D_RNN) + b_gates[:, None, None, :]
    g = jax.nn.sigmoid(g.astype(jnp.float32))
    r_t, i_t = g[0], g[1]
    log_a = RG_C * r_t * jax.nn.log_sigmoid(a_param.astype(jnp.float32))
    a_t = jnp.exp(log_a)
    b_t = jnp.sqrt(-jnp.expm1(2.0 * log_a)) * (i_t * u.astype(jnp.float32))
    return a_t, b_t


def linear_scan(a, b, h0, reverse):
    def combine(e1, e2):
        a1, b1 = e1
        a2, b2 = e2
        return a1 * a2, a2 * b1 + b2
    a_cum, b_cum = lax.associative_scan(combine, (a, b), reverse=reverse, axis=1)
    return a_cum * h0[:, None, :] + b_cum


def rglru_mixer(h_lat, h_ctx, w_in, conv_w, conv_b, gate_w, gate_b, a_param, w_out, with_ctx_out):
    bsz = h_lat.shape[0]
    proj_l = h_lat @ w_in
    proj_c = h_ctx @ w_in
    gate_l = jax.nn.gelu(proj_l[..., :D_RNN])
    u_l = depthwise_conv(proj_l[..., D_RNN:], conv_w, conv_b)
    u_c = depthwise_conv(proj_c[..., D_RNN:], conv_w, conv_b)
    h0 = jnp.zeros((bsz, D_RNN), jnp.float32)
    lat_states, ctx_states = [], []
    for d, reverse in enumerate((False, True)):
        a_c, b_c = rglru_coeffs(u_c, gate_w[d], gate_b[d], a_param[d])
        h_c = linear_scan(a_c, b_c, h0, reverse)
        h_c_final = h_c[:, 0] if reverse else h_c[:, -1]
        a_l, b_l = rglru_coeffs(u_l, gate_w[d], gate_b[d], a_param[d])
        lat_states.append(linear_scan(a_l, b_l, h_c_final, reverse))
        ctx_states.append(h_c)
    y_l = ((lat_states[0] + lat_states[1]).astype(gate_l.dtype) * gate_l) @ w_out
    if with_ctx_out:
        gate_c = jax.nn.gelu(proj_c[..., :D_RNN])
        y_c = ((ctx_states[0] + ctx_states[1]).astype(gate_c.dtype) * gate_c) @ w_out
    else:
        y_c = None
    return y_l, y_c


def swiglu(h, w_gate_up, w_down):
    gate, up = jnp.split(h @ w_gate_up, 2, axis=-1)
    return (jax.nn.silu(gate) * up) @ w_down


def moe_swiglu(h, router_w, w_gate_up, w_down):
    logits = (h @ router_w).astype(jnp.float32)
    top_val, top_idx = lax.top_k(logits, TOP_K)
    top_w = jax.nn.softmax(top_val, axis=-1)
    gates = jnp.einsum('bnk,bnke->bne', top_w,
                       jax.nn.one_hot(top_idx, N_EXPERTS, dtype=jnp.float32)).astype(h.dtype)
    y = jnp.zeros_like(h)
    for e in range(N_EXPERTS):
        y = y + gates[..., e:e + 1] * swiglu(h, w_gate_up[e], w_down[e])
    return y


def setup_inputs(seed: int = 0) -> dict:
    key = jax.random.key(seed)
    ks = jax.random.split(key, 25)
    f32 = jnp.float32

    def nrm(k, shape, scale):
        return jax.random.normal(k, shape, f32) * scale

    D = D_MODEL
    a0 = jax.random.uniform(ks[17], (N_ODD, 2, D_RNN), f32, 0.9, 0.999)
    s = a0 ** (1.0 / RG_C)
    return {
        'x': nrm(ks[0], (BATCH, SEQ, D), 1.0),
        'c': nrm(ks[1], (BATCH, D), 1.0),
        'ctx': nrm(ks[2], (BATCH, CTX_LEN, D), 1.0),
        'c_ctx': nrm(ks[3], (D,), 1.0),
        'mod_w': nrm(ks[4], (DEPTH, D, 6 * D), 0.5 * D ** -0.5),
        'mod_b': nrm(ks[5], (DEPTH, 6 * D), 0.02),
        'norm_mix_g': 1.0 + nrm(ks[6], (DEPTH, D), 0.02),
        'norm_ffn_g': 1.0 + nrm(ks[7], (DEPTH, D), 0.02),
        'attn_w_qkv': nrm(ks[8], (N_EVEN, D, 3 * D), D ** -0.5),
        'attn_w_o': nrm(ks[9], (N_EVEN, D, D), D ** -0.5),
        'attn_lambda': nrm(ks[10], (N_EVEN, 4, HEAD_DIM), 0.1),
        'attn_subln_g': 1.0 + nrm(ks[11], (N_EVEN, V_HEAD_DIM), 0.02),
        'lru_w_in': nrm(ks[12], (N_ODD, D, 2 * D_RNN), D ** -0.5),
        'lru_conv_w': nrm(ks[13], (N_ODD, CONV_W, D_RNN), CONV_W ** -0.5),
        'lru_conv_b': nrm(ks[14], (N_ODD, D_RNN), 0.01),
        'lru_gate_w': nrm(ks[15], (N_ODD, 2, 2, N_LRU_BLOCKS, LRU_BLOCK, LRU_BLOCK), LRU_BLOCK ** -0.5),
        'lru_gate_b': nrm(ks[16], (N_ODD, 2, 2, D_RNN), 0.01),
        'lru_a_param': jnp.log(s) - jnp.log1p(-s),
        'lru_w_out': nrm(ks[18], (N_ODD, D_RNN, D), D_RNN ** -0.5),
        'ffn_w_gate_up': nrm(ks[19], (N_EVEN, D, 2 * D_FF), D ** -0.5),
        'ffn_w_down': nrm(ks[20], (N_EVEN, D_FF, D), D_FF ** -0.5),
        'moe_router_w': nrm(ks[21], (N_ODD, D, N_EXPERTS), D ** -0.5),
        'moe_w_gate_up': nrm(ks[22], (N_ODD, N_EXPERTS, D, 2 * D_FF_EXPERT), D ** -0.5),
        'moe_w_down': nrm(ks[23], (N_ODD, N_EXPERTS, D_FF_EXPERT, D), D_FF_EXPERT ** -0.5),
        'final_norm_g': 1.0 + nrm(ks[24], (D,), 0.02),
    }


def reference(x, c, ctx, c_ctx, mod_w, mod_b, norm_mix_g, norm_ffn_g, attn_w_qkv, attn_w_o,
              attn_lambda, attn_subln_g, lru_w_in, lru_conv_w, lru_conv_b, lru_gate_w, lru_gate_b,
              lru_a_param, lru_w_out, ffn_w_gate_up, ffn_w_down, moe_router_w, moe_w_gate_up,
              moe_w_down, final_norm_g):
    n_lat = x.shape[1]
    cos, sin = axial_rope_tables(n_lat)
    silu_c = jax.nn.silu(c)
    silu_cc = jax.nn.silu(c_ctx)

    def channel_mixer(i, j, f):
        if i % 2 == 0:
            return swiglu(f, ffn_w_gate_up[j], ffn_w_down[j])
        return moe_swiglu(f, moe_router_w[j], moe_w_gate_up[j], moe_w_down[j])

    for i in range(DEPTH):
        last = i == DEPTH - 1
        j = i // N_MIXERS
        mod_l = (silu_c @ mod_w[i] + mod_b[i])[:, None, :]
        mod_c = silu_cc @ mod_w[i] + mod_b[i]
        sh1, sc1, g1, sh2, sc2, g2 = jnp.split(mod_l, 6, axis=-1)
        sh1c, sc1c, g1c, sh2c, sc2c, g2c = jnp.split(mod_c, 6, axis=-1)
        h_l = modulate(x, norm_mix_g[i], sh1, sc1)
        h_c = modulate(ctx, norm_mix_g[i], sh1c, sc1c)
        if i % N_MIXERS == 0:
            lambda_init = 0.8 - 0.6 * math.exp(-0.3 * i)
            y_l, y_c = diff_attention(h_l, h_c, attn_w_qkv[j], attn_w_o[j], attn_lambda[j],
                                      attn_subln_g[j], lambda_init, cos, sin, not last)
        else:
            y_l, y_c = rglru_mixer(h_l, h_c, lru_w_in[j], lru_conv_w[j], lru_conv_b[j], lru_gate_w[j],
                                   lru_gate_b[j], lru_a_param[j], lru_w_out[j], not last)
        x = x + g1 * y_l
        f_l = modulate(x, norm_ffn_g[i], sh2, sc2)
        x = x + g2 * channel_mixer(i, j, f_l)
        if not last:
            ctx = ctx + g1c * y_c
            f_c = modulate(ctx, norm_ffn_g[i], sh2c, sc2c)
            ctx = ctx + g2c * channel_mixer(i, j, f_c)
    return rmsnorm(x, final_norm_g)
```

```python
import contextlib
import math
import numpy as np
import concourse.bass as bass
import concourse.mybir as mybir
from concourse.bass_utils import run_bass_kernel_spmd

F32 = mybir.dt.float32
BF16 = mybir.dt.bfloat16
I32 = mybir.dt.int32
AF = mybir.ActivationFunctionType
ALU = mybir.AluOpType
AX = mybir.AxisListType

D = 1024
KC = 8
CTX = 256
NH = 8
DFF = 2816
DFE = 3584
NE = 8
EPS = 1e-6
EPOCH = 30000
SAME_ENGINE_SYNC = True


class DmaSem:
    def __init__(self, k):
        self.k = k
        self.sem = k.new_sem()
        self.count = 0

    def bump(self):
        if self.count + 16 > EPOCH:
            self.sem = self.k.new_sem()
            self.count = 0
        self.count += 16
        return self.sem, self.count


class Buf:
    def __init__(self, k, name, t):
        self.k = k
        self.name = name
        self.t = t
        self.lw = None
        self.rd = {}
        self.ld = None
        self.st = None

    def __getitem__(self, key):
        return self.t[key]


class HalfView:
    def __init__(self, t, h):
        self.t = t
        self.h = h

    def __getitem__(self, key):
        return self.t[(key[0], self.h) + tuple(key[1:])]


class K:
    def __init__(self, nc):
        self.nc = nc
        self.es = contextlib.ExitStack()
        self.engs = {"pe": nc.tensor, "act": nc.scalar, "dve": nc.vector, "pool": nc.gpsimd, "sp": nc.sync}
        self.cnt = {e: 0 for e in self.engs}
        self.esems = {e: [] for e in self.engs}
        self.waited = {e: {} for e in self.engs}
        self.nsem = 0
        self.dsems = []
        self.free_dsems = []
        self.live_dsems = []
        self.uid = 0

    def new_sem(self):
        self.nsem += 1
        return self.es.enter_context(self.nc.semaphore("s%d" % self.nsem))

    def esem(self, eng, seq):
        i = (seq - 1) // EPOCH
        while len(self.esems[eng]) <= i:
            self.esems[eng].append(self.new_sem())
        return self.esems[eng][i], (seq - 1) % EPOCH + 1

    def new_dsem(self):
        if self.free_dsems:
            d = self.free_dsems.pop()
        else:
            d = DmaSem(self)
            self.dsems.append(d)
        self.live_dsems.append(d)
        return d

    def sb(self, stack, name, shape, dt):
        self.uid += 1
        t = stack.enter_context(self.nc.sbuf_tensor("%s_%d" % (name, self.uid), list(shape), dt))
        return Buf(self, name, t)

    def ps(self, stack, name, shape, dt):
        self.uid += 1
        t = stack.enter_context(self.nc.psum_tensor("%s_%d" % (name, self.uid), list(shape), dt))
        return Buf(self, name, t)

    def _wait(self, eng, dep):
        w = self.waited[eng]
        if dep[0] == "e":
            _, src, seq = dep
            if src == eng and (not SAME_ENGINE_SYNC or eng == "pe"):
                return
            if w.get(src, 0) >= seq:
                return
            w[src] = seq
            sem, val = self.esem(src, seq)
            self.engs[eng].wait_ge(sem, val)
        else:
            _, sem, val, sid = dep
            key = ("d", sid)
            if w.get(key, 0) >= val:
                return
            w[key] = val
            self.engs[eng].wait_ge(sem, val)

    def _deps(self, eng, R, W, skip_waw_dma=False):
        for b in R:
            if b.lw is not None:
                self._wait(eng, b.lw)
        for b in W:
            if b.lw is not None and not (skip_waw_dma and b.lw[0] == "d"):
                self._wait(eng, b.lw)
            for d in b.rd.values():
                self._wait(eng, d)

    def I(self, eng, fn, R=(), W=()):
        self._deps(eng, R, W)
        ins = fn(self.engs[eng])
        self.cnt[eng] += 1
        seq = self.cnt[eng]
        sem, _ = self.esem(eng, seq)
        ins.then_inc(sem, 1)
        tag = ("e", eng, seq)
        for b in R:
            b.rd[eng] = tag
        for b in W:
            b.lw = tag
            b.rd = {}
        return ins

    def dma(self, q, out, in_, R=(), W=(), add=False, **kw):
        self._deps(q, R, W, skip_waw_dma=add)
        if W:
            if W[0].ld is None:
                W[0].ld = self.new_dsem()
            ds = W[0].ld
        else:
            if R[0].st is None:
                R[0].st = self.new_dsem()
            ds = R[0].st
        ins = self.engs[q].dma_start(out=out, in_=in_, **kw)
        sem, val = ds.bump()
        ins.then_inc(sem, 16)
        tag = ("d", sem, val, id(sem))
        for b in W:
            b.lw = tag
            b.rd = {}
        for b in R:
            b.rd[("st", id(sem))] = tag
        return ins

    def load(self, out, in_, W, **kw):
        return self.dma("sp", out, in_, W=W, **kw)

    def store(self, out, in_, R, **kw):
        return self.dma("pool", out, in_, R=R, **kw)

    def barrier(self):
        pool = self.engs["pool"]
        for d in self.dsems:
            if d.count > 0:
                self._wait("pool", ("d", d.sem, d.count, id(d.sem)))
        for e in ("pe", "act", "dve"):
            if self.cnt[e] > 0:
                self._wait("pool", ("e", e, self.cnt[e]))
        ins = pool.nop()
        self.cnt["pool"] += 1
        seq = self.cnt["pool"]
        sem, _ = self.esem("pool", seq)
        ins.then_inc(sem, 1)
        for e in ("pe", "act", "dve", "sp"):
            self._wait(e, ("e", "pool", seq))
        self.free_dsems.extend(self.live_dsems)
        self.live_dsems = []


class Prog:
    def __init__(self, nlat=8192, layers=(0, 1, 2, 3), dbg=(), do_final=True, ncores=4, conv_all=True):
        self.nlat = nlat
        self.T = CTX + nlat
        self.layers = list(layers)
        self.dbg = set(dbg)
        self.do_final = do_final
        self.in_shapes = {}
        nc = bass.Bass("TRN2", target_bir_lowering=False)
        self.nc = nc
        self.k = K(nc)
        self.blocks = [(0, CTX, True)] + [(CTX + i * 512, 512, False) for i in range(nlat // 512)]
        sbs = []
        cur = []
        tot = 0
        for b in self.blocks:
            if tot + b[1] > 1792:
                sbs.append(cur)
                cur, tot = [], 0
            cur.append(b)
            tot += b[1]
        if cur:
            sbs.append(cur)
        self.superblocks = sbs
        self.decl()

    def din(self, name, shape, dt=F32):
        if not self.used(name):
            shape = [1, 1]
        self.in_shapes[name] = list(shape)
        return self.nc.dram_tensor(name, list(shape), dt, kind="ExternalInput").ap()

    def used(self, name):
        att = any(l % 2 == 0 for l in self.layers)
        lru = any(l % 2 == 1 for l in self.layers)
        if name.startswith("attn_") or name.startswith("ffn_") or name in ("perm", "ropec"):
            return att
        if name.startswith("lru_") or name.startswith("moe_"):
            return lru
        return True

    def dscr(self, name, shape, dt):
        kind = "ExternalOutput" if name in self.dbg else "Internal"
        return self.nc.dram_tensor(name, list(shape), dt, kind=kind).ap()

    def decl(self):
        T = self.T
        i = {}
        i["x"] = self.din("x", [self.nlat, D])
        i["c"] = self.din("c", [D])
        i["ctx"] = self.din("ctx", [CTX, D])
        i["c_ctx"] = self.din("c_ctx", [D])
        i["mod_w"] = self.din("mod_w", [4, D, 6 * D])
        i["mod_b"] = self.din("mod_b", [4, 6 * D])
        i["norm_mix_g"] = self.din("norm_mix_g", [4, D])
        i["norm_ffn_g"] = self.din("norm_ffn_g", [4, D])
        i["attn_w_qkv"] = self.din("attn_w_qkv", [2, D, 3 * D])
        i["attn_w_o"] = self.din("attn_w_o", [2, D, D])
        i["attn_lambda"] = self.din("attn_lambda", [2, 4 * 64])
        i["attn_subln_g"] = self.din("attn_subln_g", [2, 128])
        i["lru_w_in"] = self.din("lru_w_in", [2, D, 2 * D])
        i["lru_conv_w"] = self.din("lru_conv_w", [2, 4, D])
        i["lru_conv_b"] = self.din("lru_conv_b", [2, D])
        i["lru_gate_w"] = self.din("lru_gate_w", [2, 2, 2, 8, 128, 128])
        i["lru_gate_b"] = self.din("lru_gate_b", [2, 2, 2, D])
        i["lru_a_param"] = self.din("lru_a_param", [2, 2, D])
        i["lru_w_out"] = self.din("lru_w_out", [2, D, D])
        i["ffn_w_gate_up"] = self.din("ffn_w_gate_up", [2, D, 2 * DFF])
        i["ffn_w_down"] = self.din("ffn_w_down", [2, DFF, D])
        i["moe_router_w"] = self.din("moe_router_w", [2, D, NE])
        i["moe_w_gate_up"] = self.din("moe_w_gate_up", [2, NE, D, 2 * DFE])
        i["moe_w_down"] = self.din("moe_w_down", [2, NE, DFE, D])
        i["final_norm_g"] = self.din("final_norm_g", [D])
        i["ident"] = self.din("ident", [128, 128])
        i["ones"] = self.din("ones", [128, 128])
        i["ropec"] = self.din("ropec", [128, 8])
        i["perm"] = self.din("perm", [128, 128])
        self.inp = i
        self.out = self.nc.dram_tensor("out", [self.nlat, D], F32, kind="ExternalOutput").ap()
        s = {}
        s["XT"] = self.dscr("XT", [D, T], F32)
        s["FT"] = self.dscr("FT", [D, T], BF16)
        s["ST"] = self.dscr("ST", [D, T], BF16)
        s["G"] = self.dscr("G", [NE, T], F32)
        s["QT"] = self.dscr("QT", [D, T], BF16)
        s["KT"] = self.dscr("KT", [D, T], BF16)
        s["V"] = self.dscr("V", [T, D], BF16)
        s["GATE"] = self.dscr("GATE", [D, T], BF16)
        s["R"] = self.dscr("R", [D, T], F32)
        s["w_qkv"] = self.dscr("w_qkv", [2, D, 3 * D], BF16)
        s["w_o"] = self.dscr("w_o", [2, D, D], BF16)
        s["w_in"] = self.dscr("w_in", [2, D, 2 * D], BF16)
        s["w_out"] = self.dscr("w_out", [2, D, D], BF16)
        s["w_fgu"] = self.dscr("w_fgu", [2, D, 2 * DFF], BF16)
        s["w_fd"] = self.dscr("w_fd", [2, DFF, D], BF16)
        s["w_mgu"] = self.dscr("w_mgu", [2, NE, D, 2 * DFE], BF16)
        s["w_md"] = self.dscr("w_md", [2, NE, DFE, D], BF16)
        self.scr = s

    def build(self):
        nc, k = self.nc, self.k
        with nc.allow_low_precision("bf16 matmul operands, fp32 accumulation"):
            with contextlib.ExitStack() as g:
                self.g = g
                self.consts(g)
                self.phase_convert()
                self.phase_mod()
                self.phase_x0()
                for li in self.layers:
                    j = li // 2
                    if li % 2 == 0:
                        self.phase_att1(li, j)
                        self.phase_att2(li, j)
                        self.phase_post(li, j, self.scr["w_o"][j])
                        self.phase_ffn(li, [(self.scr["w_fgu"][j], self.scr["w_fd"][j])], DFF, 2, gated=False)
                    else:
                        self.phase_lru1(li, j)
                        self.phase_lru2(li, j)
                        self.phase_post(li, j, self.scr["w_out"][j], router=True)
                        self.phase_ffn(li, [(self.scr["w_mgu"][j][e], self.scr["w_md"][j][e]) for e in range(NE)],
                                       DFE, 4, gated=True)
                if self.do_final:
                    self.phase_final()
                k.barrier()
            k.es.close()
        return nc

    def consts(self, g):
        k = self.k
        self.pspair = [k.ps(g, "pp%d" % i, [128, 2, 512], F32) for i in range(4)]
        self.psum = []
        for i in range(8):
            b = Buf(k, "ps%d" % i, HalfView(self.pspair[i // 2].t, i % 2))
            self.psum.append(b)
        self.ident = k.sb(g, "ident", [128, 128], F32)
        self.ones = k.sb(g, "ones", [128, 128], F32)
        self.modT = k.sb(g, "modT", [128, 4, 48, 2], F32)
        self.A1 = k.sb(g, "A1", [128, 4, 8, 2], F32)
        self.A2 = k.sb(g, "A2", [128, 4, 8, 2], F32)
        self.epsb = k.sb(g, "epsb", [128, 1], F32)
        k.load(self.ident[:], self.inp["ident"][:, :], W=[self.ident])
        k.load(self.ones[:], self.inp["ones"][:, :], W=[self.ones])
        k.I("dve", lambda e: e.memset(self.epsb[:], EPS), W=[self.epsb])

    def cvt_list(self, li):
        j = li // 2
        if li % 2 == 0:
            return [("attn_w_qkv", "w_qkv", j), ("attn_w_o", "w_o", j), ("ffn_w_gate_up", "w_fgu", j),
                    ("ffn_w_down", "w_fd", j)]
        return [("lru_w_in", "w_in", j), ("lru_w_out", "w_out", j), ("moe_w_gate_up", "w_mgu", j),
                ("moe_w_down", "w_md", j)]

    def cvt_chunks(self, pairs):
        k = self.k
        out = []
        for src, dst, j in pairs:
            a = self.inp[src][j]
            b = self.scr[dst][j]
            n = 1
            for s_ in a.shape:
                n *= s_
            a2 = a.flatten().rearrange("(r c) -> r c", c=2048)
            b2 = b.flatten().rearrange("(r c) -> r c", c=2048)
            rows = n // 2048
            step = 4096
            ds = DmaSem(k)
            rs = list(range(0, rows, step))
            for r0 in rs:
                r1 = min(rows, r0 + step)
                out.append((dst, j, a2[r0:r1, :], b2[r0:r1, :], ds, r0 == rs[-1]))
        return out

    def issue_chunk(self, ch):
        dst, j, a, b, ds, is_last = ch
        ins = self.k.engs["pool"].dma_start(out=b, in_=a)
        sem, val = ds.bump()
        ins.then_inc(sem, 16)
        if is_last:
            self.cvt_tag[(dst, j)] = ("d", ds.sem, ds.count, id(ds.sem))

    def issue_cvt(self, pairs):
        for ch in self.cvt_chunks(pairs):
            self.issue_chunk(ch)

    def wait_cvt(self, dst, j):
        tag = self.cvt_tag.get((dst, j))
        if tag is not None:
            self.k._wait("sp", tag)

    def phase_convert(self):
        k = self.k
        self.cvt_tag = {}
        self.pending_cvt = []
        first = True
        for li in self.layers:
            if first or li % 2 == 1 and not any(l % 2 == 0 for l in self.layers):
                self.issue_cvt(self.cvt_list(li))
            else:
                self.pending_cvt += self.cvt_list(li)
            first = False
        if not any(l % 2 == 0 for l in self.layers) and self.pending_cvt:
            self.issue_cvt(self.pending_cvt)
            self.pending_cvt = []

    def phase_mod(self):
        k, nc = self.k, self.nc
        with contextlib.ExitStack() as st:
            cin = k.sb(st, "cin", [128, 8, 2], F32)
            sc = k.sb(st, "sc", [128, 8, 2], F32)
            wt = [k.sb(st, "modw%d" % i, [128, 6 * D], F32) for i in range(2)]
            mb = k.sb(st, "mb", [128, 48], F32)
            gm = k.sb(st, "gm", [128, 8], F32)
            k.load(cin[:, :, 0], self.inp["c"].rearrange("(k p) -> p k", p=128), W=[cin],
                   allow_slow_non_contiguous=True)
            k.load(cin[:, :, 1], self.inp["c_ctx"].rearrange("(k p) -> p k", p=128), W=[cin], add=True,
                   allow_slow_non_contiguous=True)
            k.I("act", lambda e: e.activation(out=sc[:], in_=cin[:], func=AF.Silu), R=[cin], W=[sc])
            n = 0
            macc = k.sb(st, "macc", [128, 96], F32)
            for li in self.layers:
                k.load(mb[:], self.inp["mod_b"][li].rearrange("(j p) -> p j", p=128), W=[mb],
                       allow_slow_non_contiguous=True)
                for kc in range(KC):
                    ps = self.psum[kc % 2]
                    w = wt[n % 2]
                    n += 1
                    k.load(w[:], self.inp["mod_w"][li][kc * 128:(kc + 1) * 128, :], W=[w])
                    for jc in range(48):
                        k.I("pe", lambda e, w=w, jc=jc, kc=kc, ps=ps: e.matmul(
                            ps[:, jc * 2:jc * 2 + 2], lhsT=w[:, jc * 128:(jc + 1) * 128], rhs=sc[:, kc, :],
                            start=True, stop=True), R=[w, sc], W=[ps])
                    if kc == 0:
                        k.I("dve", lambda e, ps=ps: e.tensor_copy(out=macc[:], in_=ps[:, 0:96]), R=[ps], W=[macc])
                    else:
                        k.I("dve", lambda e, ps=ps: e.tensor_tensor(out=macc[:], in0=ps[:, 0:96], in1=macc[:], op=ALU.add),
                            R=[ps, macc], W=[macc])
                for t in range(2):
                    k.I("dve", lambda e, t=t, li=li: e.tensor_tensor(
                        out=self.modT[:, li, :, t], in0=macc[:].rearrange("p (j t) -> p j t", t=2)[:, :, t],
                        in1=mb[:], op=ALU.add), R=[macc, mb], W=[self.modT])
                for (A, gname, off) in ((self.A1, "norm_mix_g", 8), (self.A2, "norm_ffn_g", 32)):
                    k.load(gm[:], self.inp[gname][li].rearrange("(k p) -> p k", p=128), W=[gm],
                           allow_slow_non_contiguous=True)
                    for t in range(2):
                        k.I("dve", lambda e, A=A, off=off, t=t, li=li: e.scalar_tensor_tensor(
                            out=A[:, li, :, t], in0=self.modT[:, li, off:off + 8, t], scalar=1.0, in1=gm[:],
                            op0=ALU.add, op1=ALU.mult), R=[self.modT, gm], W=[A])
            k.barrier()

    def mv(self, li, which, kc, isctx):
        return self.modT[:, li, which * 8 + kc, (1 if isctx else 0):(2 if isctx else 1)]

    def phase_x0(self):
        k = self.k
        XT = self.scr["XT"].rearrange("(k p) t -> p k t", p=128)
        with contextlib.ExitStack() as st:
            xin = [k.sb(st, "xin%d" % i, [128, 4, D], F32) for i in range(2)]
            xo = [k.sb(st, "xo%d" % i, [128, 8, 512], F32) for i in range(2)]
            for bi, (t0, n, isctx) in enumerate(self.blocks):
                xi, o = xin[bi % 2], xo[bi % 2]
                nt = n // 128
                src = self.inp["ctx"] if isctx else self.inp["x"][t0 - CTX:t0 - CTX + n, :]
                k.load(xi[:, 0:nt, :], src.rearrange("(j p) d -> p j d", p=128), W=[xi])
                for kc in range(KC):
                    ps = self.psum[kc]
                    for j in range(nt):
                        k.I("pe", lambda e, ps=ps, j=j, kc=kc, xi=xi: e.transpose(
                            ps[:, j * 128:(j + 1) * 128], xi[:, j, kc * 128:(kc + 1) * 128], self.ident[:]),
                            R=[xi, self.ident], W=[ps])
                    eng = "act" if kc % 2 == 0 else "dve"
                    if eng == "act":
                        k.I("act", lambda e, ps=ps, kc=kc, o=o: e.copy(out=o[:, kc, 0:n], in_=ps[:, 0:n]), R=[ps], W=[o])
                    else:
                        k.I("dve", lambda e, ps=ps, kc=kc, o=o: e.tensor_copy(out=o[:, kc, 0:n], in_=ps[:, 0:n]), R=[ps], W=[o])
                k.store(XT[:, :, t0:t0 + n], o[:, :, 0:n], R=[o])
            k.barrier()

    def modulate(self, st_tiles, x, n, li, A, shw, isctx, out_bf=None, out_f32=None, psb=None):
        k = self.k
        sq, rstd, tmp = st_tiles
        ps = psb
        for kc in range(KC):
            k.I("act", lambda e, kc=kc: e.activation(out=sq[:, 0:n], in_=x[:, kc, 0:n], func=AF.Square), R=[x], W=[sq])
            k.I("pe", lambda e, kc=kc: e.matmul(ps[:, 0:n], lhsT=self.ones[:], rhs=sq[:, 0:n],
                                                  start=(kc == 0), stop=(kc == KC - 1)), R=[sq, self.ones], W=[ps])
        k.I("act", lambda e: e.activation(out=rstd[:, 0:n], in_=ps[:, 0:n], func=AF.Sqrt, bias=self.epsb[:],
                                          scale=1.0 / D), R=[ps, self.epsb], W=[rstd])
        k.I("dve", lambda e: e.reciprocal(out=rstd[:, 0:n], in_=rstd[:, 0:n]), R=[rstd], W=[rstd])
        tcol = 1 if isctx else 0
        for kc in range(KC):
            k.I("dve", lambda e, kc=kc: e.tensor_tensor(out=tmp[:, 0:n], in0=x[:, kc, 0:n], in1=rstd[:, 0:n],
                                                        op=ALU.mult), R=[x, rstd], W=[tmp])
            Ac = A[:, li, kc, tcol:tcol + 1]
            sh = self.mv(li, shw, kc, isctx)
            if out_f32 is not None:
                k.I("act", lambda e, kc=kc, Ac=Ac, sh=sh: e.activation(out=out_f32[:, kc, 0:n], in_=tmp[:, 0:n],
                    func=AF.Identity, bias=sh, scale=Ac), R=[tmp, A, self.modT], W=[out_f32])
                k.I("pool", lambda e, kc=kc: e.tensor_copy(out=out_bf[:, kc, 0:n], in_=out_f32[:, kc, 0:n]),
                    R=[out_f32], W=[out_bf])
            else:
                k.I("act", lambda e, kc=kc, Ac=Ac, sh=sh: e.activation(out=out_bf[:, kc, 0:n], in_=tmp[:, 0:n],
                    func=AF.Identity, bias=sh, scale=Ac), R=[tmp, A, self.modT], W=[out_bf])

    def phase_post(self, li, j, w_o, router=False):
        k = self.k
        XT = self.scr["XT"].rearrange("(k p) t -> p k t", p=128)
        ST = self.scr["ST"].rearrange("(k p) t -> p k t", p=128)
        FT = self.scr["FT"].rearrange("(k p) t -> p k t", p=128)
        with contextlib.ExitStack() as st:
            wo = k.sb(st, "wo", [128, 8, D], BF16)
            self.wait_cvt("w_out" if router else "w_o", j)
            k.load(wo[:], w_o.rearrange("(k p) c -> p k c", p=128), W=[wo])
            s_in = [k.sb(st, "s_in%d" % i, [128, 8, 512], BF16) for i in range(2)]
            xt = [k.sb(st, "xt%d" % i, [128, 8, 512], F32) for i in range(2)]
            fb = [k.sb(st, "fb%d" % i, [128, 8, 512], BF16) for i in range(2)]
            sq = k.sb(st, "sq", [128, 512], F32)
            rstd = k.sb(st, "rstd", [128, 512], F32)
            tmp = k.sb(st, "tmp", [128, 512], F32)
            if router:
                f32 = [k.sb(st, "f32_%d" % i, [128, 8, 512], F32) for i in range(2)]
                wr = k.sb(st, "wr", [128, 8, NE], F32)
                k.load(wr[:], self.inp["moe_router_w"][j].rearrange("(k p) e -> p k e", p=128), W=[wr])
                lg = k.sb(st, "lg", [128, 8], F32)
                m8 = k.sb(st, "m8", [128, 8], F32)
                sm = k.sb(st, "sm", [128, 8], F32)
                eq1 = k.sb(st, "eq1", [128, 8], F32)
                eq2 = k.sb(st, "eq2", [128, 8], F32)
                gt = k.sb(st, "gt", [128, 8], F32)
                gT = [k.sb(st, "gT%d" % i, [8, 512], F32) for i in range(2)]
            for bi, (t0, n, isctx) in enumerate(self.blocks):
                si, x, f = s_in[bi % 2], xt[bi % 2], fb[bi % 2]
                k.load(si[:, :, 0:n], ST[:, :, t0:t0 + n], W=[si])
                k.load(x[:, :, 0:n], XT[:, :, t0:t0 + n], W=[x])
                for dc in range(KC):
                    ps = self.psum[dc % 4]
                    for h in range(KC):
                        k.I("pe", lambda e, ps=ps, h=h, dc=dc: e.matmul(
                            ps[:, 0:n], lhsT=wo[:, h, dc * 128:(dc + 1) * 128], rhs=si[:, h, 0:n],
                            start=(h == 0), stop=(h == KC - 1)), R=[wo, si], W=[ps])
                    g1 = self.mv(li, 2, dc, isctx)
                    k.I("dve", lambda e, ps=ps, dc=dc, g1=g1: e.scalar_tensor_tensor(
                        out=x[:, dc, 0:n], in0=ps[:, 0:n], scalar=g1, in1=x[:, dc, 0:n], op0=ALU.mult, op1=ALU.add),
                        R=[ps, x, self.modT], W=[x])
                k.store(XT[:, :, t0:t0 + n], x[:, :, 0:n], R=[x])
                if router:
                    ff = f32[bi % 2]
                    self.modulate((sq, rstd, tmp), x, n, li, self.A2, 3, isctx, out_bf=f, out_f32=ff, psb=self.psum[4])
                else:
                    self.modulate((sq, rstd, tmp), x, n, li, self.A2, 3, isctx, out_bf=f, psb=self.psum[4])
                k.store(FT[:, :, t0:t0 + n], f[:, :, 0:n], R=[f])
                if router:
                    gTb = gT[bi % 2]
                    for jt in range(n // 128):
                        psl = self.psum[5]
                        for kc in range(KC):
                            k.I("pe", lambda e, kc=kc, jt=jt: e.matmul(
                                psl[:, 0:NE], lhsT=ff[:, kc, jt * 128:(jt + 1) * 128], rhs=wr[:, kc, :],
                                start=(kc == 0), stop=(kc == KC - 1)), R=[ff, wr], W=[psl])
                        k.I("dve", lambda e: e.tensor_copy(out=lg[:], in_=psl[:, 0:NE]), R=[psl], W=[lg])
                        k.I("dve", lambda e: e.max(out=m8[:], in_=lg[:]), R=[lg], W=[m8])
                        k.I("dve", lambda e: e.tensor_tensor(out=sm[:, 0:1], in0=m8[:, 1:2], in1=m8[:, 0:1], op=ALU.subtract),
                            R=[m8], W=[sm])
                        k.I("act", lambda e: e.activation(out=sm[:, 1:2], in_=sm[:, 0:1], func=AF.Exp), R=[sm], W=[sm])
                        k.I("dve", lambda e: e.tensor_scalar_add(out=sm[:, 2:3], in0=sm[:, 1:2], scalar1=1.0), R=[sm], W=[sm])
                        k.I("dve", lambda e: e.reciprocal(out=sm[:, 3:4], in_=sm[:, 2:3]), R=[sm], W=[sm])
                        k.I("dve", lambda e: e.tensor_tensor(out=sm[:, 4:5], in0=sm[:, 1:2], in1=sm[:, 3:4], op=ALU.mult),
                            R=[sm], W=[sm])
                        k.I("dve", lambda e: e.tensor_scalar(out=eq1[:], in0=lg[:], scalar1=m8[:, 0:1], scalar2=sm[:, 3:4],
                                                             op0=ALU.is_equal, op1=ALU.mult), R=[lg, m8, sm], W=[eq1])
                        k.I("dve", lambda e: e.tensor_scalar(out=eq2[:], in0=lg[:], scalar1=m8[:, 1:2], scalar2=sm[:, 4:5],
                                                             op0=ALU.is_equal, op1=ALU.mult), R=[lg, m8, sm], W=[eq2])
                        k.I("dve", lambda e: e.tensor_tensor(out=gt[:], in0=eq1[:], in1=eq2[:], op=ALU.add),
                            R=[eq1, eq2], W=[gt])
                        pst = self.psum[6]
                        k.I("pe", lambda e: e.transpose(pst[0:8, 0:128], gt[:], self.ident[:]), R=[gt, self.ident], W=[pst])
                        k.I("act", lambda e, jt=jt: e.copy(out=gTb[:, jt * 128:(jt + 1) * 128], in_=pst[0:8, 0:128]),
                            R=[pst], W=[gTb])
                    k.store(self.scr["G"][:, t0:t0 + n], gTb[:, 0:n], R=[gTb])
            k.barrier()

    def phase_ffn(self, li, experts, F, GC, gated):
        k = self.k
        XT = self.scr["XT"].rearrange("(k p) t -> p k t", p=128)
        FT = self.scr["FT"].rearrange("(k p) t -> p k t", p=128)
        ngroups = F // (GC * 128)
        assert ngroups * GC * 128 == F
        NSB = max(sum(b[1] for b in sb) for sb in self.superblocks)
        with contextlib.ExitStack() as st:
            jj = li // 2
            for nm in (("w_mgu", "w_md") if gated else ("w_fgu", "w_fd")):
                self.wait_cvt(nm, jj)
            fT = k.sb(st, "fT", [128, 8, NSB], BF16)
            yacc = k.sb(st, "yacc", [128, 8, NSB], F32)
            wg = [k.sb(st, "wg%d" % i, [128, 8, GC * 128], BF16) for i in range(2)]
            wu = [k.sb(st, "wu%d" % i, [128, 8, GC * 128], BF16) for i in range(2)]
            wd = [k.sb(st, "wd%d" % i, [128, GC, D], BF16) for i in range(2)]
            act = [k.sb(st, "act%d" % i, [128, GC, 512], BF16) for i in range(2)]
            sg = [k.sb(st, "sg%d" % i, [128, 512], F32) for i in range(2)]
            tg = [k.sb(st, "tg%d" % i, [128, 512], F32) for i in range(2)]
            gB = [k.sb(st, "gB%d" % i, [128, NSB], F32) for i in range(2)] if gated else None
            xt = [k.sb(st, "xt%d" % i, [128, 8, 512], F32) for i in range(2)]
            nw = 0
            na = 0
            for sb in self.superblocks:
                s0 = sb[0][0]
                ns = sum(b[1] for b in sb)
                k.load(fT[:, :, 0:ns], FT[:, :, s0:s0 + ns], W=[fT])
                first = True
                for ei, (wgu_d, wd_d) in enumerate(experts):
                    wgu_v = wgu_d.rearrange("(k p) c -> p k c", p=128)
                    wd_v = wd_d.rearrange("(c p) d -> p c d", p=128)
                    if gated:
                        gb = gB[ei % 2]
                        k.load(gb[:, 0:ns], self.scr["G"][ei:ei + 1, s0:s0 + ns].partition_broadcast(128), W=[gb])
                    for gi in range(ngroups):
                        a, b_, c_ = wg[nw % 2], wu[nw % 2], wd[nw % 2]
                        nw += 1
                        c0 = gi * GC * 128
                        k.load(a[:], wgu_v[:, :, c0:c0 + GC * 128], W=[a])
                        k.load(b_[:], wgu_v[:, :, F + c0:F + c0 + GC * 128], W=[b_])
                        k.load(c_[:], wd_v[:, gi * GC:(gi + 1) * GC, :], W=[c_])
                        for (t0, n, isctx) in sb:
                            o = t0 - s0
                            ac = act[na % 2]
                            na += 1
                            for c in range(GC):
                                pg = self.psum[(2 * c) % 4]
                                pu = self.psum[(2 * c + 1) % 4]
                                for kc in range(KC):
                                    k.I("pe", lambda e, pg=pg, c=c, kc=kc, a=a: e.matmul(
                                        pg[:, 0:n], lhsT=a[:, kc, c * 128:(c + 1) * 128], rhs=fT[:, kc, o:o + n],
                                        start=(kc == 0), stop=(kc == KC - 1)), R=[a, fT], W=[pg])
                                for kc in range(KC):
                                    k.I("pe", lambda e, pu=pu, c=c, kc=kc, b_=b_: e.matmul(
                                        pu[:, 0:n], lhsT=b_[:, kc, c * 128:(c + 1) * 128], rhs=fT[:, kc, o:o + n],
                                        start=(kc == 0), stop=(kc == KC - 1)), R=[b_, fT], W=[pu])
                                s_ = sg[c % 2]
                                k.I("act", lambda e, s_=s_, pg=pg: e.activation(out=s_[:, 0:n], in_=pg[:, 0:n], func=AF.Silu),
                                    R=[pg], W=[s_])
                                if gated:
                                    t_ = tg[c % 2]
                                    k.I("dve", lambda e, t_=t_, pu=pu, gb=gb: e.tensor_tensor(
                                        out=t_[:, 0:n], in0=pu[:, 0:n], in1=gb[:, o:o + n], op=ALU.mult), R=[pu, gb], W=[t_])
                                    k.I("pool", lambda e, t_=t_, s_=s_, c=c, ac=ac: e.tensor_tensor(
                                        out=ac[:, c, 0:n], in0=s_[:, 0:n], in1=t_[:, 0:n], op=ALU.mult), R=[s_, t_], W=[ac])
                                else:
                                    k.I("dve", lambda e, pu=pu, s_=s_, c=c, ac=ac: e.tensor_tensor(
                                        out=ac[:, c, 0:n], in0=pu[:, 0:n], in1=s_[:, 0:n], op=ALU.mult), R=[pu, s_], W=[ac])
                            for dc in range(KC):
                                py = self.psum[4 + dc % 4]
                                for c in range(GC):
                                    k.I("pe", lambda e, py=py, c=c, dc=dc, c_=c_, ac=ac: e.matmul(
                                        py[:, 0:n], lhsT=c_[:, c, dc * 128:(dc + 1) * 128], rhs=ac[:, c, 0:n],
                                        start=(c == 0), stop=(c == GC - 1)), R=[c_, ac], W=[py])
                                if first and gi == 0:
                                    k.I("dve", lambda e, py=py, dc=dc: e.tensor_copy(out=yacc[:, dc, o:o + n], in_=py[:, 0:n]),
                                        R=[py], W=[yacc])
                                else:
                                    k.I("dve", lambda e, py=py, dc=dc: e.tensor_tensor(
                                        out=yacc[:, dc, o:o + n], in0=py[:, 0:n], in1=yacc[:, dc, o:o + n], op=ALU.add),
                                        R=[py, yacc], W=[yacc])
                    first = False
                for bi, (t0, n, isctx) in enumerate(sb):
                    o = t0 - s0
                    x = xt[bi % 2]
                    k.load(x[:, :, 0:n], XT[:, :, t0:t0 + n], W=[x])
                    for dc in range(KC):
                        g2 = self.mv(li, 5, dc, isctx)
                        k.I("dve", lambda e, dc=dc, g2=g2, x=x: e.scalar_tensor_tensor(
                            out=x[:, dc, 0:n], in0=yacc[:, dc, o:o + n], scalar=g2, in1=x[:, dc, 0:n],
                            op0=ALU.mult, op1=ALU.add), R=[yacc, x, self.modT], W=[x])
                    k.store(XT[:, :, t0:t0 + n], x[:, :, 0:n], R=[x])
            k.barrier()

    def phase_final(self):
        k = self.k
        XT = self.scr["XT"].rearrange("(k p) t -> p k t", p=128)
        with contextlib.ExitStack() as st:
            gf = k.sb(st, "gf", [128, 8], F32)
            k.load(gf[:], self.inp["final_norm_g"].rearrange("(k p) -> p k", p=128), W=[gf], allow_slow_non_contiguous=True)
            xt = [k.sb(st, "xt%d" % i, [128, 8, 512], F32) for i in range(2)]
            yo = [k.sb(st, "yo%d" % i, [128, 4, D], F32) for i in range(2)]
            sq = k.sb(st, "sq", [128, 512], F32)
            rstd = k.sb(st, "rstd", [128, 512], F32)
            for bi, (t0, n, isctx) in enumerate(self.blocks):
                if isctx:
                    continue
                x, y = xt[bi % 2], yo[bi % 2]
                k.load(x[:, :, 0:n], XT[:, :, t0:t0 + n], W=[x])
                ps = self.psum[0]
                for kc in range(KC):
                    k.I("act", lambda e, kc=kc: e.activation(out=sq[:, 0:n], in_=x[:, kc, 0:n], func=AF.Square), R=[x], W=[sq])
                    k.I("pe", lambda e, kc=kc: e.matmul(ps[:, 0:n], lhsT=self.ones[:], rhs=sq[:, 0:n],
                                                          start=(kc == 0), stop=(kc == KC - 1)), R=[sq, self.ones], W=[ps])
                k.I("act", lambda e: e.activation(out=rstd[:, 0:n], in_=ps[:, 0:n], func=AF.Sqrt, bias=self.epsb[:],
                                                  scale=1.0 / D), R=[ps, self.epsb], W=[rstd])
                k.I("dve", lambda e: e.reciprocal(out=rstd[:, 0:n], in_=rstd[:, 0:n]), R=[rstd], W=[rstd])
                for kc in range(KC):
                    k.I("dve", lambda e, kc=kc: e.scalar_tensor_tensor(
                        out=x[:, kc, 0:n], in0=x[:, kc, 0:n], scalar=gf[:, kc:kc + 1], in1=rstd[:, 0:n],
                        op0=ALU.mult, op1=ALU.mult), R=[x, gf, rstd], W=[x])
                for jt in range(n // 128):
                    for half in range(2):
                        pt = self.psum[1 + (jt * 2 + half) % 4]
                        for q in range(4):
                            kc = half * 4 + q
                            k.I("pe", lambda e, pt=pt, q=q, kc=kc, jt=jt: e.transpose(
                                pt[:, q * 128:(q + 1) * 128], x[:, kc, jt * 128:(jt + 1) * 128], self.ident[:]),
                                R=[x, self.ident], W=[pt])
                        if half == 0:
                            k.I("act", lambda e, pt=pt, jt=jt: e.copy(out=y[:, jt, 0:512], in_=pt[:, 0:512]), R=[pt], W=[y])
                        else:
                            k.I("dve", lambda e, pt=pt, jt=jt: e.tensor_copy(out=y[:, jt, 512:1024], in_=pt[:, 0:512]), R=[pt], W=[y])
                k.store(self.out[t0 - CTX:t0 - CTX + n, :].rearrange("(j p) d -> p j d", p=128), y[:, 0:n // 128, :], R=[y])
            k.barrier()

    def rope_tables(self, st, t0, n, T_):
        k = self.k
        rowf, colf, pos, u, ki, kf, cosT, sinT, rc = T_
        base_r = float((t0 - CTX) // 64)
        k.I("dve", lambda e: e.tensor_scalar(out=pos[:, 0:n], in0=rowf[:, 0:n], scalar1=base_r, scalar2=rc[:, 0:1],
                                             op0=ALU.add, op1=ALU.mult), R=[rowf, rc], W=[pos])
        k.I("dve", lambda e: e.scalar_tensor_tensor(out=pos[:, 0:n], in0=colf[:, 0:n], scalar=rc[:, 1:2], in1=pos[:, 0:n],
                                                    op0=ALU.mult, op1=ALU.add), R=[colf, rc, pos], W=[pos])
        for (off, outT, sc_col, bi_col) in ((0.5, sinT, 3, 4), (0.75, cosT, 5, 6)):
            k.I("dve", lambda e, off=off: e.tensor_scalar(out=u[:, 0:n], in0=pos[:, 0:n], scalar1=rc[:, 2:3], scalar2=off,
                                                          op0=ALU.mult, op1=ALU.add), R=[pos, rc], W=[u])
            k.I("dve", lambda e: e.tensor_copy(out=ki[:, 0:n], in_=u[:, 0:n]), R=[u], W=[ki])
            k.I("dve", lambda e: e.tensor_copy(out=kf[:, 0:n], in_=ki[:, 0:n]), R=[ki], W=[kf])
            k.I("dve", lambda e: e.tensor_tensor(out=u[:, 0:n], in0=u[:, 0:n], in1=kf[:, 0:n], op=ALU.subtract),
                R=[u, kf], W=[u])
            k.I("dve", lambda e: e.tensor_single_scalar(out=kf[:, 0:n], in_=u[:, 0:n], scalar=0.0, op=ALU.is_lt),
                R=[u], W=[kf])
            k.I("dve", lambda e: e.tensor_tensor(out=u[:, 0:n], in0=u[:, 0:n], in1=kf[:, 0:n], op=ALU.add),
                R=[u, kf], W=[u])
            k.I("act", lambda e, outT=outT, sc_col=sc_col, bi_col=bi_col: e.activation(
                out=outT[:, 0:n], in_=u[:, 0:n], func=AF.Sin, bias=rc[:, bi_col:bi_col + 1],
                scale=rc[:, sc_col:sc_col + 1]), R=[u, rc], W=[outT])

    def phase_att1(self, li, j):
        k = self.k
        XT = self.scr["XT"].rearrange("(k p) t -> p k t", p=128)
        QT = self.scr["QT"].rearrange("(k p) t -> p k t", p=128)
        KT = self.scr["KT"].rearrange("(k p) t -> p k t", p=128)
        with contextlib.ExitStack() as st:
            wq = k.sb(st, "wq", [128, 8, 3 * D], BF16)
            self.wait_cvt("w_qkv", j)
            wv = self.scr["w_qkv"][j].rearrange("(k p) c -> p k c", p=128)
            for kc in range(KC):
                k.load(wq[:, kc, :], wv[:, kc, :], W=[wq], add=True)
            rc = k.sb(st, "rc", [128, 8], F32)
            k.load(rc[:], self.inp["ropec"][:, :], W=[rc])
            pf = k.sb(st, "pf", [128, 128], F32)
            k.load(pf[:], self.inp["perm"][:, :], W=[pf])
            pb = k.sb(st, "pb", [128, 128], BF16)
            k.I("dve", lambda e: e.tensor_copy(out=pb[:], in_=pf[:]), R=[pf], W=[pb])
            ri = k.sb(st, "ri", [128, 8, 64], I32)
            ci = k.sb(st, "ci", [128, 8, 64], I32)
            rowf = k.sb(st, "rowf", [128, 512], F32)
            colf = k.sb(st, "colf", [128, 512], F32)
            k.I("pool", lambda e: e.iota(ri[:], pattern=[[1, 8], [0, 64]], base=0, channel_multiplier=0), W=[ri])
            k.I("pool", lambda e: e.iota(ci[:], pattern=[[0, 8], [1, 64]], base=0, channel_multiplier=0), W=[ci])
            k.I("dve", lambda e: e.tensor_copy(out=rowf[:], in_=ri[:].rearrange("p a b -> p (a b)")), R=[ri], W=[rowf])
            k.I("dve", lambda e: e.tensor_copy(out=colf[:], in_=ci[:].rearrange("p a b -> p (a b)")), R=[ci], W=[colf])
            pos = k.sb(st, "pos", [128, 512], F32)
            u = k.sb(st, "u", [128, 512], F32)
            ki = k.sb(st, "ki", [128, 512], I32)
            kf = k.sb(st, "kf", [128, 512], F32)
            cosT = k.sb(st, "cosT", [128, 512], F32)
            sinT = k.sb(st, "sinT", [128, 512], F32)
            xt = [k.sb(st, "xt%d" % i, [128, 8, 512], F32) for i in range(2)]
            hb = [k.sb(st, "hb%d" % i, [128, 8, 512], BF16) for i in range(2)]
            qo = [k.sb(st, "qo%d" % i, [128, 8, 512], BF16) for i in range(2)]
            ko = [k.sb(st, "ko%d" % i, [128, 8, 512], BF16) for i in range(2)]
            vb = [k.sb(st, "vb%d" % i, [128, 4, D], BF16) for i in range(2)]
            qb = [k.sb(st, "qb%d" % i, [128, 512], BF16) for i in range(2)]
            t1 = [k.sb(st, "t1_%d" % i, [128, 512], F32) for i in range(2)]
            t2 = [k.sb(st, "t2_%d" % i, [128, 512], F32) for i in range(2)]
            sq = k.sb(st, "sq", [128, 512], F32)
            rstd = k.sb(st, "rstd", [128, 512], F32)
            tmp = k.sb(st, "tmp", [128, 512], F32)
            nq = 0
            for bi, (t0, n, isctx) in enumerate(self.blocks):
                x, h, qq, kk, v = xt[bi % 2], hb[bi % 2], qo[bi % 2], ko[bi % 2], vb[bi % 2]
                k.load(x[:, :, 0:n], XT[:, :, t0:t0 + n], W=[x])
                self.modulate((sq, rstd, tmp), x, n, li, self.A1, 0, isctx, out_bf=h, psb=self.psum[7])
                if not isctx:
                    self.rope_tables(st, t0, n, (rowf, colf, pos, u, ki, kf, cosT, sinT, rc))
                for hd in range(NH):
                    for (isq, col0, dst) in ((True, hd * 128, qq), (False, D + hd * 128, kk)):
                        ps = self.psum[nq % 3]
                        pr = self.psum[3 + nq % 3]
                        for kc in range(KC):
                            k.I("pe", lambda e, ps=ps, kc=kc, col0=col0: e.matmul(
                                ps[:, 0:n], lhsT=wq[:, kc, col0:col0 + 128], rhs=h[:, kc, 0:n],
                                start=(kc == 0), stop=(kc == KC - 1)), R=[wq, h], W=[ps])
                        scl = 0.125 if isq else 1.0
                        if isctx:
                            k.I("act", lambda e, ps=ps, dst=dst, hd=hd, scl=scl: e.activation(
                                out=dst[:, hd, 0:n], in_=ps[:, 0:n], func=AF.Copy, scale=scl), R=[ps], W=[dst])
                        else:
                            b_ = qb[nq % 2]
                            a1, a2 = t1[nq % 2], t2[nq % 2]
                            k.I("act", lambda e, ps=ps, b_=b_, scl=scl: e.activation(
                                out=b_[:, 0:n], in_=ps[:, 0:n], func=AF.Copy, scale=scl), R=[ps], W=[b_])
                            k.I("pe", lambda e, pr=pr, b_=b_: e.matmul(pr[:, 0:n], lhsT=pb[:], rhs=b_[:, 0:n],
                                                                       start=True, stop=True), R=[pb, b_], W=[pr])
                            k.I("dve", lambda e, a1=a1, b_=b_: e.tensor_tensor(out=a1[:, 0:n], in0=b_[:, 0:n], in1=cosT[:, 0:n],
                                                                               op=ALU.mult), R=[b_, cosT], W=[a1])
                            k.I("dve", lambda e, a2=a2, pr=pr: e.tensor_tensor(out=a2[:, 0:n], in0=pr[:, 0:n], in1=sinT[:, 0:n],
                                                                               op=ALU.mult), R=[pr, sinT], W=[a2])
                            k.I("pool", lambda e, a1=a1, a2=a2, dst=dst, hd=hd: e.tensor_tensor(
                                out=dst[:, hd, 0:n], in0=a1[:, 0:n], in1=a2[:, 0:n], op=ALU.add), R=[a1, a2], W=[dst])
                        nq += 1
                k.store(QT[:, :, t0:t0 + n], qq[:, :, 0:n], R=[qq])
                k.store(KT[:, :, t0:t0 + n], kk[:, :, 0:n], R=[kk])
                for jt in range(n // 128):
                    for half in range(2):
                        ps = self.psum[6 + half]
                        for kc in range(KC):
                            k.I("pe", lambda e, ps=ps, kc=kc, jt=jt, half=half: e.matmul(
                                ps[:, 0:512], lhsT=h[:, kc, jt * 128:(jt + 1) * 128],
                                rhs=wq[:, kc, 2 * D + half * 512:2 * D + (half + 1) * 512],
                                start=(kc == 0), stop=(kc == KC - 1)), R=[wq, h], W=[ps])
                        if half == 0:
                            k.I("act", lambda e, ps=ps, jt=jt: e.copy(out=v[:, jt, 0:512], in_=ps[:, 0:512]), R=[ps], W=[v])
                        else:
                            k.I("dve", lambda e, ps=ps, jt=jt: e.tensor_copy(out=v[:, jt, 512:1024], in_=ps[:, 0:512]),
                                R=[ps], W=[v])
                k.store(self.scr["V"][t0:t0 + n, :].rearrange("(j p) d -> p j d", p=128), v[:, 0:n // 128, :], R=[v])
            k.barrier()

    def phase_att2(self, li, j):
        k = self.k
        T = self.T
        NT = T // 128
        lambda_init = 0.8 - 0.6 * math.exp(-0.3 * li)
        with contextlib.ExitStack() as st:
            lv = k.sb(st, "lv", [128, 256], F32)
            k.load(lv[:], self.inp["attn_lambda"][j:j + 1, :].partition_broadcast(128), W=[lv])
            lp = k.sb(st, "lp", [128, 128], F32)
            ls = k.sb(st, "ls", [128, 4], F32)
            k.I("dve", lambda e: e.tensor_tensor(out=lp[:, 0:64], in0=lv[:, 0:64], in1=lv[:, 64:128], op=ALU.mult), R=[lv], W=[lp])
            k.I("dve", lambda e: e.tensor_tensor(out=lp[:, 64:128], in0=lv[:, 128:192], in1=lv[:, 192:256], op=ALU.mult),
                R=[lv], W=[lp])
            k.I("dve", lambda e: e.reduce_sum(out=ls[:, 0:1], in_=lp[:, 0:64], axis=AX.X), R=[lp], W=[ls])
            k.I("dve", lambda e: e.reduce_sum(out=ls[:, 1:2], in_=lp[:, 64:128], axis=AX.X), R=[lp], W=[ls])
            k.I("act", lambda e: e.activation(out=ls[:, 0:2], in_=ls[:, 0:2], func=AF.Exp), R=[ls], W=[ls])
            k.I("dve", lambda e: e.tensor_tensor(out=ls[:, 2:3], in0=ls[:, 1:2], in1=ls[:, 0:1], op=ALU.subtract), R=[ls], W=[ls])
            k.I("dve", lambda e: e.tensor_scalar_add(out=ls[:, 2:3], in0=ls[:, 2:3], scalar1=-lambda_init), R=[ls], W=[ls])
            sgl = k.sb(st, "sgl", [128, 1], F32)
            k.load(sgl[:], self.inp["attn_subln_g"][j].rearrange("(p o) -> p o", o=1), W=[sgl])
            k.I("dve", lambda e: e.tensor_scalar_mul(out=sgl[:], in0=sgl[:], scalar1=1.0 - lambda_init), R=[sgl], W=[sgl])
            onesb = k.sb(st, "onesb", [128, 128], BF16)
            k.I("dve", lambda e: e.tensor_copy(out=onesb[:], in_=self.ones[:]), R=[self.ones], W=[onesb])
            kT = [k.sb(st, "kT%d" % i, [128, T], BF16) for i in range(2)]
            vh = [k.sb(st, "vh%d" % i, [128, NT, 128], BF16) for i in range(2)]
            qt = [k.sb(st, "qt%d" % i, [128, 512], BF16) for i in range(2)]
            ep = [k.sb(st, "ep%d" % i, [128, 2, 512], BF16) for i in range(4)]
            gs0 = [k.sb(st, "gs0_%d" % i, [128, 512], BF16) for i in range(2)]
            gs1 = [k.sb(st, "gs1_%d" % i, [128, 512], BF16) for i in range(2)]
            es0 = k.sb(st, "es0", [128, 512], F32)
            es1 = k.sb(st, "es1", [128, 512], F32)
            r0 = k.sb(st, "r0", [128, 512], F32)
            r1 = k.sb(st, "r1", [128, 512], F32)
            o0 = k.sb(st, "o0", [128, 512], F32)
            o1 = k.sb(st, "o1", [128, 512], F32)
            sq = k.sb(st, "sq", [128, 512], F32)
            ob = [k.sb(st, "ob%d" % i, [128, 512], BF16) for i in range(2)]
            acc0, acc1 = self.psum[6], self.psum[7]
            Zp = self.psum[4]
            Zq = self.psum[5]
            bg = self.cvt_chunks(self.pending_cvt) if self.pending_cvt else []
            self.pending_cvt = []
            GRP = 6
            nb = 0
            ne = 0
            ng = 0
            Vv = self.scr["V"].rearrange("(kt p) d -> p kt d", p=128)
            for hd in range(NH):
                kt_, v_ = kT[hd % 2], vh[hd % 2]
                k.load(kt_[:], self.scr["KT"][hd * 128:(hd + 1) * 128, :], W=[kt_])
                k.load(v_[:], Vv[:, :, hd * 128:(hd + 1) * 128], W=[v_])
                for (t0, n, isctx) in self.blocks:
                    q = qt[nb % 2]
                    o_ = ob[nb % 2]
                    nb += 1
                    k.load(q[:, 0:n], self.scr["QT"][hd * 128:(hd + 1) * 128, t0:t0 + n], W=[q])
                    if bg and not isctx:
                        self.issue_chunk(bg.pop(0))
                    tiles = list(range(2)) if isctx else list(range(NT))
                    ngrp_done = 0

                    def scores(pi, kt):
                        s0, s1 = self.psum[2 * pi], self.psum[2 * pi + 1]
                        k.I("pe", lambda e: e.matmul(s0[:, 0:n], lhsT=kt_[0:64, kt * 128:(kt + 1) * 128],
                                                     rhs=q[0:64, 0:n], start=True, stop=True), R=[kt_, q], W=[s0])
                        k.I("pe", lambda e: e.matmul(s1[:, 0:n], lhsT=kt_[64:128, kt * 128:(kt + 1) * 128],
                                                     rhs=q[64:128, 0:n], start=True, stop=True), R=[kt_, q], W=[s1])

                    scores(ne % 2, tiles[0])
                    for ti, kt in enumerate(tiles):
                        pi = ne % 2
                        xp = ep[ne % 4]
                        ne += 1
                        first, last = (ti == 0), (ti == len(tiles) - 1)
                        if not last:
                            scores(ne % 2, tiles[ti + 1])
                        s0, s1 = self.psum[2 * pi], self.psum[2 * pi + 1]
                        k.I("act", lambda e, pi=pi, xp=xp: e.activation(out=xp[:, :, 0:n], in_=self.pspair[pi][:, :, 0:n],
                                                                        func=AF.Exp), R=[s0, s1], W=[xp])
                        for (acc, mi) in ((acc0, 0), (acc1, 1)):
                            k.I("pe", lambda e, acc=acc, mi=mi, xp=xp, kt=kt: e.matmul(
                                acc[:, 0:n], lhsT=v_[:, kt, :], rhs=xp[:, mi, 0:n], start=first, stop=last), R=[v_, xp], W=[acc])
                        k.I("pe", lambda e, xp=xp: e.matmul(Zp[:, 0:n], lhsT=onesb[:], rhs=xp[:, 0, 0:n], start=first, stop=last),
                            R=[onesb, xp], W=[Zp])
                        k.I("pe", lambda e, xp=xp: e.matmul(Zq[:, 0:n], lhsT=onesb[:], rhs=xp[:, 1, 0:n], start=first, stop=last),
                            R=[onesb, xp], W=[Zq])
                        continue
                        gi = ti % GRP
                        g0, g1 = gs0[ng % 2], gs1[ng % 2]
                        if gi == 0:
                            pp = xp
                        elif gi == 1:
                            k.I("pool", lambda e, g1=g1, pp=pp, xp=xp: e.tensor_tensor(out=g1[:, 0:n], in0=pp[:, 1, 0:n], in1=xp[:, 1, 0:n],
                                                                                       op=ALU.add), R=[pp, xp], W=[g1])
                        else:
                            k.I("pool", lambda e, g1=g1, xp=xp: e.tensor_tensor(out=g1[:, 0:n], in0=g1[:, 0:n], in1=xp[:, 1, 0:n],
                                                                                op=ALU.add), R=[g1, xp], W=[g1])
                        if gi == GRP - 1 or last:
                            assert gi >= 1
                            for (es, g) in ((es1, g1),):
                                if ngrp_done == 0:
                                    k.I("dve", lambda e, es=es, g=g: e.tensor_copy(out=es[:, 0:n], in_=g[:, 0:n]), R=[g], W=[es])
                                else:
                                    k.I("dve", lambda e, es=es, g=g: e.tensor_tensor(out=es[:, 0:n], in0=g[:, 0:n], in1=es[:, 0:n],
                                                                                     op=ALU.add), R=[g, es], W=[es])
                            ngrp_done += 1
                            ng += 1
                    Z0 = Zp
                    Z1 = Zq
                    k.I("dve", lambda e: e.reciprocal(out=r0[:, 0:n], in_=Z0[:, 0:n]), R=[Z0], W=[r0])
                    k.I("dve", lambda e: e.reciprocal(out=r1[:, 0:n], in_=Z1[:, 0:n]), R=[Z1], W=[r1])
                    k.I("dve", lambda e: e.tensor_tensor(out=o0[:, 0:n], in0=acc0[:, 0:n], in1=r0[:, 0:n], op=ALU.mult),
                        R=[acc0, r0], W=[o0])
                    k.I("dve", lambda e: e.tensor_tensor(out=o1[:, 0:n], in0=acc1[:, 0:n], in1=r1[:, 0:n], op=ALU.mult),
                        R=[acc1, r1], W=[o1])
                    k.I("dve", lambda e: e.scalar_tensor_tensor(out=o0[:, 0:n], in0=o1[:, 0:n], scalar=ls[:, 2:3], in1=o0[:, 0:n],
                                                                op0=ALU.mult, op1=ALU.add), R=[o1, ls, o0], W=[o0])
                    k.I("act", lambda e: e.activation(out=sq[:, 0:n], in_=o0[:, 0:n], func=AF.Square), R=[o0], W=[sq])
                    pss = self.psum[(ne % 2) * 2]
                    k.I("pe", lambda e, pss=pss: e.matmul(pss[:, 0:n], lhsT=self.ones[:], rhs=sq[:, 0:n], start=True, stop=True),
                        R=[self.ones, sq], W=[pss])
                    k.I("act", lambda e, pss=pss: e.activation(out=r0[:, 0:n], in_=pss[:, 0:n], func=AF.Sqrt, bias=self.epsb[:],
                                                               scale=1.0 / 128.0), R=[pss, self.epsb], W=[r0])
                    k.I("dve", lambda e: e.reciprocal(out=r0[:, 0:n], in_=r0[:, 0:n]), R=[r0], W=[r0])
                    k.I("dve", lambda e, o_=o_: e.scalar_tensor_tensor(out=o_[:, 0:n], in0=o0[:, 0:n], scalar=sgl[:, 0:1],
                                                                       in1=r0[:, 0:n], op0=ALU.mult, op1=ALU.mult),
                        R=[o0, sgl, r0], W=[o_])
                    k.store(self.scr["ST"][hd * 128:(hd + 1) * 128, t0:t0 + n], o_[:, 0:n], R=[o_])
            while bg:
                self.issue_chunk(bg.pop(0))
            k.barrier()

    def phase_lru1(self, li, j):
        k = self.k
        XT = self.scr["XT"].rearrange("(k p) t -> p k t", p=128)
        GT = self.scr["GATE"].rearrange("(k p) t -> p k t", p=128)
        RT = self.scr["R"].rearrange("(k p) t -> p k t", p=128)
        with contextlib.ExitStack() as st:
            win = k.sb(st, "win", [128, 8, 2 * D], BF16)
            self.wait_cvt("w_in", j)
            wv = self.scr["w_in"][j].rearrange("(k p) c -> p k c", p=128)
            for kc in range(KC):
                k.load(win[:, kc, :], wv[:, kc, :], W=[win], add=True)
            xt = [k.sb(st, "xt%d" % i, [128, 8, 512], F32) for i in range(2)]
            hb = [k.sb(st, "hb%d" % i, [128, 8, 512], BF16) for i in range(2)]
            gt = [k.sb(st, "gt%d" % i, [128, 8, 512], BF16) for i in range(2)]
            rt = [k.sb(st, "rt%d" % i, [128, 8, 512], F32) for i in range(2)]
            g1 = [k.sb(st, "g1_%d" % i, [128, 512], F32) for i in range(2)]
            g2 = [k.sb(st, "g2_%d" % i, [128, 512], F32) for i in range(2)]
            sq = k.sb(st, "sq", [128, 512], F32)
            rstd = k.sb(st, "rstd", [128, 512], F32)
            tmp = k.sb(st, "tmp", [128, 512], F32)
            for bi, (t0, n, isctx) in enumerate(self.blocks):
                x, h, g_, r_ = xt[bi % 2], hb[bi % 2], gt[bi % 2], rt[bi % 2]
                k.load(x[:, :, 0:n], XT[:, :, t0:t0 + n], W=[x])
                self.modulate((sq, rstd, tmp), x, n, li, self.A1, 0, isctx, out_bf=h, psb=self.psum[7])
                for c in range(16):
                    ps = self.psum[c % 4]
                    for kc in range(KC):
                        k.I("pe", lambda e, ps=ps, kc=kc, c=c: e.matmul(
                            ps[:, 0:n], lhsT=win[:, kc, c * 128:(c + 1) * 128], rhs=h[:, kc, 0:n],
                            start=(kc == 0), stop=(kc == KC - 1)), R=[win, h], W=[ps])
                    if c < 8:
                        a, b_ = g1[c % 2], g2[c % 2]
                        k.I("act", lambda e, ps=ps, a=a: e.activation(out=a[:, 0:n], in_=ps[:, 0:n], func=AF.Square), R=[ps], W=[a])
                        k.I("dve", lambda e, a=a: e.tensor_scalar(out=a[:, 0:n], in0=a[:, 0:n], scalar1=0.044715, scalar2=1.0,
                                                                  op0=ALU.mult, op1=ALU.add), R=[a], W=[a])
                        k.I("dve", lambda e, a=a, ps=ps: e.tensor_tensor(out=a[:, 0:n], in0=ps[:, 0:n], in1=a[:, 0:n], op=ALU.mult),
                            R=[ps, a], W=[a])
                        k.I("act", lambda e, a=a, b_=b_: e.activation(out=b_[:, 0:n], in_=a[:, 0:n], func=AF.Sigmoid,
                                                                      scale=1.5957691216057308), R=[a], W=[b_])
                        k.I("dve", lambda e, b_=b_, ps=ps, c=c: e.tensor_tensor(out=g_[:, c, 0:n], in0=ps[:, 0:n], in1=b_[:, 0:n],
                                                                                op=ALU.mult), R=[ps, b_], W=[g_])
                    else:
                        k.I("act", lambda e, ps=ps, c=c: e.copy(out=r_[:, c - 8, 0:n], in_=ps[:, 0:n]), R=[ps], W=[r_])
                k.store(GT[:, :, t0:t0 + n], g_[:, :, 0:n], R=[g_])
                k.store(RT[:, :, t0:t0 + n], r_[:, :, 0:n], R=[r_])
            k.barrier()

    def phase_lru2(self, li, j):
        k = self.k
        T = self.T
        segs = [(0, CTX), (CTX, T)]
        with contextlib.ExitStack() as st:
            Rb = k.sb(st, "Rb", [128, T], F32)
            U = k.sb(st, "U", [128, T], F32)
            A = k.sb(st, "A", [128, T], F32)
            B1 = k.sb(st, "B1", [128, T], F32)
            gw = k.sb(st, "gw", [128, 2, 2, 128], F32)
            gb = k.sb(st, "gb", [128, 4], F32)
            ap_ = k.sb(st, "ap", [128, 2], F32)
            sc8 = k.sb(st, "sc8", [128, 2], F32)
            cw = k.sb(st, "cw", [128, 4], F32)
            cb = k.sb(st, "cb", [128, 1], F32)
            rr = [k.sb(st, "rr%d" % i, [128, 512], F32) for i in range(2)]
            ii = [k.sb(st, "ii%d" % i, [128, 512], F32) for i in range(2)]
            s2 = [k.sb(st, "s2_%d" % i, [128, 512], F32) for i in range(2)]
            gl = [k.sb(st, "gl%d" % i, [128, 512], BF16) for i in range(2)]
            so = [k.sb(st, "so%d" % i, [128, 512], BF16) for i in range(2)]
            hs = [k.sb(st, "hs%d" % i, [128, 512], F32) for i in range(2)]
            nn = 0
            for kc in range(KC):
                sl = slice(kc * 128, (kc + 1) * 128)
                k.load(Rb[:], self.scr["R"][sl, :], W=[Rb])
                k.load(gw[:], self.inp["lru_gate_w"][j][:, :, kc].rearrange("d g c o -> c d g o"), W=[gw])
                k.load(gb[:], self.inp["lru_gate_b"][j][:, :, sl].rearrange("d g p -> p (d g)"), W=[gb],
                       allow_slow_non_contiguous=True)
                k.load(ap_[:], self.inp["lru_a_param"][j][:, sl].rearrange("d p -> p d"), W=[ap_], allow_slow_non_contiguous=True)
                k.load(cw[:], self.inp["lru_conv_w"][j][:, sl].rearrange("w p -> p w"), W=[cw], allow_slow_non_contiguous=True)
                k.load(cb[:], self.inp["lru_conv_b"][j][sl].rearrange("(p o) -> p o", o=1), W=[cb])
                k.I("act", lambda e: e.activation(out=sc8[:], in_=ap_[:], func=AF.Sigmoid), R=[ap_], W=[sc8])
                k.I("act", lambda e: e.activation(out=sc8[:], in_=sc8[:], func=AF.Ln), R=[sc8], W=[sc8])
                k.I("dve", lambda e: e.tensor_scalar_mul(out=sc8[:], in0=sc8[:], scalar1=8.0), R=[sc8], W=[sc8])
                k.I("dve", lambda e: e.tensor_scalar(out=U[:], in0=Rb[:], scalar1=cw[:, 2:3], scalar2=cb[:, 0:1],
                                                     op0=ALU.mult, op1=ALU.add), R=[Rb, cw, cb], W=[U])
                for (s_, e_) in segs:
                    for (tap, dlo, dhi, slo, shi) in ((0, s_ + 2, e_, s_, e_ - 2), (1, s_ + 1, e_, s_, e_ - 1),
                                                      (3, s_, e_ - 1, s_ + 1, e_)):
                        k.I("dve", lambda e, tap=tap, dlo=dlo, dhi=dhi, slo=slo, shi=shi: e.scalar_tensor_tensor(
                            out=U[:, dlo:dhi], in0=Rb[:, slo:shi], scalar=cw[:, tap:tap + 1], in1=U[:, dlo:dhi],
                            op0=ALU.mult, op1=ALU.add), R=[Rb, cw, U], W=[U])
                for d in range(2):
                    Bd = B1 if d == 0 else Rb
                    for (t0, n, isctx) in self.blocks:
                        pr, pi = self.psum[(2 * nn) % 4], self.psum[(2 * nn + 1) % 4]
                        r_, i_, q_ = rr[nn % 2], ii[nn % 2], s2[nn % 2]
                        nn += 1
                        k.I("pe", lambda e, pr=pr: e.matmul(pr[:, 0:n], lhsT=gw[:, d, 0, :], rhs=U[:, t0:t0 + n], start=True, stop=True),
                            R=[gw, U], W=[pr])
                        k.I("pe", lambda e, pi=pi: e.matmul(pi[:, 0:n], lhsT=gw[:, d, 1, :], rhs=U[:, t0:t0 + n], start=True, stop=True),
                            R=[gw, U], W=[pi])
                        k.I("act", lambda e, pr=pr, r_=r_: e.activation(out=r_[:, 0:n], in_=pr[:, 0:n], func=AF.Sigmoid,
                                                                        bias=gb[:, 2 * d:2 * d + 1]), R=[pr, gb], W=[r_])
                        k.I("act", lambda e, pi=pi, i_=i_: e.activation(out=i_[:, 0:n], in_=pi[:, 0:n], func=AF.Sigmoid,
                                                                        bias=gb[:, 2 * d + 1:2 * d + 2]), R=[pi, gb], W=[i_])
                        k.I("act", lambda e, r_=r_: e.activation(out=A[:, t0:t0 + n], in_=r_[:, 0:n], func=AF.Exp,
                                                                 scale=sc8[:, d:d + 1]), R=[r_, sc8], W=[A])
                        k.I("act", lambda e, q_=q_: e.activation(out=q_[:, 0:n], in_=A[:, t0:t0 + n], func=AF.Square), R=[A], W=[q_])
                        k.I("act", lambda e, q_=q_: e.activation(out=q_[:, 0:n], in_=q_[:, 0:n], func=AF.Sqrt, bias=self.ones[:, 0:1],
                                                                 scale=-1.0), R=[q_, self.ones], W=[q_])
                        k.I("dve", lambda e, i_=i_: e.tensor_tensor(out=i_[:, 0:n], in0=i_[:, 0:n], in1=U[:, t0:t0 + n], op=ALU.mult),
                            R=[i_, U], W=[i_])
                        k.I("dve", lambda e, i_=i_, q_=q_, Bd=Bd: e.tensor_tensor(out=Bd[:, t0:t0 + n], in0=i_[:, 0:n], in1=q_[:, 0:n],
                                                                                  op=ALU.mult), R=[i_, q_], W=[Bd])
                    if d == 0:
                        k.I("dve", lambda e, Bd=Bd: e.tensor_tensor_scan(out=Bd[:, 0:CTX], data0=A[:, 0:CTX], data1=Bd[:, 0:CTX],
                                                                         initial=0.0, op0=ALU.mult, op1=ALU.add), R=[A, Bd], W=[Bd])
                        k.I("dve", lambda e, Bd=Bd: e.tensor_tensor_scan(out=Bd[:, CTX:T], data0=A[:, CTX:T], data1=Bd[:, CTX:T],
                                                                         initial=Bd[:, CTX - 1:CTX], op0=ALU.mult, op1=ALU.add),
                            R=[A, Bd], W=[Bd])
                    else:
                        k.I("dve", lambda e, Bd=Bd: e.tensor_tensor_scan(
                            out=Bd[:, 0:CTX][:, ::-1], data0=A[:, 0:CTX][:, ::-1], data1=Bd[:, 0:CTX][:, ::-1],
                            initial=0.0, op0=ALU.mult, op1=ALU.add), R=[A, Bd], W=[Bd])
                        k.I("dve", lambda e, Bd=Bd: e.tensor_tensor_scan(
                            out=Bd[:, CTX:T][:, ::-1], data0=A[:, CTX:T][:, ::-1], data1=Bd[:, CTX:T][:, ::-1],
                            initial=Bd[:, 0:1], op0=ALU.mult, op1=ALU.add), R=[A, Bd], W=[Bd])
                for bi, (t0, n, isctx) in enumerate(self.blocks):
                    g_, s_o, h_ = gl[bi % 2], so[bi % 2], hs[bi % 2]
                    k.load(g_[:, 0:n], self.scr["GATE"][sl, t0:t0 + n], W=[g_])
                    k.I("dve", lambda e, h_=h_: e.tensor_tensor(out=h_[:, 0:n], in0=B1[:, t0:t0 + n], in1=Rb[:, t0:t0 + n], op=ALU.add),
                        R=[B1, Rb], W=[h_])
                    k.I("pool", lambda e, h_=h_, g_=g_, s_o=s_o: e.tensor_tensor(out=s_o[:, 0:n], in0=h_[:, 0:n], in1=g_[:, 0:n],
                                                                                op=ALU.mult), R=[h_, g_], W=[s_o])
                    k.store(self.scr["ST"][sl, t0:t0 + n], s_o[:, 0:n], R=[s_o])
            k.barrier()


def host_consts():
    ident = np.eye(128, dtype=np.float32)
    ones = np.ones((128, 128), dtype=np.float32)
    perm = np.zeros((128, 128), dtype=np.float32)
    ropec = np.zeros((128, 8), dtype=np.float32)
    for p in range(128):
        d = p % 64
        a = d // 32
        hf = (d % 32) // 16
        i = d % 16
        partner = p + 16 if hf == 0 else p - 16
        perm[partner, p] = 1.0
        invf = 1.0 / (10000.0 ** ((2.0 * i) / 32.0))
        sgn = -1.0 if hf == 0 else 1.0
        ropec[p, 0] = 1.0 if a == 0 else 0.0
        ropec[p, 1] = 1.0 if a == 1 else 0.0
        ropec[p, 2] = invf / (2.0 * math.pi)
        ropec[p, 3] = sgn * 2.0 * math.pi
        ropec[p, 4] = -sgn * math.pi
        ropec[p, 5] = 2.0 * math.pi
        ropec[p, 6] = -math.pi
    return {"ident": ident, "ones": ones, "perm": perm, "ropec": ropec}


def make_in_map(inputs, b, nlat=8192, shapes=None):
    m = {}

    def f(a):
        return np.ascontiguousarray(np.asarray(a, dtype=np.float32))
    m["x"] = f(inputs["x"][b][:nlat])
    m["c"] = f(inputs["c"][b])
    m["ctx"] = f(inputs["ctx"][b])
    for name in ("c_ctx", "mod_w", "mod_b", "norm_mix_g", "norm_ffn_g", "attn_w_qkv", "attn_w_o", "lru_w_in",
                 "lru_conv_w", "lru_conv_b", "lru_gate_w", "lru_gate_b", "lru_a_param", "lru_w_out",
                 "ffn_w_gate_up", "ffn_w_down", "moe_router_w", "moe_w_gate_up", "moe_w_down", "final_norm_g"):
        m[name] = f(inputs[name])
    m["attn_lambda"] = f(inputs["attn_lambda"]).reshape(2, 256)
    m["attn_subln_g"] = f(inputs["attn_subln_g"])
    m.update(host_consts())
    if shapes is not None:
        for kk in list(m):
            if shapes[kk] == [1, 1] and m[kk].shape != (1, 1):
                m[kk] = np.zeros((1, 1), np.float32)
    return m


def kernel(**inputs):
    prog = Prog()
    nc = prog.build()
    B = inputs["x"].shape[0]
    in_maps = [make_in_map(inputs, b) for b in range(B)]
    res = run_bass_kernel_spmd(nc, in_maps, core_ids=list(range(B)))
    return np.stack([np.asarray(res.results[b]["out"], dtype=np.float32) for b in range(B)], axis=0)
```

```python
import contextlib
import math
import numpy as np
import concourse.bass as bass
import concourse.mybir as mybir
from concourse.bass_utils import run_bass_kernel_spmd

F32 = mybir.dt.float32
BF16 = mybir.dt.bfloat16
I32 = mybir.dt.int32
AF = mybir.ActivationFunctionType
ALU = mybir.AluOpType
AX = mybir.AxisListType

D = 1024
KC = 8
CTX = 256
NH = 8
DFF = 2816
DFE = 3584
NE = 8
EPS = 1e-6
EPOCH = 30000
SAME_ENGINE_SYNC = True


class DmaSem:
    def __init__(self, k):
        self.k = k
        self.sem = k.new_sem()
        self.count = 0

    def bump(self):
        if self.count + 16 > EPOCH:
            self.sem = self.k.new_sem()
            self.count = 0
        self.count += 16
        return self.sem, self.count


class Buf:
    def __init__(self, k, name, t):
        self.k = k
        self.name = name
        self.t = t
        self.lw = None
        self.rd = {}
        self.ld = None
        self.st = None

    def __getitem__(self, key):
        return self.t[key]


class HalfView:
    def __init__(self, t, h):
        self.t = t
        self.h = h

    def __getitem__(self, key):
        return self.t[(key[0], self.h) + tuple(key[1:])]


class K:
    def __init__(self, nc):
        self.nc = nc
        self.es = contextlib.ExitStack()
        self.engs = {"pe": nc.tensor, "act": nc.scalar, "dve": nc.vector, "pool": nc.gpsimd, "sp": nc.sync}
        self.cnt = {e: 0 for e in self.engs}
        self.esems = {e: [] for e in self.engs}
        self.waited = {e: {} for e in self.engs}
        self.nsem = 0
        self.dsems = []
        self.free_dsems = []
        self.live_dsems = []
        self.uid = 0

    def new_sem(self):
        self.nsem += 1
        return self.es.enter_context(self.nc.semaphore("s%d" % self.nsem))

    def esem(self, eng, seq):
        i = (seq - 1) // EPOCH
        while len(self.esems[eng]) <= i:
            self.esems[eng].append(self.new_sem())
        return self.esems[eng][i], (seq - 1) % EPOCH + 1

    def new_dsem(self):
        if self.free_dsems:
            d = self.free_dsems.pop()
        else:
            d = DmaSem(self)
            self.dsems.append(d)
        self.live_dsems.append(d)
        return d

    def sb(self, stack, name, shape, dt):
        self.uid += 1
        t = stack.enter_context(self.nc.sbuf_tensor("%s_%d" % (name, self.uid), list(shape), dt))
        return Buf(self, name, t)

    def ps(self, stack, name, shape, dt):
        self.uid += 1
        t = stack.enter_context(self.nc.psum_tensor("%s_%d" % (name, self.uid), list(shape), dt))
        return Buf(self, name, t)

    def _wait(self, eng, dep):
        w = self.waited[eng]
        if dep[0] == "e":
            _, src, seq = dep
            if src == eng and (not SAME_ENGINE_SYNC or eng == "pe"):
                return
            if w.get(src, 0) >= seq:
                return
            w[src] = seq
            sem, val = self.esem(src, seq)
            self.engs[eng].wait_ge(sem, val)
        else:
            _, sem, val, sid = dep
            key = ("d", sid)
            if w.get(key, 0) >= val:
                return
            w[key] = val
            self.engs[eng].wait_ge(sem, val)

    def _deps(self, eng, R, W, skip_waw_dma=False):
        for b in R:
            if b.lw is not None:
                self._wait(eng, b.lw)
        for b in W:
            if b.lw is not None and not (skip_waw_dma and b.lw[0] == "d"):
                self._wait(eng, b.lw)
            for d in b.rd.values():
                self._wait(eng, d)

    def I(self, eng, fn, R=(), W=()):
        self._deps(eng, R, W)
        ins = fn(self.engs[eng])
        self.cnt[eng] += 1
        seq = self.cnt[eng]
        sem, _ = self.esem(eng, seq)
        ins.then_inc(sem, 1)
        tag = ("e", eng, seq)
        for b in R:
            b.rd[eng] = tag
        for b in W:
            b.lw = tag
            b.rd = {}
        return ins

    def dma(self, q, out, in_, R=(), W=(), add=False, **kw):
        self._deps(q, R, W, skip_waw_dma=add)
        if W:
            if W[0].ld is None:
                W[0].ld = self.new_dsem()
            ds = W[0].ld
        else:
            if R[0].st is None:
                R[0].st = self.new_dsem()
            ds = R[0].st
        ins = self.engs[q].dma_start(out=out, in_=in_, **kw)
        sem, val = ds.bump()
        ins.then_inc(sem, 16)
        tag = ("d", sem, val, id(sem))
        for b in W:
            b.lw = tag
            b.rd = {}
        for b in R:
            b.rd[("st", id(sem))] = tag
        return ins

    def load(self, out, in_, W, **kw):
        return self.dma("sp", out, in_, W=W, **kw)

    def store(self, out, in_, R, **kw):
        return self.dma("pool", out, in_, R=R, **kw)

    def barrier(self):
        pool = self.engs["pool"]
        for d in self.dsems:
            if d.count > 0:
                self._wait("pool", ("d", d.sem, d.count, id(d.sem)))
        for e in ("pe", "act", "dve"):
            if self.cnt[e] > 0:
                self._wait("pool", ("e", e, self.cnt[e]))
        ins = pool.nop()
        self.cnt["pool"] += 1
        seq = self.cnt["pool"]
        sem, _ = self.esem("pool", seq)
        ins.then_inc(sem, 1)
        for e in ("pe", "act", "dve", "sp"):
            self._wait(e, ("e", "pool", seq))
        self.free_dsems.extend(self.live_dsems)
        self.live_dsems = []


class Prog:
    def __init__(self, nlat=8192, layers=(0, 1, 2, 3), dbg=(), do_final=True, ncores=4, conv_all=True):
        self.nlat = nlat
        self.T = CTX + nlat
        self.layers = list(layers)
        self.dbg = set(dbg)
        self.do_final = do_final
        self.in_shapes = {}
        nc = bass.Bass("TRN2", target_bir_lowering=False)
        self.nc = nc
        self.k = K(nc)
        self.blocks = [(0, CTX, True)] + [(CTX + i * 512, 512, False) for i in range(nlat // 512)]
        sbs = []
        cur = []
        tot = 0
        for b in self.blocks:
            if tot + b[1] > 1792:
                sbs.append(cur)
                cur, tot = [], 0
            cur.append(b)
            tot += b[1]
        if cur:
            sbs.append(cur)
        self.superblocks = sbs
        self.decl()

    def din(self, name, shape, dt=F32):
        if not self.used(name):
            shape = [1, 1]
        self.in_shapes[name] = list(shape)
        return self.nc.dram_tensor(name, list(shape), dt, kind="ExternalInput").ap()

    def used(self, name):
        att = any(l % 2 == 0 for l in self.layers)
        lru = any(l % 2 == 1 for l in self.layers)
        if name.startswith("attn_") or name.startswith("ffn_") or name in ("perm", "ropec"):
            return att
        if name.startswith("lru_") or name.startswith("moe_"):
            return lru
        return True

    def dscr(self, name, shape, dt):
        kind = "ExternalOutput" if name in self.dbg else "Internal"
        return self.nc.dram_tensor(name, list(shape), dt, kind=kind).ap()

    def decl(self):
        T = self.T
        i = {}
        i["x"] = self.din("x", [self.nlat, D])
        i["c"] = self.din("c", [D])
        i["ctx"] = self.din("ctx", [CTX, D])
        i["c_ctx"] = self.din("c_ctx", [D])
        i["mod_w"] = self.din("mod_w", [4, D, 6 * D])
        i["mod_b"] = self.din("mod_b", [4, 6 * D])
        i["norm_mix_g"] = self.din("norm_mix_g", [4, D])
        i["norm_ffn_g"] = self.din("norm_ffn_g", [4, D])
        i["attn_w_qkv"] = self.din("attn_w_qkv", [2, D, 3 * D])
        i["attn_w_o"] = self.din("attn_w_o", [2, D, D])
        i["attn_lambda"] = self.din("attn_lambda", [2, 4 * 64])
        i["attn_subln_g"] = self.din("attn_subln_g", [2, 128])
        i["lru_w_in"] = self.din("lru_w_in", [2, D, 2 * D])
        i["lru_conv_w"] = self.din("lru_conv_w", [2, 4, D])
        i["lru_conv_b"] = self.din("lru_conv_b", [2, D])
        i["lru_gate_w"] = self.din("lru_gate_w", [2, 2, 2, 8, 128, 128])
        i["lru_gate_b"] = self.din("lru_gate_b", [2, 2, 2, D])
        i["lru_a_param"] = self.din("lru_a_param", [2, 2, D])
        i["lru_w_out"] = self.din("lru_w_out", [2, D, D])
        i["ffn_w_gate_up"] = self.din("ffn_w_gate_up", [2, D, 2 * DFF])
        i["ffn_w_down"] = self.din("ffn_w_down", [2, DFF, D])
        i["moe_router_w"] = self.din("moe_router_w", [2, D, NE])
        i["moe_w_gate_up"] = self.din("moe_w_gate_up", [2, NE, D, 2 * DFE])
        i["moe_w_down"] = self.din("moe_w_down", [2, NE, DFE, D])
        i["final_norm_g"] = self.din("final_norm_g", [D])
        i["ident"] = self.din("ident", [128, 128])
        i["ones"] = self.din("ones", [128, 128])
        i["ropec"] = self.din("ropec", [128, 8])
        i["perm"] = self.din("perm", [128, 128])
        self.inp = i
        self.out = self.nc.dram_tensor("out", [self.nlat, D], F32, kind="ExternalOutput").ap()
        s = {}
        s["XT"] = self.dscr("XT", [D, T], F32)
        s["FT"] = self.dscr("FT", [D, T], BF16)
        s["ST"] = self.dscr("ST", [D, T], BF16)
        s["G"] = self.dscr("G", [NE, T], F32)
        s["QT"] = self.dscr("QT", [D, T], BF16)
        s["KT"] = self.dscr("KT", [D, T], BF16)
        s["V"] = self.dscr("V", [T, D], BF16)
        s["GATE"] = self.dscr("GATE", [D, T], BF16)
        s["R"] = self.dscr("R", [D, T], F32)
        s["w_qkv"] = self.dscr("w_qkv", [2, D, 3 * D], BF16)
        s["w_o"] = self.dscr("w_o", [2, D, D], BF16)
        s["w_in"] = self.dscr("w_in", [2, D, 2 * D], BF16)
        s["w_out"] = self.dscr("w_out", [2, D, D], BF16)
        s["w_fgu"] = self.dscr("w_fgu", [2, D, 2 * DFF], BF16)
        s["w_fd"] = self.dscr("w_fd", [2, DFF, D], BF16)
        s["w_mgu"] = self.dscr("w_mgu", [2, NE, D, 2 * DFE], BF16)
        s["w_md"] = self.dscr("w_md", [2, NE, DFE, D], BF16)
        self.scr = s

    def build(self):
        nc, k = self.nc, self.k
        with nc.allow_low_precision("bf16 matmul operands, fp32 accumulation"):
            with contextlib.ExitStack() as g:
                self.g = g
                self.consts(g)
                self.phase_convert()
                self.phase_mod()
                self.phase_x0()
                for li in self.layers:
                    j = li // 2
                    if li % 2 == 0:
                        self.phase_att1(li, j)
                        self.phase_att2(li, j)
                        self.phase_post(li, j, self.scr["w_o"][j])
                        self.phase_ffn(li, [(self.scr["w_fgu"][j], self.scr["w_fd"][j])], DFF, 2, gated=False)
                    else:
                        self.phase_lru1(li, j)
                        self.phase_lru2(li, j)
                        self.phase_post(li, j, self.scr["w_out"][j], router=True)
                        self.phase_ffn(li, [(self.scr["w_mgu"][j][e], self.scr["w_md"][j][e]) for e in range(NE)],
                                       DFE, 4, gated=True)
                if self.do_final:
                    self.phase_final()
                k.barrier()
            k.es.close()
        return nc

    def consts(self, g):
        k = self.k
        self.pspair = [k.ps(g, "pp%d" % i, [128, 2, 512], F32) for i in range(4)]
        self.psum = []
        for i in range(8):
            b = Buf(k, "ps%d" % i, HalfView(self.pspair[i // 2].t, i % 2))
            self.psum.append(b)
        self.ident = k.sb(g, "ident", [128, 128], F32)
        self.ones = k.sb(g, "ones", [128, 128], F32)
        self.modT = k.sb(g, "modT", [128, 4, 48, 2], F32)
        self.A1 = k.sb(g, "A1", [128, 4, 8, 2], F32)
        self.A2 = k.sb(g, "A2", [128, 4, 8, 2], F32)
        self.epsb = k.sb(g, "epsb", [128, 1], F32)
        k.load(self.ident[:], self.inp["ident"][:, :], W=[self.ident])
        k.load(self.ones[:], self.inp["ones"][:, :], W=[self.ones])
        k.I("dve", lambda e: e.memset(self.epsb[:], EPS), W=[self.epsb])

    def cvt_list(self, li):
        j = li // 2
        if li % 2 == 0:
            return [("attn_w_qkv", "w_qkv", j), ("attn_w_o", "w_o", j), ("ffn_w_gate_up", "w_fgu", j),
                    ("ffn_w_down", "w_fd", j)]
        return [("lru_w_in", "w_in", j), ("lru_w_out", "w_out", j), ("moe_w_gate_up", "w_mgu", j),
                ("moe_w_down", "w_md", j)]

    def cvt_chunks(self, pairs):
        k = self.k
        out = []
        for src, dst, j in pairs:
            a = self.inp[src][j]
            b = self.scr[dst][j]
            n = 1
            for s_ in a.shape:
                n *= s_
            a2 = a.flatten().rearrange("(r c) -> r c", c=2048)
            b2 = b.flatten().rearrange("(r c) -> r c", c=2048)
            rows = n // 2048
            step = 4096
            ds = DmaSem(k)
            rs = list(range(0, rows, step))
            for r0 in rs:
                r1 = min(rows, r0 + step)
                out.append((dst, j, a2[r0:r1, :], b2[r0:r1, :], ds, r0 == rs[-1]))
        return out

    def issue_chunk(self, ch):
        dst, j, a, b, ds, is_last = ch
        ins = self.k.engs["pool"].dma_start(out=b, in_=a)
        sem, val = ds.bump()
        ins.then_inc(sem, 16)
        if is_last:
            self.cvt_tag[(dst, j)] = ("d", ds.sem, ds.count, id(ds.sem))

    def issue_cvt(self, pairs):
        for ch in self.cvt_chunks(pairs):
            self.issue_chunk(ch)

    def wait_cvt(self, dst, j):
        tag = self.cvt_tag.get((dst, j))
        if tag is not None:
            self.k._wait("sp", tag)

    def phase_convert(self):
        k = self.k
        self.cvt_tag = {}
        self.pending_cvt = []
        first = True
        for li in self.layers:
            if first or li % 2 == 1 and not any(l % 2 == 0 for l in self.layers):
                self.issue_cvt(self.cvt_list(li))
            else:
                self.pending_cvt += self.cvt_list(li)
            first = False
        if not any(l % 2 == 0 for l in self.layers) and self.pending_cvt:
            self.issue_cvt(self.pending_cvt)
            self.pending_cvt = []

    def phase_mod(self):
        k, nc = self.k, self.nc
        with contextlib.ExitStack() as st:
            cin = k.sb(st, "cin", [128, 8, 2], F32)
            sc = k.sb(st, "sc", [128, 8, 2], F32)
            wt = [k.sb(st, "modw%d" % i, [128, 6 * D], F32) for i in range(2)]
            mb = k.sb(st, "mb", [128, 48], F32)
            gm = k.sb(st, "gm", [128, 8], F32)
            k.load(cin[:, :, 0], self.inp["c"].rearrange("(k p) -> p k", p=128), W=[cin],
                   allow_slow_non_contiguous=True)
            k.load(cin[:, :, 1], self.inp["c_ctx"].rearrange("(k p) -> p k", p=128), W=[cin], add=True,
                   allow_slow_non_contiguous=True)
            k.I("act", lambda e: e.activation(out=sc[:], in_=cin[:], func=AF.Silu), R=[cin], W=[sc])
            n = 0
            macc = k.sb(st, "macc", [128, 96], F32)
            for li in self.layers:
                k.load(mb[:], self.inp["mod_b"][li].rearrange("(j p) -> p j", p=128), W=[mb],
                       allow_slow_non_contiguous=True)
                for kc in range(KC):
                    ps = self.psum[kc % 2]
                    w = wt[n % 2]
                    n += 1
                    k.load(w[:], self.inp["mod_w"][li][kc * 128:(kc + 1) * 128, :], W=[w])
                    for jc in range(48):
                        k.I("pe", lambda e, w=w, jc=jc, kc=kc, ps=ps: e.matmul(
                            ps[:, jc * 2:jc * 2 + 2], lhsT=w[:, jc * 128:(jc + 1) * 128], rhs=sc[:, kc, :],
                            start=True, stop=True), R=[w, sc], W=[ps])
                    if kc == 0:
                        k.I("dve", lambda e, ps=ps: e.tensor_copy(out=macc[:], in_=ps[:, 0:96]), R=[ps], W=[macc])
                    else:
                        k.I("dve", lambda e, ps=ps: e.tensor_tensor(out=macc[:], in0=ps[:, 0:96], in1=macc[:], op=ALU.add),
                            R=[ps, macc], W=[macc])
                for t in range(2):
                    k.I("dve", lambda e, t=t, li=li: e.tensor_tensor(
                        out=self.modT[:, li, :, t], in0=macc[:].rearrange("p (j t) -> p j t", t=2)[:, :, t],
                        in1=mb[:], op=ALU.add), R=[macc, mb], W=[self.modT])
                for (A, gname, off) in ((self.A1, "norm_mix_g", 8), (self.A2, "norm_ffn_g", 32)):
                    k.load(gm[:], self.inp[gname][li].rearrange("(k p) -> p k", p=128), W=[gm],
                           allow_slow_non_contiguous=True)
                    for t in range(2):
                        k.I("dve", lambda e, A=A, off=off, t=t, li=li: e.scalar_tensor_tensor(
                            out=A[:, li, :, t], in0=self.modT[:, li, off:off + 8, t], scalar=1.0, in1=gm[:],
                            op0=ALU.add, op1=ALU.mult), R=[self.modT, gm], W=[A])
            k.barrier()

    def mv(self, li, which, kc, isctx):
        return self.modT[:, li, which * 8 + kc, (1 if isctx else 0):(2 if isctx else 1)]

    def phase_x0(self):
        k = self.k
        XT = self.scr["XT"].rearrange("(k p) t -> p k t", p=128)
        with contextlib.ExitStack() as st:
            xin = [k.sb(st, "xin%d" % i, [128, 4, D], F32) for i in range(2)]
            xo = [k.sb(st, "xo%d" % i, [128, 8, 512], F32) for i in range(2)]
            for bi, (t0, n, isctx) in enumerate(self.blocks):
                xi, o = xin[bi % 2], xo[bi % 2]
                nt = n // 128
                src = self.inp["ctx"] if isctx else self.inp["x"][t0 - CTX:t0 - CTX + n, :]
                k.load(xi[:, 0:nt, :], src.rearrange("(j p) d -> p j d", p=128), W=[xi])
                for kc in range(KC):
                    ps = self.psum[kc]
                    for j in range(nt):
                        k.I("pe", lambda e, ps=ps, j=j, kc=kc, xi=xi: e.transpose(
                            ps[:, j * 128:(j + 1) * 128], xi[:, j, kc * 128:(kc + 1) * 128], self.ident[:]),
                            R=[xi, self.ident], W=[ps])
                    eng = "act" if kc % 2 == 0 else "dve"
                    if eng == "act":
                        k.I("act", lambda e, ps=ps, kc=kc, o=o: e.copy(out=o[:, kc, 0:n], in_=ps[:, 0:n]), R=[ps], W=[o])
                    else:
                        k.I("dve", lambda e, ps=ps, kc=kc, o=o: e.tensor_copy(out=o[:, kc, 0:n], in_=ps[:, 0:n]), R=[ps], W=[o])
                k.store(XT[:, :, t0:t0 + n], o[:, :, 0:n], R=[o])
            k.barrier()

    def modulate(self, st_tiles, x, n, li, A, shw, isctx, out_bf=None, out_f32=None, psb=None):
        k = self.k
        sq, rstd, tmp = st_tiles
        ps = psb
        for kc in range(KC):
            k.I("act", lambda e, kc=kc: e.activation(out=sq[:, 0:n], in_=x[:, kc, 0:n], func=AF.Square), R=[x], W=[sq])
            k.I("pe", lambda e, kc=kc: e.matmul(ps[:, 0:n], lhsT=self.ones[:], rhs=sq[:, 0:n],
                                                  start=(kc == 0), stop=(kc == KC - 1)), R=[sq, self.ones], W=[ps])
        k.I("act", lambda e: e.activation(out=rstd[:, 0:n], in_=ps[:, 0:n], func=AF.Sqrt, bias=self.epsb[:],
                                          scale=1.0 / D), R=[ps, self.epsb], W=[rstd])
        k.I("dve", lambda e: e.reciprocal(out=rstd[:, 0:n], in_=rstd[:, 0:n]), R=[rstd], W=[rstd])
        tcol = 1 if isctx else 0
        for kc in range(KC):
            k.I("dve", lambda e, kc=kc: e.tensor_tensor(out=tmp[:, 0:n], in0=x[:, kc, 0:n], in1=rstd[:, 0:n],
                                                        op=ALU.mult), R=[x, rstd], W=[tmp])
            Ac = A[:, li, kc, tcol:tcol + 1]
            sh = self.mv(li, shw, kc, isctx)
            if out_f32 is not None:
                k.I("act", lambda e, kc=kc, Ac=Ac, sh=sh: e.activation(out=out_f32[:, kc, 0:n], in_=tmp[:, 0:n],
                    func=AF.Identity, bias=sh, scale=Ac), R=[tmp, A, self.modT], W=[out_f32])
                k.I("pool", lambda e, kc=kc: e.tensor_copy(out=out_bf[:, kc, 0:n], in_=out_f32[:, kc, 0:n]),
                    R=[out_f32], W=[out_bf])
            else:
                k.I("act", lambda e, kc=kc, Ac=Ac, sh=sh: e.activation(out=out_bf[:, kc, 0:n], in_=tmp[:, 0:n],
                    func=AF.Identity, bias=sh, scale=Ac), R=[tmp, A, self.modT], W=[out_bf])

    def phase_post(self, li, j, w_o, router=False):
        k = self.k
        XT = self.scr["XT"].rearrange("(k p) t -> p k t", p=128)
        ST = self.scr["ST"].rearrange("(k p) t -> p k t", p=128)
        FT = self.scr["FT"].rearrange("(k p) t -> p k t", p=128)
        with contextlib.ExitStack() as st:
            wo = k.sb(st, "wo", [128, 8, D], BF16)
            self.wait_cvt("w_out" if router else "w_o", j)
            k.load(wo[:], w_o.rearrange("(k p) c -> p k c", p=128), W=[wo])
            s_in = [k.sb(st, "s_in%d" % i, [128, 8, 512], BF16) for i in range(2)]
            xt = [k.sb(st, "xt%d" % i, [128, 8, 512], F32) for i in range(2)]
            fb = [k.sb(st, "fb%d" % i, [128, 8, 512], BF16) for i in range(2)]
            sq = k.sb(st, "sq", [128, 512], F32)
            rstd = k.sb(st, "rstd", [128, 512], F32)
            tmp = k.sb(st, "tmp", [128, 512], F32)
            if router:
                f32 = [k.sb(st, "f32_%d" % i, [128, 8, 512], F32) for i in range(2)]
                wr = k.sb(st, "wr", [128, 8, NE], F32)
                k.load(wr[:], self.inp["moe_router_w"][j].rearrange("(k p) e -> p k e", p=128), W=[wr])
                lg = k.sb(st, "lg", [128, 8], F32)
                m8 = k.sb(st, "m8", [128, 8], F32)
                sm = k.sb(st, "sm", [128, 8], F32)
                eq1 = k.sb(st, "eq1", [128, 8], F32)
                eq2 = k.sb(st, "eq2", [128, 8], F32)
                gt = k.sb(st, "gt", [128, 8], F32)
                gT = [k.sb(st, "gT%d" % i, [8, 512], F32) for i in range(2)]
            for bi, (t0, n, isctx) in enumerate(self.blocks):
                si, x, f = s_in[bi % 2], xt[bi % 2], fb[bi % 2]
                k.load(si[:, :, 0:n], ST[:, :, t0:t0 + n], W=[si])
                k.load(x[:, :, 0:n], XT[:, :, t0:t0 + n], W=[x])
                for dc in range(KC):
                    ps = self.psum[dc % 4]
                    for h in range(KC):
                        k.I("pe", lambda e, ps=ps, h=h, dc=dc: e.matmul(
                            ps[:, 0:n], lhsT=wo[:, h, dc * 128:(dc + 1) * 128], rhs=si[:, h, 0:n],
                            start=(h == 0), stop=(h == KC - 1)), R=[wo, si], W=[ps])
                    g1 = self.mv(li, 2, dc, isctx)
                    k.I("dve", lambda e, ps=ps, dc=dc, g1=g1: e.scalar_tensor_tensor(
                        out=x[:, dc, 0:n], in0=ps[:, 0:n], scalar=g1, in1=x[:, dc, 0:n], op0=ALU.mult, op1=ALU.add),
                        R=[ps, x, self.modT], W=[x])
                k.store(XT[:, :, t0:t0 + n], x[:, :, 0:n], R=[x])
                if router:
                    ff = f32[bi % 2]
                    self.modulate((sq, rstd, tmp), x, n, li, self.A2, 3, isctx, out_bf=f, out_f32=ff, psb=self.psum[4])
                else:
                    self.modulate((sq, rstd, tmp), x, n, li, self.A2, 3, isctx, out_bf=f, psb=self.psum[4])
                k.store(FT[:, :, t0:t0 + n], f[:, :, 0:n], R=[f])
                if router:
                    gTb = gT[bi % 2]
                    for jt in range(n // 128):
                        psl = self.psum[5]
                        for kc in range(KC):
                            k.I("pe", lambda e, kc=kc, jt=jt: e.matmul(
                                psl[:, 0:NE], lhsT=ff[:, kc, jt * 128:(jt + 1) * 128], rhs=wr[:, kc, :],
                                start=(kc == 0), stop=(kc == KC - 1)), R=[ff, wr], W=[psl])
                        k.I("dve", lambda e: e.tensor_copy(out=lg[:], in_=psl[:, 0:NE]), R=[psl], W=[lg])
                        k.I("dve", lambda e: e.max(out=m8[:], in_=lg[:]), R=[lg], W=[m8])
                        k.I("dve", lambda e: e.tensor_tensor(out=sm[:, 0:1], in0=m8[:, 1:2], in1=m8[:, 0:1], op=ALU.subtract),
                            R=[m8], W=[sm])
                        k.I("act", lambda e: e.activation(out=sm[:, 1:2], in_=sm[:, 0:1], func=AF.Exp), R=[sm], W=[sm])
                        k.I("dve", lambda e: e.tensor_scalar_add(out=sm[:, 2:3], in0=sm[:, 1:2], scalar1=1.0), R=[sm], W=[sm])
                        k.I("dve", lambda e: e.reciprocal(out=sm[:, 3:4], in_=sm[:, 2:3]), R=[sm], W=[sm])
                        k.I("dve", lambda e: e.tensor_tensor(out=sm[:, 4:5], in0=sm[:, 1:2], in1=sm[:, 3:4], op=ALU.mult),
                            R=[sm], W=[sm])
                        k.I("dve", lambda e: e.tensor_scalar(out=eq1[:], in0=lg[:], scalar1=m8[:, 0:1], scalar2=sm[:, 3:4],
                                                             op0=ALU.is_equal, op1=ALU.mult), R=[lg, m8, sm], W=[eq1])
                        k.I("dve", lambda e: e.tensor_scalar(out=eq2[:], in0=lg[:], scalar1=m8[:, 1:2], scalar2=sm[:, 4:5],
                                                             op0=ALU.is_equal, op1=ALU.mult), R=[lg, m8, sm], W=[eq2])
                        k.I("dve", lambda e: e.tensor_tensor(out=gt[:], in0=eq1[:], in1=eq2[:], op=ALU.add),
                            R=[eq1, eq2], W=[gt])
                        pst = self.psum[6]
                        k.I("pe", lambda e: e.transpose(pst[0:8, 0:128], gt[:], self.ident[:]), R=[gt, self.ident], W=[pst])
                        k.I("act", lambda e, jt=jt: e.copy(out=gTb[:, jt * 128:(jt + 1) * 128], in_=pst[0:8, 0:128]),
                            R=[pst], W=[gTb])
                    k.store(self.scr["G"][:, t0:t0 + n], gTb[:, 0:n], R=[gTb])
            k.barrier()

    def phase_ffn(self, li, experts, F, GC, gated):
        k = self.k
        XT = self.scr["XT"].rearrange("(k p) t -> p k t", p=128)
        FT = self.scr["FT"].rearrange("(k p) t -> p k t", p=128)
        ngroups = F // (GC * 128)
        assert ngroups * GC * 128 == F
        NSB = max(sum(b[1] for b in sb) for sb in self.superblocks)
        with contextlib.ExitStack() as st:
            jj = li // 2
            for nm in (("w_mgu", "w_md") if gated else ("w_fgu", "w_fd")):
                self.wait_cvt(nm, jj)
            fT = k.sb(st, "fT", [128, 8, NSB], BF16)
            yacc = k.sb(st, "yacc", [128, 8, NSB], F32)
            wg = [k.sb(st, "wg%d" % i, [128, 8, GC * 128], BF16) for i in range(2)]
            wu = [k.sb(st, "wu%d" % i, [128, 8, GC * 128], BF16) for i in range(2)]
            wd = [k.sb(st, "wd%d" % i, [128, GC, D], BF16) for i in range(2)]
            act = [k.sb(st, "act%d" % i, [128, GC, 512], BF16) for i in range(2)]
            sg = [k.sb(st, "sg%d" % i, [128, 512], F32) for i in range(2)]
            tg = [k.sb(st, "tg%d" % i, [128, 512], F32) for i in range(2)]
            gB = [k.sb(st, "gB%d" % i, [128, NSB], F32) for i in range(2)] if gated else None
            xt = [k.sb(st, "xt%d" % i, [128, 8, 512], F32) for i in range(2)]
            nw = 0
            na = 0
            for sb in self.superblocks:
                s0 = sb[0][0]
                ns = sum(b[1] for b in sb)
                k.load(fT[:, :, 0:ns], FT[:, :, s0:s0 + ns], W=[fT])
                first = True
                pend = None

                def emit_down(ac, c_, o, n, is_first):
                    for dc in range(KC):
                        py = self.psum[4 + dc % 4]
                        for c in range(GC):
                            k.I("pe", lambda e, py=py, c=c, dc=dc: e.matmul(
                                py[:, 0:n], lhsT=c_[:, c, dc * 128:(dc + 1) * 128], rhs=ac[:, c, 0:n],
                                start=(c == 0), stop=(c == GC - 1)), R=[c_, ac], W=[py])
                        if is_first:
                            k.I("dve", lambda e, py=py, dc=dc: e.tensor_copy(out=yacc[:, dc, o:o + n], in_=py[:, 0:n]),
                                R=[py], W=[yacc])
                        else:
                            k.I("dve", lambda e, py=py, dc=dc: e.tensor_tensor(
                                out=yacc[:, dc, o:o + n], in0=py[:, 0:n], in1=yacc[:, dc, o:o + n], op=ALU.add),
                                R=[py, yacc], W=[yacc])

                for ei, (wgu_d, wd_d) in enumerate(experts):
                    wgu_v = wgu_d.rearrange("(k p) c -> p k c", p=128)
                    wd_v = wd_d.rearrange("(c p) d -> p c d", p=128)
                    if gated:
                        gb = gB[ei % 2]
                        k.load(gb[:, 0:ns], self.scr["G"][ei:ei + 1, s0:s0 + ns].partition_broadcast(128), W=[gb])
                    for gi in range(ngroups):
                        a, b_, c_ = wg[nw % 2], wu[nw % 2], wd[nw % 2]
                        nw += 1
                        c0 = gi * GC * 128
                        k.load(a[:], wgu_v[:, :, c0:c0 + GC * 128], W=[a])
                        k.load(b_[:], wgu_v[:, :, F + c0:F + c0 + GC * 128], W=[b_])
                        k.load(c_[:], wd_v[:, gi * GC:(gi + 1) * GC, :], W=[c_])
                        for (t0, n, isctx) in sb:
                            o = t0 - s0
                            ac = act[na % 2]
                            na += 1
                            for c in range(GC):
                                pg = self.psum[(2 * c) % 4]
                                pu = self.psum[(2 * c + 1) % 4]
                                for kc in range(KC):
                                    k.I("pe", lambda e, pg=pg, c=c, kc=kc, a=a: e.matmul(
                                        pg[:, 0:n], lhsT=a[:, kc, c * 128:(c + 1) * 128], rhs=fT[:, kc, o:o + n],
                                        start=(kc == 0), stop=(kc == KC - 1)), R=[a, fT], W=[pg])
                                for kc in range(KC):
                                    k.I("pe", lambda e, pu=pu, c=c, kc=kc, b_=b_: e.matmul(
                                        pu[:, 0:n], lhsT=b_[:, kc, c * 128:(c + 1) * 128], rhs=fT[:, kc, o:o + n],
                                        start=(kc == 0), stop=(kc == KC - 1)), R=[b_, fT], W=[pu])
                                s_ = sg[c % 2]
                                k.I("act", lambda e, s_=s_, pg=pg: e.activation(out=s_[:, 0:n], in_=pg[:, 0:n], func=AF.Silu),
                                    R=[pg], W=[s_])
                                if gated:
                                    t_ = tg[c % 2]
                                    k.I("dve", lambda e, t_=t_, pu=pu, gb=gb: e.tensor_tensor(
                                        out=t_[:, 0:n], in0=pu[:, 0:n], in1=gb[:, o:o + n], op=ALU.mult), R=[pu, gb], W=[t_])
                                    k.I("pool", lambda e, t_=t_, s_=s_, c=c, ac=ac: e.tensor_tensor(
                                        out=ac[:, c, 0:n], in0=s_[:, 0:n], in1=t_[:, 0:n], op=ALU.mult), R=[s_, t_], W=[ac])
                                else:
                                    k.I("dve", lambda e, pu=pu, s_=s_, c=c, ac=ac: e.tensor_tensor(
                                        out=ac[:, c, 0:n], in0=pu[:, 0:n], in1=s_[:, 0:n], op=ALU.mult), R=[pu, s_], W=[ac])
                            if pend is not None:
                                emit_down(*pend)
                            pend = (ac, c_, o, n, first and gi == 0)
                    first = False
                if pend is not None:
                    emit_down(*pend)
                    pend = None
                for bi, (t0, n, isctx) in enumerate(sb):
                    o = t0 - s0
                    x = xt[bi % 2]
                    k.load(x[:, :, 0:n], XT[:, :, t0:t0 + n], W=[x])
                    for dc in range(KC):
                        g2 = self.mv(li, 5, dc, isctx)
                        k.I("dve", lambda e, dc=dc, g2=g2, x=x: e.scalar_tensor_tensor(
                            out=x[:, dc, 0:n], in0=yacc[:, dc, o:o + n], scalar=g2, in1=x[:, dc, 0:n],
                            op0=ALU.mult, op1=ALU.add), R=[yacc, x, self.modT], W=[x])
                    k.store(XT[:, :, t0:t0 + n], x[:, :, 0:n], R=[x])
            k.barrier()

    def phase_final(self):
        k = self.k
        XT = self.scr["XT"].rearrange("(k p) t -> p k t", p=128)
        with contextlib.ExitStack() as st:
            gf = k.sb(st, "gf", [128, 8], F32)
            k.load(gf[:], self.inp["final_norm_g"].rearrange("(k p) -> p k", p=128), W=[gf], allow_slow_non_contiguous=True)
            xt = [k.sb(st, "xt%d" % i, [128, 8, 512], F32) for i in range(2)]
            yo = [k.sb(st, "yo%d" % i, [128, 4, D], F32) for i in range(2)]
            sq = k.sb(st, "sq", [128, 512], F32)
            rstd = k.sb(st, "rstd", [128, 512], F32)
            for bi, (t0, n, isctx) in enumerate(self.blocks):
                if isctx:
                    continue
                x, y = xt[bi % 2], yo[bi % 2]
                k.load(x[:, :, 0:n], XT[:, :, t0:t0 + n], W=[x])
                ps = self.psum[0]
                for kc in range(KC):
                    k.I("act", lambda e, kc=kc: e.activation(out=sq[:, 0:n], in_=x[:, kc, 0:n], func=AF.Square), R=[x], W=[sq])
                    k.I("pe", lambda e, kc=kc: e.matmul(ps[:, 0:n], lhsT=self.ones[:], rhs=sq[:, 0:n],
                                                          start=(kc == 0), stop=(kc == KC - 1)), R=[sq, self.ones], W=[ps])
                k.I("act", lambda e: e.activation(out=rstd[:, 0:n], in_=ps[:, 0:n], func=AF.Sqrt, bias=self.epsb[:],
                                                  scale=1.0 / D), R=[ps, self.epsb], W=[rstd])
                k.I("dve", lambda e: e.reciprocal(out=rstd[:, 0:n], in_=rstd[:, 0:n]), R=[rstd], W=[rstd])
                for kc in range(KC):
                    k.I("dve", lambda e, kc=kc: e.scalar_tensor_tensor(
                        out=x[:, kc, 0:n], in0=x[:, kc, 0:n], scalar=gf[:, kc:kc + 1], in1=rstd[:, 0:n],
                        op0=ALU.mult, op1=ALU.mult), R=[x, gf, rstd], W=[x])
                for jt in range(n // 128):
                    for half in range(2):
                        pt = self.psum[1 + (jt * 2 + half) % 4]
                        for q in range(4):
                            kc = half * 4 + q
                            k.I("pe", lambda e, pt=pt, q=q, kc=kc, jt=jt: e.transpose(
                                pt[:, q * 128:(q + 1) * 128], x[:, kc, jt * 128:(jt + 1) * 128], self.ident[:]),
                                R=[x, self.ident], W=[pt])
                        if half == 0:
                            k.I("act", lambda e, pt=pt, jt=jt: e.copy(out=y[:, jt, 0:512], in_=pt[:, 0:512]), R=[pt], W=[y])
                        else:
                            k.I("dve", lambda e, pt=pt, jt=jt: e.tensor_copy(out=y[:, jt, 512:1024], in_=pt[:, 0:512]), R=[pt], W=[y])
                k.store(self.out[t0 - CTX:t0 - CTX + n, :].rearrange("(j p) d -> p j d", p=128), y[:, 0:n // 128, :], R=[y])
            k.barrier()

    def rope_tables(self, st, t0, n, T_):
        k = self.k
        rowf, colf, pos, u, ki, kf, cosT, sinT, rc = T_
        base_r = float((t0 - CTX) // 64)
        k.I("dve", lambda e: e.tensor_scalar(out=pos[:, 0:n], in0=rowf[:, 0:n], scalar1=base_r, scalar2=rc[:, 0:1],
                                             op0=ALU.add, op1=ALU.mult), R=[rowf, rc], W=[pos])
        k.I("dve", lambda e: e.scalar_tensor_tensor(out=pos[:, 0:n], in0=colf[:, 0:n], scalar=rc[:, 1:2], in1=pos[:, 0:n],
                                                    op0=ALU.mult, op1=ALU.add), R=[colf, rc, pos], W=[pos])
        for (off, outT, sc_col, bi_col) in ((0.5, sinT, 3, 4), (0.75, cosT, 5, 6)):
            k.I("dve", lambda e, off=off: e.tensor_scalar(out=u[:, 0:n], in0=pos[:, 0:n], scalar1=rc[:, 2:3], scalar2=off,
                                                          op0=ALU.mult, op1=ALU.add), R=[pos, rc], W=[u])
            k.I("dve", lambda e: e.tensor_copy(out=ki[:, 0:n], in_=u[:, 0:n]), R=[u], W=[ki])
            k.I("dve", lambda e: e.tensor_copy(out=kf[:, 0:n], in_=ki[:, 0:n]), R=[ki], W=[kf])
            k.I("dve", lambda e: e.tensor_tensor(out=u[:, 0:n], in0=u[:, 0:n], in1=kf[:, 0:n], op=ALU.subtract),
                R=[u, kf], W=[u])
            k.I("dve", lambda e: e.tensor_single_scalar(out=kf[:, 0:n], in_=u[:, 0:n], scalar=0.0, op=ALU.is_lt),
                R=[u], W=[kf])
            k.I("dve", lambda e: e.tensor_tensor(out=u[:, 0:n], in0=u[:, 0:n], in1=kf[:, 0:n], op=ALU.add),
                R=[u, kf], W=[u])
            k.I("act", lambda e, outT=outT, sc_col=sc_col, bi_col=bi_col: e.activation(
                out=outT[:, 0:n], in_=u[:, 0:n], func=AF.Sin, bias=rc[:, bi_col:bi_col + 1],
                scale=rc[:, sc_col:sc_col + 1]), R=[u, rc], W=[outT])

    def phase_att1(self, li, j):
        k = self.k
        XT = self.scr["XT"].rearrange("(k p) t -> p k t", p=128)
        QT = self.scr["QT"].rearrange("(k p) t -> p k t", p=128)
        KT = self.scr["KT"].rearrange("(k p) t -> p k t", p=128)
        with contextlib.ExitStack() as st:
            wq = k.sb(st, "wq", [128, 8, 3 * D], BF16)
            self.wait_cvt("w_qkv", j)
            wv = self.scr["w_qkv"][j].rearrange("(k p) c -> p k c", p=128)
            for kc in range(KC):
                k.load(wq[:, kc, :], wv[:, kc, :], W=[wq], add=True)
            rc = k.sb(st, "rc", [128, 8], F32)
            k.load(rc[:], self.inp["ropec"][:, :], W=[rc])
            pf = k.sb(st, "pf", [128, 128], F32)
            k.load(pf[:], self.inp["perm"][:, :], W=[pf])
            pb = k.sb(st, "pb", [128, 128], BF16)
            k.I("dve", lambda e: e.tensor_copy(out=pb[:], in_=pf[:]), R=[pf], W=[pb])
            ri = k.sb(st, "ri", [128, 8, 64], I32)
            ci = k.sb(st, "ci", [128, 8, 64], I32)
            rowf = k.sb(st, "rowf", [128, 512], F32)
            colf = k.sb(st, "colf", [128, 512], F32)
            k.I("pool", lambda e: e.iota(ri[:], pattern=[[1, 8], [0, 64]], base=0, channel_multiplier=0), W=[ri])
            k.I("pool", lambda e: e.iota(ci[:], pattern=[[0, 8], [1, 64]], base=0, channel_multiplier=0), W=[ci])
            k.I("dve", lambda e: e.tensor_copy(out=rowf[:], in_=ri[:].rearrange("p a b -> p (a b)")), R=[ri], W=[rowf])
            k.I("dve", lambda e: e.tensor_copy(out=colf[:], in_=ci[:].rearrange("p a b -> p (a b)")), R=[ci], W=[colf])
            pos = k.sb(st, "pos", [128, 512], F32)
            u = k.sb(st, "u", [128, 512], F32)
            ki = k.sb(st, "ki", [128, 512], I32)
            kf = k.sb(st, "kf", [128, 512], F32)
            cosT = k.sb(st, "cosT", [128, 512], F32)
            sinT = k.sb(st, "sinT", [128, 512], F32)
            xt = [k.sb(st, "xt%d" % i, [128, 8, 512], F32) for i in range(2)]
            hb = [k.sb(st, "hb%d" % i, [128, 8, 512], BF16) for i in range(2)]
            qo = [k.sb(st, "qo%d" % i, [128, 8, 512], BF16) for i in range(2)]
            ko = [k.sb(st, "ko%d" % i, [128, 8, 512], BF16) for i in range(2)]
            vb = [k.sb(st, "vb%d" % i, [128, 4, D], BF16) for i in range(2)]
            qb = [k.sb(st, "qb%d" % i, [128, 512], BF16) for i in range(2)]
            t1 = [k.sb(st, "t1_%d" % i, [128, 512], F32) for i in range(2)]
            t2 = [k.sb(st, "t2_%d" % i, [128, 512], F32) for i in range(2)]
            sq = k.sb(st, "sq", [128, 512], F32)
            rstd = k.sb(st, "rstd", [128, 512], F32)
            tmp = k.sb(st, "tmp", [128, 512], F32)
            nq = 0
            for bi, (t0, n, isctx) in enumerate(self.blocks):
                x, h, qq, kk, v = xt[bi % 2], hb[bi % 2], qo[bi % 2], ko[bi % 2], vb[bi % 2]
                k.load(x[:, :, 0:n], XT[:, :, t0:t0 + n], W=[x])
                self.modulate((sq, rstd, tmp), x, n, li, self.A1, 0, isctx, out_bf=h, psb=self.psum[7])
                if not isctx:
                    self.rope_tables(st, t0, n, (rowf, colf, pos, u, ki, kf, cosT, sinT, rc))
                for hd in range(NH):
                    for (isq, col0, dst) in ((True, hd * 128, qq), (False, D + hd * 128, kk)):
                        ps = self.psum[nq % 3]
                        pr = self.psum[3 + nq % 3]
                        for kc in range(KC):
                            k.I("pe", lambda e, ps=ps, kc=kc, col0=col0: e.matmul(
                                ps[:, 0:n], lhsT=wq[:, kc, col0:col0 + 128], rhs=h[:, kc, 0:n],
                                start=(kc == 0), stop=(kc == KC - 1)), R=[wq, h], W=[ps])
                        scl = 0.125 if isq else 1.0
                        if isctx:
                            k.I("act", lambda e, ps=ps, dst=dst, hd=hd, scl=scl: e.activation(
                                out=dst[:, hd, 0:n], in_=ps[:, 0:n], func=AF.Copy, scale=scl), R=[ps], W=[dst])
                        else:
                            b_ = qb[nq % 2]
                            a1, a2 = t1[nq % 2], t2[nq % 2]
                            k.I("act", lambda e, ps=ps, b_=b_, scl=scl: e.activation(
                                out=b_[:, 0:n], in_=ps[:, 0:n], func=AF.Copy, scale=scl), R=[ps], W=[b_])
                            k.I("pe", lambda e, pr=pr, b_=b_: e.matmul(pr[:, 0:n], lhsT=pb[:], rhs=b_[:, 0:n],
                                                                       start=True, stop=True), R=[pb, b_], W=[pr])
                            k.I("dve", lambda e, a1=a1, b_=b_: e.tensor_tensor(out=a1[:, 0:n], in0=b_[:, 0:n], in1=cosT[:, 0:n],
                                                                               op=ALU.mult), R=[b_, cosT], W=[a1])
                            k.I("dve", lambda e, a2=a2, pr=pr: e.tensor_tensor(out=a2[:, 0:n], in0=pr[:, 0:n], in1=sinT[:, 0:n],
                                                                               op=ALU.mult), R=[pr, sinT], W=[a2])
                            k.I("pool", lambda e, a1=a1, a2=a2, dst=dst, hd=hd: e.tensor_tensor(
                                out=dst[:, hd, 0:n], in0=a1[:, 0:n], in1=a2[:, 0:n], op=ALU.add), R=[a1, a2], W=[dst])
                        nq += 1
                k.store(QT[:, :, t0:t0 + n], qq[:, :, 0:n], R=[qq])
                k.store(KT[:, :, t0:t0 + n], kk[:, :, 0:n], R=[kk])
                for jt in range(n // 128):
                    for half in range(2):
                        ps = self.psum[6 + half]
                        for kc in range(KC):
                            k.I("pe", lambda e, ps=ps, kc=kc, jt=jt, half=half: e.matmul(
                                ps[:, 0:512], lhsT=h[:, kc, jt * 128:(jt + 1) * 128],
                                rhs=wq[:, kc, 2 * D + half * 512:2 * D + (half + 1) * 512],
                                start=(kc == 0), stop=(kc == KC - 1)), R=[wq, h], W=[ps])
                        if half == 0:
                            k.I("act", lambda e, ps=ps, jt=jt: e.copy(out=v[:, jt, 0:512], in_=ps[:, 0:512]), R=[ps], W=[v])
                        else:
                            k.I("dve", lambda e, ps=ps, jt=jt: e.tensor_copy(out=v[:, jt, 512:1024], in_=ps[:, 0:512]),
                                R=[ps], W=[v])
                k.store(self.scr["V"][t0:t0 + n, :].rearrange("(j p) d -> p j d", p=128), v[:, 0:n // 128, :], R=[v])
            k.barrier()

    def phase_att2(self, li, j):
        k = self.k
        T = self.T
        NT = T // 128
        lambda_init = 0.8 - 0.6 * math.exp(-0.3 * li)
        with contextlib.ExitStack() as st:
            lv = k.sb(st, "lv", [128, 256], F32)
            k.load(lv[:], self.inp["attn_lambda"][j:j + 1, :].partition_broadcast(128), W=[lv])
            lp = k.sb(st, "lp", [128, 128], F32)
            ls = k.sb(st, "ls", [128, 4], F32)
            k.I("dve", lambda e: e.tensor_tensor(out=lp[:, 0:64], in0=lv[:, 0:64], in1=lv[:, 64:128], op=ALU.mult), R=[lv], W=[lp])
            k.I("dve", lambda e: e.tensor_tensor(out=lp[:, 64:128], in0=lv[:, 128:192], in1=lv[:, 192:256], op=ALU.mult),
                R=[lv], W=[lp])
            k.I("dve", lambda e: e.reduce_sum(out=ls[:, 0:1], in_=lp[:, 0:64], axis=AX.X), R=[lp], W=[ls])
            k.I("dve", lambda e: e.reduce_sum(out=ls[:, 1:2], in_=lp[:, 64:128], axis=AX.X), R=[lp], W=[ls])
            k.I("act", lambda e: e.activation(out=ls[:, 0:2], in_=ls[:, 0:2], func=AF.Exp), R=[ls], W=[ls])
            k.I("dve", lambda e: e.tensor_tensor(out=ls[:, 2:3], in0=ls[:, 1:2], in1=ls[:, 0:1], op=ALU.subtract), R=[ls], W=[ls])
            k.I("dve", lambda e: e.tensor_scalar_add(out=ls[:, 2:3], in0=ls[:, 2:3], scalar1=-lambda_init), R=[ls], W=[ls])
            sgl = k.sb(st, "sgl", [128, 1], F32)
            k.load(sgl[:], self.inp["attn_subln_g"][j].rearrange("(p o) -> p o", o=1), W=[sgl])
            k.I("dve", lambda e: e.tensor_scalar_mul(out=sgl[:], in0=sgl[:], scalar1=1.0 - lambda_init), R=[sgl], W=[sgl])
            onesb = k.sb(st, "onesb", [128, 128], BF16)
            k.I("dve", lambda e: e.tensor_copy(out=onesb[:], in_=self.ones[:]), R=[self.ones], W=[onesb])
            kT = [k.sb(st, "kT%d" % i, [128, T], BF16) for i in range(2)]
            vh = [k.sb(st, "vh%d" % i, [128, NT, 128], BF16) for i in range(2)]
            qt = [k.sb(st, "qt%d" % i, [128, 512], BF16) for i in range(2)]
            ep = [k.sb(st, "ep%d" % i, [128, 2, 512], BF16) for i in range(4)]
            gs0 = [k.sb(st, "gs0_%d" % i, [128, 512], BF16) for i in range(2)]
            gs1 = [k.sb(st, "gs1_%d" % i, [128, 512], BF16) for i in range(2)]
            es0 = k.sb(st, "es0", [128, 512], F32)
            es1 = k.sb(st, "es1", [128, 512], F32)
            r0 = k.sb(st, "r0", [128, 512], F32)
            r1 = k.sb(st, "r1", [128, 512], F32)
            o0 = k.sb(st, "o0", [128, 512], F32)
            o1 = k.sb(st, "o1", [128, 512], F32)
            sq = k.sb(st, "sq", [128, 512], F32)
            ob = [k.sb(st, "ob%d" % i, [128, 512], BF16) for i in range(2)]
            acc0, acc1 = self.psum[6], self.psum[7]
            Zp = self.psum[4]
            Zq = self.psum[5]
            bg = self.cvt_chunks(self.pending_cvt) if self.pending_cvt else []
            self.pending_cvt = []
            GRP = 6
            nb = 0
            ne = 0
            ng = 0
            Vv = self.scr["V"].rearrange("(kt p) d -> p kt d", p=128)
            for hd in range(NH):
                kt_, v_ = kT[hd % 2], vh[hd % 2]
                k.load(kt_[:], self.scr["KT"][hd * 128:(hd + 1) * 128, :], W=[kt_])
                k.load(v_[:], Vv[:, :, hd * 128:(hd + 1) * 128], W=[v_])
                for (t0, n, isctx) in self.blocks:
                    q = qt[nb % 2]
                    o_ = ob[nb % 2]
                    nb += 1
                    k.load(q[:, 0:n], self.scr["QT"][hd * 128:(hd + 1) * 128, t0:t0 + n], W=[q])
                    if bg and not isctx:
                        self.issue_chunk(bg.pop(0))
                    tiles = list(range(2)) if isctx else list(range(NT))
                    ngrp_done = 0

                    def scores(pi, kt):
                        s0, s1 = self.psum[2 * pi], self.psum[2 * pi + 1]
                        k.I("pe", lambda e: e.matmul(s0[:, 0:n], lhsT=kt_[0:64, kt * 128:(kt + 1) * 128],
                                                     rhs=q[0:64, 0:n], start=True, stop=True), R=[kt_, q], W=[s0])
                        k.I("pe", lambda e: e.matmul(s1[:, 0:n], lhsT=kt_[64:128, kt * 128:(kt + 1) * 128],
                                                     rhs=q[64:128, 0:n], start=True, stop=True), R=[kt_, q], W=[s1])

                    scores(ne % 2, tiles[0])
                    for ti, kt in enumerate(tiles):
                        pi = ne % 2
                        xp = ep[ne % 4]
                        ne += 1
                        first, last = (ti == 0), (ti == len(tiles) - 1)
                        if not last:
                            scores(ne % 2, tiles[ti + 1])
                        s0, s1 = self.psum[2 * pi], self.psum[2 * pi + 1]
                        k.I("act", lambda e, pi=pi, xp=xp: e.activation(out=xp[:, :, 0:n], in_=self.pspair[pi][:, :, 0:n],
                                                                        func=AF.Exp), R=[s0, s1], W=[xp])
                        for (acc, mi) in ((acc0, 0), (acc1, 1)):
                            k.I("pe", lambda e, acc=acc, mi=mi, xp=xp, kt=kt: e.matmul(
                                acc[:, 0:n], lhsT=v_[:, kt, :], rhs=xp[:, mi, 0:n], start=first, stop=last), R=[v_, xp], W=[acc])
                        k.I("pe", lambda e, xp=xp: e.matmul(Zp[:, 0:n], lhsT=onesb[:], rhs=xp[:, 0, 0:n], start=first, stop=last),
                            R=[onesb, xp], W=[Zp])
                        k.I("pe", lambda e, xp=xp: e.matmul(Zq[:, 0:n], lhsT=onesb[:], rhs=xp[:, 1, 0:n], start=first, stop=last),
                            R=[onesb, xp], W=[Zq])
                        continue
                        gi = ti % GRP
                        g0, g1 = gs0[ng % 2], gs1[ng % 2]
                        if gi == 0:
                            pp = xp
                        elif gi == 1:
                            k.I("pool", lambda e, g1=g1, pp=pp, xp=xp: e.tensor_tensor(out=g1[:, 0:n], in0=pp[:, 1, 0:n], in1=xp[:, 1, 0:n],
                                                                                       op=ALU.add), R=[pp, xp], W=[g1])
                        else:
                            k.I("pool", lambda e, g1=g1, xp=xp: e.tensor_tensor(out=g1[:, 0:n], in0=g1[:, 0:n], in1=xp[:, 1, 0:n],
                                                                                op=ALU.add), R=[g1, xp], W=[g1])
                        if gi == GRP - 1 or last:
                            assert gi >= 1
                            for (es, g) in ((es1, g1),):
                                if ngrp_done == 0:
                                    k.I("dve", lambda e, es=es, g=g: e.tensor_copy(out=es[:, 0:n], in_=g[:, 0:n]), R=[g], W=[es])
                                else:
                                    k.I("dve", lambda e, es=es, g=g: e.tensor_tensor(out=es[:, 0:n], in0=g[:, 0:n], in1=es[:, 0:n],
                                                                                     op=ALU.add), R=[g, es], W=[es])
                            ngrp_done += 1
                            ng += 1
                    Z0 = Zp
                    Z1 = Zq
                    k.I("dve", lambda e: e.reciprocal(out=r0[:, 0:n], in_=Z0[:, 0:n]), R=[Z0], W=[r0])
                    k.I("dve", lambda e: e.reciprocal(out=r1[:, 0:n], in_=Z1[:, 0:n]), R=[Z1], W=[r1])
                    k.I("dve", lambda e: e.tensor_tensor(out=o0[:, 0:n], in0=acc0[:, 0:n], in1=r0[:, 0:n], op=ALU.mult),
                        R=[acc0, r0], W=[o0])
                    k.I("dve", lambda e: e.tensor_tensor(out=o1[:, 0:n], in0=acc1[:, 0:n], in1=r1[:, 0:n], op=ALU.mult),
                        R=[acc1, r1], W=[o1])
                    k.I("dve", lambda e: e.scalar_tensor_tensor(out=o0[:, 0:n], in0=o1[:, 0:n], scalar=ls[:, 2:3], in1=o0[:, 0:n],
                                                                op0=ALU.mult, op1=ALU.add), R=[o1, ls, o0], W=[o0])
                    k.I("act", lambda e: e.activation(out=sq[:, 0:n], in_=o0[:, 0:n], func=AF.Square), R=[o0], W=[sq])
                    pss = self.psum[(ne % 2) * 2]
                    k.I("pe", lambda e, pss=pss: e.matmul(pss[:, 0:n], lhsT=self.ones[:], rhs=sq[:, 0:n], start=True, stop=True),
                        R=[self.ones, sq], W=[pss])
                    k.I("act", lambda e, pss=pss: e.activation(out=r0[:, 0:n], in_=pss[:, 0:n], func=AF.Sqrt, bias=self.epsb[:],
                                                               scale=1.0 / 128.0), R=[pss, self.epsb], W=[r0])
                    k.I("dve", lambda e: e.reciprocal(out=r0[:, 0:n], in_=r0[:, 0:n]), R=[r0], W=[r0])
                    k.I("dve", lambda e, o_=o_: e.scalar_tensor_tensor(out=o_[:, 0:n], in0=o0[:, 0:n], scalar=sgl[:, 0:1],
                                                                       in1=r0[:, 0:n], op0=ALU.mult, op1=ALU.mult),
                        R=[o0, sgl, r0], W=[o_])
                    k.store(self.scr["ST"][hd * 128:(hd + 1) * 128, t0:t0 + n], o_[:, 0:n], R=[o_])
            while bg:
                self.issue_chunk(bg.pop(0))
            k.barrier()

    def phase_lru1(self, li, j):
        k = self.k
        XT = self.scr["XT"].rearrange("(k p) t -> p k t", p=128)
        GT = self.scr["GATE"].rearrange("(k p) t -> p k t", p=128)
        RT = self.scr["R"].rearrange("(k p) t -> p k t", p=128)
        with contextlib.ExitStack() as st:
            win = k.sb(st, "win", [128, 8, 2 * D], BF16)
            self.wait_cvt("w_in", j)
            wv = self.scr["w_in"][j].rearrange("(k p) c -> p k c", p=128)
            for kc in range(KC):
                k.load(win[:, kc, :], wv[:, kc, :], W=[win], add=True)
            xt = [k.sb(st, "xt%d" % i, [128, 8, 512], F32) for i in range(2)]
            hb = [k.sb(st, "hb%d" % i, [128, 8, 512], BF16) for i in range(2)]
            gt = [k.sb(st, "gt%d" % i, [128, 8, 512], BF16) for i in range(2)]
            rt = [k.sb(st, "rt%d" % i, [128, 8, 512], F32) for i in range(2)]
            g1 = [k.sb(st, "g1_%d" % i, [128, 512], F32) for i in range(2)]
            g2 = [k.sb(st, "g2_%d" % i, [128, 512], F32) for i in range(2)]
            sq = k.sb(st, "sq", [128, 512], F32)
            rstd = k.sb(st, "rstd", [128, 512], F32)
            tmp = k.sb(st, "tmp", [128, 512], F32)
            for bi, (t0, n, isctx) in enumerate(self.blocks):
                x, h, g_, r_ = xt[bi % 2], hb[bi % 2], gt[bi % 2], rt[bi % 2]
                k.load(x[:, :, 0:n], XT[:, :, t0:t0 + n], W=[x])
                self.modulate((sq, rstd, tmp), x, n, li, self.A1, 0, isctx, out_bf=h, psb=self.psum[7])
                for c in range(16):
                    ps = self.psum[c % 4]
                    for kc in range(KC):
                        k.I("pe", lambda e, ps=ps, kc=kc, c=c: e.matmul(
                            ps[:, 0:n], lhsT=win[:, kc, c * 128:(c + 1) * 128], rhs=h[:, kc, 0:n],
                            start=(kc == 0), stop=(kc == KC - 1)), R=[win, h], W=[ps])
                    if c < 8:
                        a, b_ = g1[c % 2], g2[c % 2]
                        k.I("act", lambda e, ps=ps, a=a: e.activation(out=a[:, 0:n], in_=ps[:, 0:n], func=AF.Square), R=[ps], W=[a])
                        k.I("dve", lambda e, a=a: e.tensor_scalar(out=a[:, 0:n], in0=a[:, 0:n], scalar1=0.044715, scalar2=1.0,
                                                                  op0=ALU.mult, op1=ALU.add), R=[a], W=[a])
                        k.I("dve", lambda e, a=a, ps=ps: e.tensor_tensor(out=a[:, 0:n], in0=ps[:, 0:n], in1=a[:, 0:n], op=ALU.mult),
                            R=[ps, a], W=[a])
                        k.I("act", lambda e, a=a, b_=b_: e.activation(out=b_[:, 0:n], in_=a[:, 0:n], func=AF.Sigmoid,
                                                                      scale=1.5957691216057308), R=[a], W=[b_])
                        k.I("dve", lambda e, b_=b_, ps=ps, c=c: e.tensor_tensor(out=g_[:, c, 0:n], in0=ps[:, 0:n], in1=b_[:, 0:n],
                                                                                op=ALU.mult), R=[ps, b_], W=[g_])
                    else:
                        k.I("act", lambda e, ps=ps, c=c: e.copy(out=r_[:, c - 8, 0:n], in_=ps[:, 0:n]), R=[ps], W=[r_])
                k.store(GT[:, :, t0:t0 + n], g_[:, :, 0:n], R=[g_])
                k.store(RT[:, :, t0:t0 + n], r_[:, :, 0:n], R=[r_])
            k.barrier()

    def phase_lru2(self, li, j):
        k = self.k
        T = self.T
        segs = [(0, CTX), (CTX, T)]
        with contextlib.ExitStack() as st:
            Rb = k.sb(st, "Rb", [128, T], F32)
            U = k.sb(st, "U", [128, T], F32)
            A = k.sb(st, "A", [128, T], F32)
            B1 = k.sb(st, "B1", [128, T], F32)
            gw = k.sb(st, "gw", [128, 2, 2, 128], F32)
            gb = k.sb(st, "gb", [128, 4], F32)
            ap_ = k.sb(st, "ap", [128, 2], F32)
            sc8 = k.sb(st, "sc8", [128, 2], F32)
            cw = k.sb(st, "cw", [128, 4], F32)
            cb = k.sb(st, "cb", [128, 1], F32)
            rr = [k.sb(st, "rr%d" % i, [128, 512], F32) for i in range(2)]
            ii = [k.sb(st, "ii%d" % i, [128, 512], F32) for i in range(2)]
            s2 = [k.sb(st, "s2_%d" % i, [128, 512], F32) for i in range(2)]
            gl = [k.sb(st, "gl%d" % i, [128, 512], BF16) for i in range(2)]
            so = [k.sb(st, "so%d" % i, [128, 512], BF16) for i in range(2)]
            hs = [k.sb(st, "hs%d" % i, [128, 512], F32) for i in range(2)]
            nn = 0
            for kc in range(KC):
                sl = slice(kc * 128, (kc + 1) * 128)
                k.load(Rb[:], self.scr["R"][sl, :], W=[Rb])
                k.load(gw[:], self.inp["lru_gate_w"][j][:, :, kc].rearrange("d g c o -> c d g o"), W=[gw])
                k.load(gb[:], self.inp["lru_gate_b"][j][:, :, sl].rearrange("d g p -> p (d g)"), W=[gb],
                       allow_slow_non_contiguous=True)
                k.load(ap_[:], self.inp["lru_a_param"][j][:, sl].rearrange("d p -> p d"), W=[ap_], allow_slow_non_contiguous=True)
                k.load(cw[:], self.inp["lru_conv_w"][j][:, sl].rearrange("w p -> p w"), W=[cw], allow_slow_non_contiguous=True)
                k.load(cb[:], self.inp["lru_conv_b"][j][sl].rearrange("(p o) -> p o", o=1), W=[cb])
                k.I("act", lambda e: e.activation(out=sc8[:], in_=ap_[:], func=AF.Sigmoid), R=[ap_], W=[sc8])
                k.I("act", lambda e: e.activation(out=sc8[:], in_=sc8[:], func=AF.Ln), R=[sc8], W=[sc8])
                k.I("dve", lambda e: e.tensor_scalar_mul(out=sc8[:], in0=sc8[:], scalar1=8.0), R=[sc8], W=[sc8])
                k.I("dve", lambda e: e.tensor_scalar(out=U[:], in0=Rb[:], scalar1=cw[:, 2:3], scalar2=cb[:, 0:1],
                                                     op0=ALU.mult, op1=ALU.add), R=[Rb, cw, cb], W=[U])
                for (s_, e_) in segs:
                    for (tap, dlo, dhi, slo, shi) in ((0, s_ + 2, e_, s_, e_ - 2), (1, s_ + 1, e_, s_, e_ - 1),
                                                      (3, s_, e_ - 1, s_ + 1, e_)):
                        k.I("dve", lambda e, tap=tap, dlo=dlo, dhi=dhi, slo=slo, shi=shi: e.scalar_tensor_tensor(
                            out=U[:, dlo:dhi], in0=Rb[:, slo:shi], scalar=cw[:, tap:tap + 1], in1=U[:, dlo:dhi],
                            op0=ALU.mult, op1=ALU.add), R=[Rb, cw, U], W=[U])
                for d in range(2):
                    Bd = B1 if d == 0 else Rb
                    for (t0, n, isctx) in self.blocks:
                        pr, pi = self.psum[(2 * nn) % 4], self.psum[(2 * nn + 1) % 4]
                        r_, i_, q_ = rr[nn % 2], ii[nn % 2], s2[nn % 2]
                        nn += 1
                        k.I("pe", lambda e, pr=pr: e.matmul(pr[:, 0:n], lhsT=gw[:, d, 0, :], rhs=U[:, t0:t0 + n], start=True, stop=True),
                            R=[gw, U], W=[pr])
                        k.I("pe", lambda e, pi=pi: e.matmul(pi[:, 0:n], lhsT=gw[:, d, 1, :], rhs=U[:, t0:t0 + n], start=True, stop=True),
                            R=[gw, U], W=[pi])
                        k.I("act", lambda e, pr=pr, r_=r_: e.activation(out=r_[:, 0:n], in_=pr[:, 0:n], func=AF.Sigmoid,
                                                                        bias=gb[:, 2 * d:2 * d + 1]), R=[pr, gb], W=[r_])
                        k.I("act", lambda e, pi=pi, i_=i_: e.activation(out=i_[:, 0:n], in_=pi[:, 0:n], func=AF.Sigmoid,
                                                                        bias=gb[:, 2 * d + 1:2 * d + 2]), R=[pi, gb], W=[i_])
                        k.I("act", lambda e, r_=r_: e.activation(out=A[:, t0:t0 + n], in_=r_[:, 0:n], func=AF.Exp,
                                                                 scale=sc8[:, d:d + 1]), R=[r_, sc8], W=[A])
                        k.I("act", lambda e, q_=q_: e.activation(out=q_[:, 0:n], in_=A[:, t0:t0 + n], func=AF.Square), R=[A], W=[q_])
                        k.I("act", lambda e, q_=q_: e.activation(out=q_[:, 0:n], in_=q_[:, 0:n], func=AF.Sqrt, bias=self.ones[:, 0:1],
                                                                 scale=-1.0), R=[q_, self.ones], W=[q_])
                        k.I("dve", lambda e, i_=i_: e.tensor_tensor(out=i_[:, 0:n], in0=i_[:, 0:n], in1=U[:, t0:t0 + n], op=ALU.mult),
                            R=[i_, U], W=[i_])
                        k.I("dve", lambda e, i_=i_, q_=q_, Bd=Bd: e.tensor_tensor(out=Bd[:, t0:t0 + n], in0=i_[:, 0:n], in1=q_[:, 0:n],
                                                                                  op=ALU.mult), R=[i_, q_], W=[Bd])
                    if d == 0:
                        k.I("dve", lambda e, Bd=Bd: e.tensor_tensor_scan(out=Bd[:, 0:CTX], data0=A[:, 0:CTX], data1=Bd[:, 0:CTX],
                                                                         initial=0.0, op0=ALU.mult, op1=ALU.add), R=[A, Bd], W=[Bd])
                        k.I("dve", lambda e, Bd=Bd: e.tensor_tensor_scan(out=Bd[:, CTX:T], data0=A[:, CTX:T], data1=Bd[:, CTX:T],
                                                                         initial=Bd[:, CTX - 1:CTX], op0=ALU.mult, op1=ALU.add),
                            R=[A, Bd], W=[Bd])
                    else:
                        k.I("dve", lambda e, Bd=Bd: e.tensor_tensor_scan(
                            out=Bd[:, 0:CTX][:, ::-1], data0=A[:, 0:CTX][:, ::-1], data1=Bd[:, 0:CTX][:, ::-1],
                            initial=0.0, op0=ALU.mult, op1=ALU.add), R=[A, Bd], W=[Bd])
                        k.I("dve", lambda e, Bd=Bd: e.tensor_tensor_scan(
                            out=Bd[:, CTX:T][:, ::-1], data0=A[:, CTX:T][:, ::-1], data1=Bd[:, CTX:T][:, ::-1],
                            initial=Bd[:, 0:1], op0=ALU.mult, op1=ALU.add), R=[A, Bd], W=[Bd])
                for bi, (t0, n, isctx) in enumerate(self.blocks):
                    g_, s_o, h_ = gl[bi % 2], so[bi % 2], hs[bi % 2]
                    k.load(g_[:, 0:n], self.scr["GATE"][sl, t0:t0 + n], W=[g_])
                    k.I("dve", lambda e, h_=h_: e.tensor_tensor(out=h_[:, 0:n], in0=B1[:, t0:t0 + n], in1=Rb[:, t0:t0 + n], op=ALU.add),
                        R=[B1, Rb], W=[h_])
                    k.I("pool", lambda e, h_=h_, g_=g_, s_o=s_o: e.tensor_tensor(out=s_o[:, 0:n], in0=h_[:, 0:n], in1=g_[:, 0:n],
                                                                                op=ALU.mult), R=[h_, g_], W=[s_o])
                    k.store(self.scr["ST"][sl, t0:t0 + n], s_o[:, 0:n], R=[s_o])
            k.barrier()


def host_consts():
    ident = np.eye(128, dtype=np.float32)
    ones = np.ones((128, 128), dtype=np.float32)
    perm = np.zeros((128, 128), dtype=np.float32)
    ropec = np.zeros((128, 8), dtype=np.float32)
    for p in range(128):
        d = p % 64
        a = d // 32
        hf = (d % 32) // 16
        i = d % 16
        partner = p + 16 if hf == 0 else p - 16
        perm[partner, p] = 1.0
        invf = 1.0 / (10000.0 ** ((2.0 * i) / 32.0))
        sgn = -1.0 if hf == 0 else 1.0
        ropec[p, 0] = 1.0 if a == 0 else 0.0
        ropec[p, 1] = 1.0 if a == 1 else 0.0
        ropec[p, 2] = invf / (2.0 * math.pi)
        ropec[p, 3] = sgn * 2.0 * math.pi
        ropec[p, 4] = -sgn * math.pi
        ropec[p, 5] = 2.0 * math.pi
        ropec[p, 6] = -math.pi
    return {"ident": ident, "ones": ones, "perm": perm, "ropec": ropec}


def make_in_map(inputs, b, nlat=8192, shapes=None):
    m = {}

    def f(a):
        return np.ascontiguousarray(np.asarray(a, dtype=np.float32))
    m["x"] = f(inputs["x"][b][:nlat])
    m["c"] = f(inputs["c"][b])
    m["ctx"] = f(inputs["ctx"][b])
    for name in ("c_ctx", "mod_w", "mod_b", "norm_mix_g", "norm_ffn_g", "attn_w_qkv", "attn_w_o", "lru_w_in",
                 "lru_conv_w", "lru_conv_b", "lru_gate_w", "lru_gate_b", "lru_a_param", "lru_w_out",
                 "ffn_w_gate_up", "ffn_w_down", "moe_router_w", "moe_w_gate_up", "moe_w_down", "final_norm_g"):
        m[name] = f(inputs[name])
    m["attn_lambda"] = f(inputs["attn_lambda"]).reshape(2, 256)
    m["attn_subln_g"] = f(inputs["attn_subln_g"])
    m.update(host_consts())
    if shapes is not None:
        for kk in list(m):
            if shapes[kk] == [1, 1] and m[kk].shape != (1, 1):
                m[kk] = np.zeros((1, 1), np.float32)
    return m


def kernel(**inputs):
    prog = Prog()
    nc = prog.build()
    B = inputs["x"].shape[0]
    in_maps = [make_in_map(inputs, b) for b in range(B)]
    res = run_bass_kernel_spmd(nc, in_maps, core_ids=list(range(B)))
    return np.stack([np.asarray(res.results[b]["out"], dtype=np.float32) for b in range(B)], axis=0)
```

```python
import contextlib
import math
import numpy as np
import concourse.bass as bass
import concourse.mybir as mybir
from concourse.bass_utils import run_bass_kernel_spmd

F32 = mybir.dt.float32
BF16 = mybir.dt.bfloat16
I32 = mybir.dt.int32
AF = mybir.ActivationFunctionType
ALU = mybir.AluOpType
AX = mybir.AxisListType

D = 1024
KC = 8
CTX = 256
NH = 8
DFF = 2816
DFE = 3584
NE = 8
EPS = 1e-6
EPOCH = 30000
SAME_ENGINE_SYNC = True


class DmaSem:
    def __init__(self, k):
        self.k = k
        self.sem = k.new_sem()
        self.count = 0

    def bump(self):
        if self.count + 16 > EPOCH:
            self.sem = self.k.new_sem()
            self.count = 0
        self.count += 16
        return self.sem, self.count


class Buf:
    def __init__(self, k, name, t):
        self.k = k
        self.name = name
        self.t = t
        self.lw = None
        self.rd = {}
        self.ld = None
        self.st = None

    def __getitem__(self, key):
        return self.t[key]


class HalfView:
    def __init__(self, t, h):
        self.t = t
        self.h = h

    def __getitem__(self, key):
        return self.t[(key[0], self.h) + tuple(key[1:])]


class K:
    def __init__(self, nc):
        self.nc = nc
        self.es = contextlib.ExitStack()
        self.engs = {"pe": nc.tensor, "act": nc.scalar, "dve": nc.vector, "pool": nc.gpsimd, "sp": nc.sync}
        self.cnt = {e: 0 for e in self.engs}
        self.esems = {e: [] for e in self.engs}
        self.waited = {e: {} for e in self.engs}
        self.nsem = 0
        self.dsems = []
        self.free_dsems = []
        self.live_dsems = []
        self.uid = 0

    def new_sem(self):
        self.nsem += 1
        return self.es.enter_context(self.nc.semaphore("s%d" % self.nsem))

    def esem(self, eng, seq):
        i = (seq - 1) // EPOCH
        while len(self.esems[eng]) <= i:
            self.esems[eng].append(self.new_sem())
        return self.esems[eng][i], (seq - 1) % EPOCH + 1

    def new_dsem(self):
        if self.free_dsems:
            d = self.free_dsems.pop()
        else:
            d = DmaSem(self)
            self.dsems.append(d)
        self.live_dsems.append(d)
        return d

    def sb(self, stack, name, shape, dt):
        self.uid += 1
        t = stack.enter_context(self.nc.sbuf_tensor("%s_%d" % (name, self.uid), list(shape), dt))
        return Buf(self, name, t)

    def ps(self, stack, name, shape, dt):
        self.uid += 1
        t = stack.enter_context(self.nc.psum_tensor("%s_%d" % (name, self.uid), list(shape), dt))
        return Buf(self, name, t)

    def _wait(self, eng, dep):
        w = self.waited[eng]
        if dep[0] == "e":
            _, src, seq = dep
            if src == eng and (not SAME_ENGINE_SYNC or eng == "pe"):
                return
            if w.get(src, 0) >= seq:
                return
            w[src] = seq
            sem, val = self.esem(src, seq)
            self.engs[eng].wait_ge(sem, val)
        else:
            _, sem, val, sid = dep
            key = ("d", sid)
            if w.get(key, 0) >= val:
                return
            w[key] = val
            self.engs[eng].wait_ge(sem, val)

    def _deps(self, eng, R, W, skip_waw_dma=False):
        for b in R:
            if b.lw is not None:
                self._wait(eng, b.lw)
        for b in W:
            if b.lw is not None and not (skip_waw_dma and b.lw[0] == "d"):
                self._wait(eng, b.lw)
            for d in b.rd.values():
                self._wait(eng, d)

    def I(self, eng, fn, R=(), W=()):
        self._deps(eng, R, W)
        ins = fn(self.engs[eng])
        self.cnt[eng] += 1
        seq = self.cnt[eng]
        sem, _ = self.esem(eng, seq)
        ins.then_inc(sem, 1)
        tag = ("e", eng, seq)
        for b in R:
            b.rd[eng] = tag
        for b in W:
            b.lw = tag
            b.rd = {}
        return ins

    def dma(self, q, out, in_, R=(), W=(), add=False, **kw):
        self._deps(q, R, W, skip_waw_dma=add)
        if W:
            if W[0].ld is None:
                W[0].ld = self.new_dsem()
            ds = W[0].ld
        else:
            if R[0].st is None:
                R[0].st = self.new_dsem()
            ds = R[0].st
        ins = self.engs[q].dma_start(out=out, in_=in_, **kw)
        sem, val = ds.bump()
        ins.then_inc(sem, 16)
        tag = ("d", sem, val, id(sem))
        for b in W:
            b.lw = tag
            b.rd = {}
        for b in R:
            b.rd[("st", id(sem))] = tag
        return ins

    def load(self, out, in_, W, **kw):
        return self.dma("sp", out, in_, W=W, **kw)

    def store(self, out, in_, R, **kw):
        return self.dma("pool", out, in_, R=R, **kw)

    def barrier(self):
        pool = self.engs["pool"]
        for d in self.dsems:
            if d.count > 0:
                self._wait("pool", ("d", d.sem, d.count, id(d.sem)))
        for e in ("pe", "act", "dve"):
            if self.cnt[e] > 0:
                self._wait("pool", ("e", e, self.cnt[e]))
        ins = pool.nop()
        self.cnt["pool"] += 1
        seq = self.cnt["pool"]
        sem, _ = self.esem("pool", seq)
        ins.then_inc(sem, 1)
        for e in ("pe", "act", "dve", "sp"):
            self._wait(e, ("e", "pool", seq))
        self.free_dsems.extend(self.live_dsems)
        self.live_dsems = []


class Prog:
    def __init__(self, nlat=8192, layers=(0, 1, 2, 3), dbg=(), do_final=True, ncores=4, conv_all=True):
        self.nlat = nlat
        self.T = CTX + nlat
        self.layers = list(layers)
        self.dbg = set(dbg)
        self.do_final = do_final
        self.in_shapes = {}
        nc = bass.Bass("TRN2", target_bir_lowering=False)
        self.nc = nc
        self.k = K(nc)
        self.blocks = [(0, CTX, True)] + [(CTX + i * 512, 512, False) for i in range(nlat // 512)]
        sbs = []
        cur = []
        tot = 0
        for b in self.blocks:
            if tot + b[1] > 1792:
                sbs.append(cur)
                cur, tot = [], 0
            cur.append(b)
            tot += b[1]
        if cur:
            sbs.append(cur)
        self.superblocks = sbs
        self.decl()

    def din(self, name, shape, dt=F32):
        if not self.used(name):
            shape = [1, 1]
        self.in_shapes[name] = list(shape)
        return self.nc.dram_tensor(name, list(shape), dt, kind="ExternalInput").ap()

    def used(self, name):
        att = any(l % 2 == 0 for l in self.layers)
        lru = any(l % 2 == 1 for l in self.layers)
        if name.startswith("attn_") or name.startswith("ffn_") or name in ("perm", "ropec"):
            return att
        if name.startswith("lru_") or name.startswith("moe_"):
            return lru
        return True

    def dscr(self, name, shape, dt):
        kind = "ExternalOutput" if name in self.dbg else "Internal"
        return self.nc.dram_tensor(name, list(shape), dt, kind=kind).ap()

    def decl(self):
        T = self.T
        i = {}
        i["x"] = self.din("x", [self.nlat, D])
        i["c"] = self.din("c", [D])
        i["ctx"] = self.din("ctx", [CTX, D])
        i["c_ctx"] = self.din("c_ctx", [D])
        i["mod_w"] = self.din("mod_w", [4, D, 6 * D])
        i["mod_b"] = self.din("mod_b", [4, 6 * D])
        i["norm_mix_g"] = self.din("norm_mix_g", [4, D])
        i["norm_ffn_g"] = self.din("norm_ffn_g", [4, D])
        i["attn_w_qkv"] = self.din("attn_w_qkv", [2, D, 3 * D])
        i["attn_w_o"] = self.din("attn_w_o", [2, D, D])
        i["attn_lambda"] = self.din("attn_lambda", [2, 4 * 64])
        i["attn_subln_g"] = self.din("attn_subln_g", [2, 128])
        i["lru_w_in"] = self.din("lru_w_in", [2, D, 2 * D])
        i["lru_conv_w"] = self.din("lru_conv_w", [2, 4, D])
        i["lru_conv_b"] = self.din("lru_conv_b", [2, D])
        i["lru_gate_w"] = self.din("lru_gate_w", [2, 2, 2, 8, 128, 128])
        i["lru_gate_b"] = self.din("lru_gate_b", [2, 2, 2, D])
        i["lru_a_param"] = self.din("lru_a_param", [2, 2, D])
        i["lru_w_out"] = self.din("lru_w_out", [2, D, D])
        i["ffn_w_gate_up"] = self.din("ffn_w_gate_up", [2, D, 2 * DFF])
        i["ffn_w_down"] = self.din("ffn_w_down", [2, DFF, D])
        i["moe_router_w"] = self.din("moe_router_w", [2, D, NE])
        i["moe_w_gate_up"] = self.din("moe_w_gate_up", [2, NE, D, 2 * DFE])
        i["moe_w_down"] = self.din("moe_w_down", [2, NE, DFE, D])
        i["final_norm_g"] = self.din("final_norm_g", [D])
        i["ident"] = self.din("ident", [128, 128])
        i["ones"] = self.din("ones", [128, 128])
        i["ropec"] = self.din("ropec", [128, 8])
        i["perm"] = self.din("perm", [128, 128])
        self.inp = i
        self.out = self.nc.dram_tensor("out", [self.nlat, D], F32, kind="ExternalOutput").ap()
        s = {}
        s["XT"] = self.dscr("XT", [D, T], F32)
        s["FT"] = self.dscr("FT", [D, T], BF16)
        s["ST"] = self.dscr("ST", [D, T], BF16)
        s["G"] = self.dscr("G", [NE, T], F32)
        s["QT"] = self.dscr("QT", [D, T], BF16)
        s["KT"] = self.dscr("KT", [D, T], BF16)
        s["V"] = self.dscr("V", [T, D], BF16)
        s["GATE"] = self.dscr("GATE", [D, T], BF16)
        s["R"] = self.dscr("R", [D, T], F32)
        s["w_qkv"] = self.dscr("w_qkv", [2, D, 3 * D], BF16)
        s["w_o"] = self.dscr("w_o", [2, D, D], BF16)
        s["w_in"] = self.dscr("w_in", [2, D, 2 * D], BF16)
        s["w_out"] = self.dscr("w_out", [2, D, D], BF16)
        s["w_fgu"] = self.dscr("w_fgu", [2, D, 2 * DFF], BF16)
        s["w_fd"] = self.dscr("w_fd", [2, DFF, D], BF16)
        s["w_mgu"] = self.dscr("w_mgu", [2, NE, D, 2 * DFE], BF16)
        s["w_md"] = self.dscr("w_md", [2, NE, DFE, D], BF16)
        self.scr = s

    def build(self):
        nc, k = self.nc, self.k
        with nc.allow_low_precision("bf16 matmul operands, fp32 accumulation"):
            with contextlib.ExitStack() as g:
                self.g = g
                self.consts(g)
                self.phase_convert()
                self.phase_mod()
                self.phase_x0()
                for li in self.layers:
                    j = li // 2
                    if li % 2 == 0:
                        self.phase_att1(li, j)
                        self.phase_att2(li, j)
                        self.phase_post(li, j, self.scr["w_o"][j])
                        self.phase_ffn(li, [(self.scr["w_fgu"][j], self.scr["w_fd"][j])], DFF, 2, gated=False)
                    else:
                        self.phase_lru1(li, j)
                        self.phase_lru2(li, j)
                        self.phase_post(li, j, self.scr["w_out"][j], router=True)
                        self.phase_ffn(li, [(self.scr["w_mgu"][j][e], self.scr["w_md"][j][e]) for e in range(NE)],
                                       DFE, 4, gated=True)
                if self.do_final:
                    self.phase_final()
                k.barrier()
            k.es.close()
        return nc

    def consts(self, g):
        k = self.k
        self.pspair = [k.ps(g, "pp%d" % i, [128, 2, 512], F32) for i in range(4)]
        self.psum = []
        for i in range(8):
            b = Buf(k, "ps%d" % i, HalfView(self.pspair[i // 2].t, i % 2))
            self.psum.append(b)
        self.ident = k.sb(g, "ident", [128, 128], F32)
        self.ones = k.sb(g, "ones", [128, 128], F32)
        self.modT = k.sb(g, "modT", [128, 4, 48, 2], F32)
        self.A1 = k.sb(g, "A1", [128, 4, 8, 2], F32)
        self.A2 = k.sb(g, "A2", [128, 4, 8, 2], F32)
        self.epsb = k.sb(g, "epsb", [128, 1], F32)
        k.load(self.ident[:], self.inp["ident"][:, :], W=[self.ident])
        k.load(self.ones[:], self.inp["ones"][:, :], W=[self.ones])
        k.I("dve", lambda e: e.memset(self.epsb[:], EPS), W=[self.epsb])

    def cvt_list(self, li):
        j = li // 2
        if li % 2 == 0:
            return [("attn_w_qkv", "w_qkv", j), ("attn_w_o", "w_o", j), ("ffn_w_gate_up", "w_fgu", j),
                    ("ffn_w_down", "w_fd", j)]
        return [("lru_w_in", "w_in", j), ("lru_w_out", "w_out", j), ("moe_w_gate_up", "w_mgu", j),
                ("moe_w_down", "w_md", j)]

    def cvt_chunks(self, pairs):
        k = self.k
        out = []
        for src, dst, j in pairs:
            a = self.inp[src][j]
            b = self.scr[dst][j]
            n = 1
            for s_ in a.shape:
                n *= s_
            a2 = a.flatten().rearrange("(r c) -> r c", c=2048)
            b2 = b.flatten().rearrange("(r c) -> r c", c=2048)
            rows = n // 2048
            step = 4096
            ds = DmaSem(k)
            rs = list(range(0, rows, step))
            for r0 in rs:
                r1 = min(rows, r0 + step)
                out.append((dst, j, a2[r0:r1, :], b2[r0:r1, :], ds, r0 == rs[-1]))
        return out

    def issue_chunk(self, ch):
        dst, j, a, b, ds, is_last = ch
        ins = self.k.engs["pool"].dma_start(out=b, in_=a)
        sem, val = ds.bump()
        ins.then_inc(sem, 16)
        if is_last:
            self.cvt_tag[(dst, j)] = ("d", ds.sem, ds.count, id(ds.sem))

    def issue_cvt(self, pairs):
        for ch in self.cvt_chunks(pairs):
            self.issue_chunk(ch)

    def wait_cvt(self, dst, j):
        tag = self.cvt_tag.get((dst, j))
        if tag is not None:
            self.k._wait("sp", tag)

    def phase_convert(self):
        k = self.k
        self.cvt_tag = {}
        self.pending_cvt = []
        first = True
        for li in self.layers:
            if first or li % 2 == 1 and not any(l % 2 == 0 for l in self.layers):
                self.issue_cvt(self.cvt_list(li))
            else:
                self.pending_cvt += self.cvt_list(li)
            first = False
        if not any(l % 2 == 0 for l in self.layers) and self.pending_cvt:
            self.issue_cvt(self.pending_cvt)
            self.pending_cvt = []

    def phase_mod(self):
        k, nc = self.k, self.nc
        with contextlib.ExitStack() as st:
            cin = k.sb(st, "cin", [128, 8, 2], F32)
            sc = k.sb(st, "sc", [128, 8, 2], F32)
            wt = [k.sb(st, "modw%d" % i, [128, 6 * D], F32) for i in range(2)]
            mb = k.sb(st, "mb", [128, 48], F32)
            gm = k.sb(st, "gm", [128, 8], F32)
            k.load(cin[:, :, 0], self.inp["c"].rearrange("(k p) -> p k", p=128), W=[cin],
                   allow_slow_non_contiguous=True)
            k.load(cin[:, :, 1], self.inp["c_ctx"].rearrange("(k p) -> p k", p=128), W=[cin], add=True,
                   allow_slow_non_contiguous=True)
            k.I("act", lambda e: e.activation(out=sc[:], in_=cin[:], func=AF.Silu), R=[cin], W=[sc])
            n = 0
            macc = k.sb(st, "macc", [128, 96], F32)
            for li in self.layers:
                k.load(mb[:], self.inp["mod_b"][li].rearrange("(j p) -> p j", p=128), W=[mb],
                       allow_slow_non_contiguous=True)
                for kc in range(KC):
                    ps = self.psum[kc % 2]
                    w = wt[n % 2]
                    n += 1
                    k.load(w[:], self.inp["mod_w"][li][kc * 128:(kc + 1) * 128, :], W=[w])
                    for jc in range(48):
                        k.I("pe", lambda e, w=w, jc=jc, kc=kc, ps=ps: e.matmul(
                            ps[:, jc * 2:jc * 2 + 2], lhsT=w[:, jc * 128:(jc + 1) * 128], rhs=sc[:, kc, :],
                            start=True, stop=True), R=[w, sc], W=[ps])
                    if kc == 0:
                        k.I("dve", lambda e, ps=ps: e.tensor_copy(out=macc[:], in_=ps[:, 0:96]), R=[ps], W=[macc])
                    else:
                        k.I("dve", lambda e, ps=ps: e.tensor_tensor(out=macc[:], in0=ps[:, 0:96], in1=macc[:], op=ALU.add),
                            R=[ps, macc], W=[macc])
                for t in range(2):
                    k.I("dve", lambda e, t=t, li=li: e.tensor_tensor(
                        out=self.modT[:, li, :, t], in0=macc[:].rearrange("p (j t) -> p j t", t=2)[:, :, t],
                        in1=mb[:], op=ALU.add), R=[macc, mb], W=[self.modT])
                for (A, gname, off) in ((self.A1, "norm_mix_g", 8), (self.A2, "norm_ffn_g", 32)):
                    k.load(gm[:], self.inp[gname][li].rearrange("(k p) -> p k", p=128), W=[gm],
                           allow_slow_non_contiguous=True)
                    for t in range(2):
                        k.I("dve", lambda e, A=A, off=off, t=t, li=li: e.scalar_tensor_tensor(
                            out=A[:, li, :, t], in0=self.modT[:, li, off:off + 8, t], scalar=1.0, in1=gm[:],
                            op0=ALU.add, op1=ALU.mult), R=[self.modT, gm], W=[A])
            k.barrier()

    def mv(self, li, which, kc, isctx):
        return self.modT[:, li, which * 8 + kc, (1 if isctx else 0):(2 if isctx else 1)]

    def phase_x0(self):
        k = self.k
        XT = self.scr["XT"].rearrange("(k p) t -> p k t", p=128)
        with contextlib.ExitStack() as st:
            xin = [k.sb(st, "xin%d" % i, [128, 4, D], F32) for i in range(2)]
            xo = [k.sb(st, "xo%d" % i, [128, 8, 512], F32) for i in range(2)]
            for bi, (t0, n, isctx) in enumerate(self.blocks):
                xi, o = xin[bi % 2], xo[bi % 2]
                nt = n // 128
                src = self.inp["ctx"] if isctx else self.inp["x"][t0 - CTX:t0 - CTX + n, :]
                k.load(xi[:, 0:nt, :], src.rearrange("(j p) d -> p j d", p=128), W=[xi])
                for kc in range(KC):
                    ps = self.psum[kc]
                    for j in range(nt):
                        k.I("pe", lambda e, ps=ps, j=j, kc=kc, xi=xi: e.transpose(
                            ps[:, j * 128:(j + 1) * 128], xi[:, j, kc * 128:(kc + 1) * 128], self.ident[:]),
                            R=[xi, self.ident], W=[ps])
                    eng = "act" if kc % 2 == 0 else "dve"
                    if eng == "act":
                        k.I("act", lambda e, ps=ps, kc=kc, o=o: e.copy(out=o[:, kc, 0:n], in_=ps[:, 0:n]), R=[ps], W=[o])
                    else:
                        k.I("dve", lambda e, ps=ps, kc=kc, o=o: e.tensor_copy(out=o[:, kc, 0:n], in_=ps[:, 0:n]), R=[ps], W=[o])
                k.store(XT[:, :, t0:t0 + n], o[:, :, 0:n], R=[o])
            k.barrier()

    def modulate(self, st_tiles, x, n, li, A, shw, isctx, out_bf=None, out_f32=None, psb=None):
        k = self.k
        sq, rstd, tmp = st_tiles
        ps = psb
        for kc in range(KC):
            k.I("act", lambda e, kc=kc: e.activation(out=sq[:, 0:n], in_=x[:, kc, 0:n], func=AF.Square), R=[x], W=[sq])
            k.I("pe", lambda e, kc=kc: e.matmul(ps[:, 0:n], lhsT=self.ones[:], rhs=sq[:, 0:n],
                                                  start=(kc == 0), stop=(kc == KC - 1)), R=[sq, self.ones], W=[ps])
        k.I("act", lambda e: e.activation(out=rstd[:, 0:n], in_=ps[:, 0:n], func=AF.Sqrt, bias=self.epsb[:],
                                          scale=1.0 / D), R=[ps, self.epsb], W=[rstd])
        k.I("dve", lambda e: e.reciprocal(out=rstd[:, 0:n], in_=rstd[:, 0:n]), R=[rstd], W=[rstd])
        tcol = 1 if isctx else 0
        for kc in range(KC):
            k.I("dve", lambda e, kc=kc: e.tensor_tensor(out=tmp[:, 0:n], in0=x[:, kc, 0:n], in1=rstd[:, 0:n],
                                                        op=ALU.mult), R=[x, rstd], W=[tmp])
            Ac = A[:, li, kc, tcol:tcol + 1]
            sh = self.mv(li, shw, kc, isctx)
            if out_f32 is not None:
                k.I("act", lambda e, kc=kc, Ac=Ac, sh=sh: e.activation(out=out_f32[:, kc, 0:n], in_=tmp[:, 0:n],
                    func=AF.Identity, bias=sh, scale=Ac), R=[tmp, A, self.modT], W=[out_f32])
                k.I("pool", lambda e, kc=kc: e.tensor_copy(out=out_bf[:, kc, 0:n], in_=out_f32[:, kc, 0:n]),
                    R=[out_f32], W=[out_bf])
            else:
                k.I("act", lambda e, kc=kc, Ac=Ac, sh=sh: e.activation(out=out_bf[:, kc, 0:n], in_=tmp[:, 0:n],
                    func=AF.Identity, bias=sh, scale=Ac), R=[tmp, A, self.modT], W=[out_bf])

    def phase_post(self, li, j, w_o, router=False):
        k = self.k
        XT = self.scr["XT"].rearrange("(k p) t -> p k t", p=128)
        ST = self.scr["ST"].rearrange("(k p) t -> p k t", p=128)
        FT = self.scr["FT"].rearrange("(k p) t -> p k t", p=128)
        with contextlib.ExitStack() as st:
            wo = k.sb(st, "wo", [128, 8, D], BF16)
            self.wait_cvt("w_out" if router else "w_o", j)
            k.load(wo[:], w_o.rearrange("(k p) c -> p k c", p=128), W=[wo])
            s_in = [k.sb(st, "s_in%d" % i, [128, 8, 512], BF16) for i in range(2)]
            xt = [k.sb(st, "xt%d" % i, [128, 8, 512], F32) for i in range(2)]
            fb = [k.sb(st, "fb%d" % i, [128, 8, 512], BF16) for i in range(2)]
            sq = k.sb(st, "sq", [128, 512], F32)
            rstd = k.sb(st, "rstd", [128, 512], F32)
            tmp = k.sb(st, "tmp", [128, 512], F32)
            if router:
                f32 = [k.sb(st, "f32_%d" % i, [128, 8, 512], F32) for i in range(2)]
                wr = k.sb(st, "wr", [128, 8, NE], F32)
                k.load(wr[:], self.inp["moe_router_w"][j].rearrange("(k p) e -> p k e", p=128), W=[wr])
                lg = k.sb(st, "lg", [128, 8], F32)
                m8 = k.sb(st, "m8", [128, 8], F32)
                sm = k.sb(st, "sm", [128, 8], F32)
                eq1 = k.sb(st, "eq1", [128, 8], F32)
                eq2 = k.sb(st, "eq2", [128, 8], F32)
                gt = k.sb(st, "gt", [128, 8], F32)
                gT = [k.sb(st, "gT%d" % i, [8, 512], F32) for i in range(2)]
            for bi, (t0, n, isctx) in enumerate(self.blocks):
                if isctx and li == 3:
                    continue
                si, x, f = s_in[bi % 2], xt[bi % 2], fb[bi % 2]
                k.load(si[:, :, 0:n], ST[:, :, t0:t0 + n], W=[si])
                k.load(x[:, :, 0:n], XT[:, :, t0:t0 + n], W=[x])
                for dc in range(KC):
                    ps = self.psum[dc % 4]
                    for h in range(KC):
                        k.I("pe", lambda e, ps=ps, h=h, dc=dc: e.matmul(
                            ps[:, 0:n], lhsT=wo[:, h, dc * 128:(dc + 1) * 128], rhs=si[:, h, 0:n],
                            start=(h == 0), stop=(h == KC - 1)), R=[wo, si], W=[ps])
                    g1 = self.mv(li, 2, dc, isctx)
                    k.I("dve", lambda e, ps=ps, dc=dc, g1=g1: e.scalar_tensor_tensor(
                        out=x[:, dc, 0:n], in0=ps[:, 0:n], scalar=g1, in1=x[:, dc, 0:n], op0=ALU.mult, op1=ALU.add),
                        R=[ps, x, self.modT], W=[x])
                k.store(XT[:, :, t0:t0 + n], x[:, :, 0:n], R=[x])
                if router:
                    ff = f32[bi % 2]
                    self.modulate((sq, rstd, tmp), x, n, li, self.A2, 3, isctx, out_bf=f, out_f32=ff, psb=self.psum[4])
                else:
                    self.modulate((sq, rstd, tmp), x, n, li, self.A2, 3, isctx, out_bf=f, psb=self.psum[4])
                k.store(FT[:, :, t0:t0 + n], f[:, :, 0:n], R=[f])
                if router:
                    gTb = gT[bi % 2]
                    for jt in range(n // 128):
                        psl = self.psum[5]
                        for kc in range(KC):
                            k.I("pe", lambda e, kc=kc, jt=jt: e.matmul(
                                psl[:, 0:NE], lhsT=ff[:, kc, jt * 128:(jt + 1) * 128], rhs=wr[:, kc, :],
                                start=(kc == 0), stop=(kc == KC - 1)), R=[ff, wr], W=[psl])
                        k.I("dve", lambda e: e.tensor_copy(out=lg[:], in_=psl[:, 0:NE]), R=[psl], W=[lg])
                        k.I("dve", lambda e: e.max(out=m8[:], in_=lg[:]), R=[lg], W=[m8])
                        k.I("dve", lambda e: e.tensor_tensor(out=sm[:, 0:1], in0=m8[:, 1:2], in1=m8[:, 0:1], op=ALU.subtract),
                            R=[m8], W=[sm])
                        k.I("act", lambda e: e.activation(out=sm[:, 1:2], in_=sm[:, 0:1], func=AF.Exp), R=[sm], W=[sm])
                        k.I("dve", lambda e: e.tensor_scalar_add(out=sm[:, 2:3], in0=sm[:, 1:2], scalar1=1.0), R=[sm], W=[sm])
                        k.I("dve", lambda e: e.reciprocal(out=sm[:, 3:4], in_=sm[:, 2:3]), R=[sm], W=[sm])
                        k.I("dve", lambda e: e.tensor_tensor(out=sm[:, 4:5], in0=sm[:, 1:2], in1=sm[:, 3:4], op=ALU.mult),
                            R=[sm], W=[sm])
                        k.I("dve", lambda e: e.tensor_scalar(out=eq1[:], in0=lg[:], scalar1=m8[:, 0:1], scalar2=sm[:, 3:4],
                                                             op0=ALU.is_equal, op1=ALU.mult), R=[lg, m8, sm], W=[eq1])
                        k.I("dve", lambda e: e.tensor_scalar(out=eq2[:], in0=lg[:], scalar1=m8[:, 1:2], scalar2=sm[:, 4:5],
                                                             op0=ALU.is_equal, op1=ALU.mult), R=[lg, m8, sm], W=[eq2])
                        k.I("dve", lambda e: e.tensor_tensor(out=gt[:], in0=eq1[:], in1=eq2[:], op=ALU.add),
                            R=[eq1, eq2], W=[gt])
                        pst = self.psum[6]
                        k.I("pe", lambda e: e.transpose(pst[0:8, 0:128], gt[:], self.ident[:]), R=[gt, self.ident], W=[pst])
                        k.I("act", lambda e, jt=jt: e.copy(out=gTb[:, jt * 128:(jt + 1) * 128], in_=pst[0:8, 0:128]),
                            R=[pst], W=[gTb])
                    k.store(self.scr["G"][:, t0:t0 + n], gTb[:, 0:n], R=[gTb])
            k.barrier()

    def phase_ffn(self, li, experts, F, GC, gated):
        k = self.k
        XT = self.scr["XT"].rearrange("(k p) t -> p k t", p=128)
        FT = self.scr["FT"].rearrange("(k p) t -> p k t", p=128)
        ngroups = F // (GC * 128)
        assert ngroups * GC * 128 == F
        NSB = max(sum(b[1] for b in sb) for sb in self.superblocks)
        with contextlib.ExitStack() as st:
            jj = li // 2
            for nm in (("w_mgu", "w_md") if gated else ("w_fgu", "w_fd")):
                self.wait_cvt(nm, jj)
            fT = k.sb(st, "fT", [128, 8, NSB], BF16)
            yacc = k.sb(st, "yacc", [128, 8, NSB], F32)
            wg = [k.sb(st, "wg%d" % i, [128, 8, GC * 128], BF16) for i in range(2)]
            wu = [k.sb(st, "wu%d" % i, [128, 8, GC * 128], BF16) for i in range(2)]
            wd = [k.sb(st, "wd%d" % i, [128, GC, D], BF16) for i in range(2)]
            act = [k.sb(st, "act%d" % i, [128, GC, 512], BF16) for i in range(2)]
            sg = [k.sb(st, "sg%d" % i, [128, 512], F32) for i in range(2)]
            tg = [k.sb(st, "tg%d" % i, [128, 512], F32) for i in range(2)]
            gB = [k.sb(st, "gB%d" % i, [128, NSB], F32) for i in range(2)] if gated else None
            xt = [k.sb(st, "xt%d" % i, [128, 8, 512], F32) for i in range(2)]
            nw = 0
            na = 0
            for sb in self.superblocks:
                s0 = sb[0][0]
                ns = sum(b[1] for b in sb)
                k.load(fT[:, :, 0:ns], FT[:, :, s0:s0 + ns], W=[fT])
                first = True
                pend = None

                def emit_down(ac, c_, o, n, is_first):
                    for dc in range(KC):
                        py = self.psum[4 + dc % 4]
                        for c in range(GC):
                            k.I("pe", lambda e, py=py, c=c, dc=dc: e.matmul(
                                py[:, 0:n], lhsT=c_[:, c, dc * 128:(dc + 1) * 128], rhs=ac[:, c, 0:n],
                                start=(c == 0), stop=(c == GC - 1)), R=[c_, ac], W=[py])
                        if is_first:
                            k.I("dve", lambda e, py=py, dc=dc: e.tensor_copy(out=yacc[:, dc, o:o + n], in_=py[:, 0:n]),
                                R=[py], W=[yacc])
                        else:
                            k.I("dve", lambda e, py=py, dc=dc: e.tensor_tensor(
                                out=yacc[:, dc, o:o + n], in0=py[:, 0:n], in1=yacc[:, dc, o:o + n], op=ALU.add),
                                R=[py, yacc], W=[yacc])

                for ei, (wgu_d, wd_d) in enumerate(experts):
                    wgu_v = wgu_d.rearrange("(k p) c -> p k c", p=128)
                    wd_v = wd_d.rearrange("(c p) d -> p c d", p=128)
                    if gated:
                        gb = gB[ei % 2]
                        k.load(gb[:, 0:ns], self.scr["G"][ei:ei + 1, s0:s0 + ns].partition_broadcast(128), W=[gb])
                    for gi in range(ngroups):
                        a, b_, c_ = wg[nw % 2], wu[nw % 2], wd[nw % 2]
                        nw += 1
                        c0 = gi * GC * 128
                        k.load(a[:], wgu_v[:, :, c0:c0 + GC * 128], W=[a])
                        k.load(b_[:], wgu_v[:, :, F + c0:F + c0 + GC * 128], W=[b_])
                        k.load(c_[:], wd_v[:, gi * GC:(gi + 1) * GC, :], W=[c_])
                        for (t0, n, isctx) in sb:
                            if isctx and li == 3:
                                continue
                            o = t0 - s0
                            ac = act[na % 2]
                            na += 1
                            for c in range(GC):
                                pg = self.psum[(2 * c) % 4]
                                pu = self.psum[(2 * c + 1) % 4]
                                for kc in range(KC):
                                    k.I("pe", lambda e, pg=pg, c=c, kc=kc, a=a: e.matmul(
                                        pg[:, 0:n], lhsT=a[:, kc, c * 128:(c + 1) * 128], rhs=fT[:, kc, o:o + n],
                                        start=(kc == 0), stop=(kc == KC - 1)), R=[a, fT], W=[pg])
                                for kc in range(KC):
                                    k.I("pe", lambda e, pu=pu, c=c, kc=kc, b_=b_: e.matmul(
                                        pu[:, 0:n], lhsT=b_[:, kc, c * 128:(c + 1) * 128], rhs=fT[:, kc, o:o + n],
                                        start=(kc == 0), stop=(kc == KC - 1)), R=[b_, fT], W=[pu])
                                s_ = sg[c % 2]
                                k.I("act", lambda e, s_=s_, pg=pg: e.activation(out=s_[:, 0:n], in_=pg[:, 0:n], func=AF.Silu),
                                    R=[pg], W=[s_])
                                if gated:
                                    t_ = tg[c % 2]
                                    k.I("dve", lambda e, t_=t_, pu=pu, gb=gb: e.tensor_tensor(
                                        out=t_[:, 0:n], in0=pu[:, 0:n], in1=gb[:, o:o + n], op=ALU.mult), R=[pu, gb], W=[t_])
                                    k.I("pool", lambda e, t_=t_, s_=s_, c=c, ac=ac: e.tensor_tensor(
                                        out=ac[:, c, 0:n], in0=s_[:, 0:n], in1=t_[:, 0:n], op=ALU.mult), R=[s_, t_], W=[ac])
                                else:
                                    k.I("dve", lambda e, pu=pu, s_=s_, c=c, ac=ac: e.tensor_tensor(
                                        out=ac[:, c, 0:n], in0=pu[:, 0:n], in1=s_[:, 0:n], op=ALU.mult), R=[pu, s_], W=[ac])
                            if pend is not None:
                                emit_down(*pend)
                            pend = (ac, c_, o, n, first and gi == 0)
                    first = False
                if pend is not None:
                    emit_down(*pend)
                    pend = None
                for bi, (t0, n, isctx) in enumerate(sb):
                    if isctx and li == 3:
                        continue
                    o = t0 - s0
                    x = xt[bi % 2]
                    k.load(x[:, :, 0:n], XT[:, :, t0:t0 + n], W=[x])
                    for dc in range(KC):
                        g2 = self.mv(li, 5, dc, isctx)
                        k.I("dve", lambda e, dc=dc, g2=g2, x=x: e.scalar_tensor_tensor(
                            out=x[:, dc, 0:n], in0=yacc[:, dc, o:o + n], scalar=g2, in1=x[:, dc, 0:n],
                            op0=ALU.mult, op1=ALU.add), R=[yacc, x, self.modT], W=[x])
                    k.store(XT[:, :, t0:t0 + n], x[:, :, 0:n], R=[x])
            k.barrier()

    def phase_final(self):
        k = self.k
        XT = self.scr["XT"].rearrange("(k p) t -> p k t", p=128)
        with contextlib.ExitStack() as st:
            gf = k.sb(st, "gf", [128, 8], F32)
            k.load(gf[:], self.inp["final_norm_g"].rearrange("(k p) -> p k", p=128), W=[gf], allow_slow_non_contiguous=True)
            xt = [k.sb(st, "xt%d" % i, [128, 8, 512], F32) for i in range(2)]
            yo = [k.sb(st, "yo%d" % i, [128, 4, D], F32) for i in range(2)]
            sq = k.sb(st, "sq", [128, 512], F32)
            rstd = k.sb(st, "rstd", [128, 512], F32)
            for bi, (t0, n, isctx) in enumerate(self.blocks):
                if isctx:
                    continue
                x, y = xt[bi % 2], yo[bi % 2]
                k.load(x[:, :, 0:n], XT[:, :, t0:t0 + n], W=[x])
                ps = self.psum[0]
                for kc in range(KC):
                    k.I("act", lambda e, kc=kc: e.activation(out=sq[:, 0:n], in_=x[:, kc, 0:n], func=AF.Square), R=[x], W=[sq])
                    k.I("pe", lambda e, kc=kc: e.matmul(ps[:, 0:n], lhsT=self.ones[:], rhs=sq[:, 0:n],
                                                          start=(kc == 0), stop=(kc == KC - 1)), R=[sq, self.ones], W=[ps])
                k.I("act", lambda e: e.activation(out=rstd[:, 0:n], in_=ps[:, 0:n], func=AF.Sqrt, bias=self.epsb[:],
                                                  scale=1.0 / D), R=[ps, self.epsb], W=[rstd])
                k.I("dve", lambda e: e.reciprocal(out=rstd[:, 0:n], in_=rstd[:, 0:n]), R=[rstd], W=[rstd])
                for kc in range(KC):
                    k.I("dve", lambda e, kc=kc: e.scalar_tensor_tensor(
                        out=x[:, kc, 0:n], in0=x[:, kc, 0:n], scalar=gf[:, kc:kc + 1], in1=rstd[:, 0:n],
                        op0=ALU.mult, op1=ALU.mult), R=[x, gf, rstd], W=[x])
                for jt in range(n // 128):
                    for half in range(2):
                        pt = self.psum[1 + (jt * 2 + half) % 4]
                        for q in range(4):
                            kc = half * 4 + q
                            k.I("pe", lambda e, pt=pt, q=q, kc=kc, jt=jt: e.transpose(
                                pt[:, q * 128:(q + 1) * 128], x[:, kc, jt * 128:(jt + 1) * 128], self.ident[:]),
                                R=[x, self.ident], W=[pt])
                        if half == 0:
                            k.I("act", lambda e, pt=pt, jt=jt: e.copy(out=y[:, jt, 0:512], in_=pt[:, 0:512]), R=[pt], W=[y])
                        else:
                            k.I("dve", lambda e, pt=pt, jt=jt: e.tensor_copy(out=y[:, jt, 512:1024], in_=pt[:, 0:512]), R=[pt], W=[y])
                k.store(self.out[t0 - CTX:t0 - CTX + n, :].rearrange("(j p) d -> p j d", p=128), y[:, 0:n // 128, :], R=[y])
            k.barrier()

    def rope_tables(self, st, t0, n, T_):
        k = self.k
        rowf, colf, pos, u, ki, kf, cosT, sinT, rc = T_
        base_r = float((t0 - CTX) // 64)
        k.I("dve", lambda e: e.tensor_scalar(out=pos[:, 0:n], in0=rowf[:, 0:n], scalar1=base_r, scalar2=rc[:, 0:1],
                                             op0=ALU.add, op1=ALU.mult), R=[rowf, rc], W=[pos])
        k.I("dve", lambda e: e.scalar_tensor_tensor(out=pos[:, 0:n], in0=colf[:, 0:n], scalar=rc[:, 1:2], in1=pos[:, 0:n],
                                                    op0=ALU.mult, op1=ALU.add), R=[colf, rc, pos], W=[pos])
        for (off, outT, sc_col, bi_col) in ((0.5, sinT, 3, 4), (0.75, cosT, 5, 6)):
            k.I("dve", lambda e, off=off: e.tensor_scalar(out=u[:, 0:n], in0=pos[:, 0:n], scalar1=rc[:, 2:3], scalar2=off,
                                                          op0=ALU.mult, op1=ALU.add), R=[pos, rc], W=[u])
            k.I("dve", lambda e: e.tensor_copy(out=ki[:, 0:n], in_=u[:, 0:n]), R=[u], W=[ki])
            k.I("dve", lambda e: e.tensor_copy(out=kf[:, 0:n], in_=ki[:, 0:n]), R=[ki], W=[kf])
            k.I("dve", lambda e: e.tensor_tensor(out=u[:, 0:n], in0=u[:, 0:n], in1=kf[:, 0:n], op=ALU.subtract),
                R=[u, kf], W=[u])
            k.I("dve", lambda e: e.tensor_single_scalar(out=kf[:, 0:n], in_=u[:, 0:n], scalar=0.0, op=ALU.is_lt),
                R=[u], W=[kf])
            k.I("dve", lambda e: e.tensor_tensor(out=u[:, 0:n], in0=u[:, 0:n], in1=kf[:, 0:n], op=ALU.add),
                R=[u, kf], W=[u])
            k.I("act", lambda e, outT=outT, sc_col=sc_col, bi_col=bi_col: e.activation(
                out=outT[:, 0:n], in_=u[:, 0:n], func=AF.Sin, bias=rc[:, bi_col:bi_col + 1],
                scale=rc[:, sc_col:sc_col + 1]), R=[u, rc], W=[outT])

    def phase_att1(self, li, j):
        k = self.k
        XT = self.scr["XT"].rearrange("(k p) t -> p k t", p=128)
        QT = self.scr["QT"].rearrange("(k p) t -> p k t", p=128)
        KT = self.scr["KT"].rearrange("(k p) t -> p k t", p=128)
        with contextlib.ExitStack() as st:
            wq = k.sb(st, "wq", [128, 8, 3 * D], BF16)
            self.wait_cvt("w_qkv", j)
            wv = self.scr["w_qkv"][j].rearrange("(k p) c -> p k c", p=128)
            for kc in range(KC):
                k.load(wq[:, kc, :], wv[:, kc, :], W=[wq], add=True)
            rc = k.sb(st, "rc", [128, 8], F32)
            k.load(rc[:], self.inp["ropec"][:, :], W=[rc])
            pf = k.sb(st, "pf", [128, 128], F32)
            k.load(pf[:], self.inp["perm"][:, :], W=[pf])
            pb = k.sb(st, "pb", [128, 128], BF16)
            k.I("dve", lambda e: e.tensor_copy(out=pb[:], in_=pf[:]), R=[pf], W=[pb])
            ri = k.sb(st, "ri", [128, 8, 64], I32)
            ci = k.sb(st, "ci", [128, 8, 64], I32)
            rowf = k.sb(st, "rowf", [128, 512], F32)
            colf = k.sb(st, "colf", [128, 512], F32)
            k.I("pool", lambda e: e.iota(ri[:], pattern=[[1, 8], [0, 64]], base=0, channel_multiplier=0), W=[ri])
            k.I("pool", lambda e: e.iota(ci[:], pattern=[[0, 8], [1, 64]], base=0, channel_multiplier=0), W=[ci])
            k.I("dve", lambda e: e.tensor_copy(out=rowf[:], in_=ri[:].rearrange("p a b -> p (a b)")), R=[ri], W=[rowf])
            k.I("dve", lambda e: e.tensor_copy(out=colf[:], in_=ci[:].rearrange("p a b -> p (a b)")), R=[ci], W=[colf])
            pos = k.sb(st, "pos", [128, 512], F32)
            u = k.sb(st, "u", [128, 512], F32)
            ki = k.sb(st, "ki", [128, 512], I32)
            kf = k.sb(st, "kf", [128, 512], F32)
            cosT = k.sb(st, "cosT", [128, 512], F32)
            sinT = k.sb(st, "sinT", [128, 512], F32)
            xt = [k.sb(st, "xt%d" % i, [128, 8, 512], F32) for i in range(2)]
            hb = [k.sb(st, "hb%d" % i, [128, 8, 512], BF16) for i in range(2)]
            qo = [k.sb(st, "qo%d" % i, [128, 8, 512], BF16) for i in range(2)]
            ko = [k.sb(st, "ko%d" % i, [128, 8, 512], BF16) for i in range(2)]
            vb = [k.sb(st, "vb%d" % i, [128, 4, D], BF16) for i in range(2)]
            qb = [k.sb(st, "qb%d" % i, [128, 512], BF16) for i in range(2)]
            t1 = [k.sb(st, "t1_%d" % i, [128, 512], F32) for i in range(2)]
            t2 = [k.sb(st, "t2_%d" % i, [128, 512], F32) for i in range(2)]
            sq = k.sb(st, "sq", [128, 512], F32)
            rstd = k.sb(st, "rstd", [128, 512], F32)
            tmp = k.sb(st, "tmp", [128, 512], F32)
            nq = 0
            for bi, (t0, n, isctx) in enumerate(self.blocks):
                x, h, qq, kk, v = xt[bi % 2], hb[bi % 2], qo[bi % 2], ko[bi % 2], vb[bi % 2]
                k.load(x[:, :, 0:n], XT[:, :, t0:t0 + n], W=[x])
                self.modulate((sq, rstd, tmp), x, n, li, self.A1, 0, isctx, out_bf=h, psb=self.psum[7])
                if not isctx:
                    self.rope_tables(st, t0, n, (rowf, colf, pos, u, ki, kf, cosT, sinT, rc))
                for hd in range(NH):
                    for (isq, col0, dst) in ((True, hd * 128, qq), (False, D + hd * 128, kk)):
                        ps = self.psum[nq % 3]
                        pr = self.psum[3 + nq % 3]
                        for kc in range(KC):
                            k.I("pe", lambda e, ps=ps, kc=kc, col0=col0: e.matmul(
                                ps[:, 0:n], lhsT=wq[:, kc, col0:col0 + 128], rhs=h[:, kc, 0:n],
                                start=(kc == 0), stop=(kc == KC - 1)), R=[wq, h], W=[ps])
                        scl = 0.125 if isq else 1.0
                        if isctx:
                            k.I("act", lambda e, ps=ps, dst=dst, hd=hd, scl=scl: e.activation(
                                out=dst[:, hd, 0:n], in_=ps[:, 0:n], func=AF.Copy, scale=scl), R=[ps], W=[dst])
                        else:
                            b_ = qb[nq % 2]
                            a1, a2 = t1[nq % 2], t2[nq % 2]
                            k.I("act", lambda e, ps=ps, b_=b_, scl=scl: e.activation(
                                out=b_[:, 0:n], in_=ps[:, 0:n], func=AF.Copy, scale=scl), R=[ps], W=[b_])
                            k.I("pe", lambda e, pr=pr, b_=b_: e.matmul(pr[:, 0:n], lhsT=pb[:], rhs=b_[:, 0:n],
                                                                       start=True, stop=True), R=[pb, b_], W=[pr])
                            k.I("dve", lambda e, a1=a1, b_=b_: e.tensor_tensor(out=a1[:, 0:n], in0=b_[:, 0:n], in1=cosT[:, 0:n],
                                                                               op=ALU.mult), R=[b_, cosT], W=[a1])
                            k.I("dve", lambda e, a2=a2, pr=pr: e.tensor_tensor(out=a2[:, 0:n], in0=pr[:, 0:n], in1=sinT[:, 0:n],
                                                                               op=ALU.mult), R=[pr, sinT], W=[a2])
                            k.I("pool", lambda e, a1=a1, a2=a2, dst=dst, hd=hd: e.tensor_tensor(
                                out=dst[:, hd, 0:n], in0=a1[:, 0:n], in1=a2[:, 0:n], op=ALU.add), R=[a1, a2], W=[dst])
                        nq += 1
                k.store(QT[:, :, t0:t0 + n], qq[:, :, 0:n], R=[qq])
                k.store(KT[:, :, t0:t0 + n], kk[:, :, 0:n], R=[kk])
                for jt in range(n // 128):
                    for half in range(2):
                        ps = self.psum[6 + half]
                        for kc in range(KC):
                            k.I("pe", lambda e, ps=ps, kc=kc, jt=jt, half=half: e.matmul(
                                ps[:, 0:512], lhsT=h[:, kc, jt * 128:(jt + 1) * 128],
                                rhs=wq[:, kc, 2 * D + half * 512:2 * D + (half + 1) * 512],
                                start=(kc == 0), stop=(kc == KC - 1)), R=[wq, h], W=[ps])
                        if half == 0:
                            k.I("act", lambda e, ps=ps, jt=jt: e.copy(out=v[:, jt, 0:512], in_=ps[:, 0:512]), R=[ps], W=[v])
                        else:
                            k.I("dve", lambda e, ps=ps, jt=jt: e.tensor_copy(out=v[:, jt, 512:1024], in_=ps[:, 0:512]),
                                R=[ps], W=[v])
                k.store(self.scr["V"][t0:t0 + n, :].rearrange("(j p) d -> p j d", p=128), v[:, 0:n // 128, :], R=[v])
            k.barrier()

    def phase_att2(self, li, j):
        k = self.k
        T = self.T
        NT = T // 128
        lambda_init = 0.8 - 0.6 * math.exp(-0.3 * li)
        with contextlib.ExitStack() as st:
            lv = k.sb(st, "lv", [128, 256], F32)
            k.load(lv[:], self.inp["attn_lambda"][j:j + 1, :].partition_broadcast(128), W=[lv])
            lp = k.sb(st, "lp", [128, 128], F32)
            ls = k.sb(st, "ls", [128, 4], F32)
            k.I("dve", lambda e: e.tensor_tensor(out=lp[:, 0:64], in0=lv[:, 0:64], in1=lv[:, 64:128], op=ALU.mult), R=[lv], W=[lp])
            k.I("dve", lambda e: e.tensor_tensor(out=lp[:, 64:128], in0=lv[:, 128:192], in1=lv[:, 192:256], op=ALU.mult),
                R=[lv], W=[lp])
            k.I("dve", lambda e: e.reduce_sum(out=ls[:, 0:1], in_=lp[:, 0:64], axis=AX.X), R=[lp], W=[ls])
            k.I("dve", lambda e: e.reduce_sum(out=ls[:, 1:2], in_=lp[:, 64:128], axis=AX.X), R=[lp], W=[ls])
            k.I("act", lambda e: e.activation(out=ls[:, 0:2], in_=ls[:, 0:2], func=AF.Exp), R=[ls], W=[ls])
            k.I("dve", lambda e: e.tensor_tensor(out=ls[:, 2:3], in0=ls[:, 1:2], in1=ls[:, 0:1], op=ALU.subtract), R=[ls], W=[ls])
            k.I("dve", lambda e: e.tensor_scalar_add(out=ls[:, 2:3], in0=ls[:, 2:3], scalar1=-lambda_init), R=[ls], W=[ls])
            sgl = k.sb(st, "sgl", [128, 1], F32)
            k.load(sgl[:], self.inp["attn_subln_g"][j].rearrange("(p o) -> p o", o=1), W=[sgl])
            k.I("dve", lambda e: e.tensor_scalar_mul(out=sgl[:], in0=sgl[:], scalar1=1.0 - lambda_init), R=[sgl], W=[sgl])
            onesb = k.sb(st, "onesb", [128, 128], BF16)
            k.I("dve", lambda e: e.tensor_copy(out=onesb[:], in_=self.ones[:]), R=[self.ones], W=[onesb])
            kT = [k.sb(st, "kT%d" % i, [128, T], BF16) for i in range(2)]
            vh = [k.sb(st, "vh%d" % i, [128, NT, 128], BF16) for i in range(2)]
            qt = [k.sb(st, "qt%d" % i, [128, 512], BF16) for i in range(2)]
            ep = [k.sb(st, "ep%d" % i, [128, 2, 512], BF16) for i in range(4)]
            gs0 = [k.sb(st, "gs0_%d" % i, [128, 512], BF16) for i in range(2)]
            gs1 = [k.sb(st, "gs1_%d" % i, [128, 512], BF16) for i in range(2)]
            es0 = k.sb(st, "es0", [128, 512], F32)
            es1 = k.sb(st, "es1", [128, 512], F32)
            r0 = k.sb(st, "r0", [128, 512], F32)
            r1 = k.sb(st, "r1", [128, 512], F32)
            o0 = k.sb(st, "o0", [128, 512], F32)
            o1 = k.sb(st, "o1", [128, 512], F32)
            sq = k.sb(st, "sq", [128, 512], F32)
            ob = [k.sb(st, "ob%d" % i, [128, 512], BF16) for i in range(2)]
            acc0, acc1 = self.psum[6], self.psum[7]
            Zp = self.psum[4]
            Zq = self.psum[5]
            bg = self.cvt_chunks(self.pending_cvt) if self.pending_cvt else []
            self.pending_cvt = []
            GRP = 6
            nb = 0
            ne = 0
            ng = 0
            Vv = self.scr["V"].rearrange("(kt p) d -> p kt d", p=128)
            for hd in range(NH):
                kt_, v_ = kT[hd % 2], vh[hd % 2]
                k.load(kt_[:], self.scr["KT"][hd * 128:(hd + 1) * 128, :], W=[kt_])
                k.load(v_[:], Vv[:, :, hd * 128:(hd + 1) * 128], W=[v_])
                for (t0, n, isctx) in self.blocks:
                    q = qt[nb % 2]
                    o_ = ob[nb % 2]
                    nb += 1
                    k.load(q[:, 0:n], self.scr["QT"][hd * 128:(hd + 1) * 128, t0:t0 + n], W=[q])
                    if bg and not isctx:
                        self.issue_chunk(bg.pop(0))
                    tiles = list(range(2)) if isctx else list(range(NT))
                    ngrp_done = 0

                    def scores(pi, kt):
                        s0, s1 = self.psum[2 * pi], self.psum[2 * pi + 1]
                        k.I("pe", lambda e: e.matmul(s0[:, 0:n], lhsT=kt_[0:64, kt * 128:(kt + 1) * 128],
                                                     rhs=q[0:64, 0:n], start=True, stop=True), R=[kt_, q], W=[s0])
                        k.I("pe", lambda e: e.matmul(s1[:, 0:n], lhsT=kt_[64:128, kt * 128:(kt + 1) * 128],
                                                     rhs=q[64:128, 0:n], start=True, stop=True), R=[kt_, q], W=[s1])

                    scores(ne % 2, tiles[0])
                    for ti, kt in enumerate(tiles):
                        pi = ne % 2
                        xp = ep[ne % 4]
                        ne += 1
                        first, last = (ti == 0), (ti == len(tiles) - 1)
                        if not last:
                            scores(ne % 2, tiles[ti + 1])
                        s0, s1 = self.psum[2 * pi], self.psum[2 * pi + 1]
                        k.I("act", lambda e, pi=pi, xp=xp: e.activation(out=xp[:, :, 0:n], in_=self.pspair[pi][:, :, 0:n],
                                                                        func=AF.Exp), R=[s0, s1], W=[xp])
                        for (acc, mi) in ((acc0, 0), (acc1, 1)):
                            k.I("pe", lambda e, acc=acc, mi=mi, xp=xp, kt=kt: e.matmul(
                                acc[:, 0:n], lhsT=v_[:, kt, :], rhs=xp[:, mi, 0:n], start=first, stop=last), R=[v_, xp], W=[acc])
                        k.I("pe", lambda e, xp=xp: e.matmul(Zp[:, 0:n], lhsT=onesb[:], rhs=xp[:, 0, 0:n], start=first, stop=last),
                            R=[onesb, xp], W=[Zp])
                        k.I("pe", lambda e, xp=xp: e.matmul(Zq[:, 0:n], lhsT=onesb[:], rhs=xp[:, 1, 0:n], start=first, stop=last),
                            R=[onesb, xp], W=[Zq])
                        continue
                        gi = ti % GRP
                        g0, g1 = gs0[ng % 2], gs1[ng % 2]
                        if gi == 0:
                            pp = xp
                        elif gi == 1:
                            k.I("pool", lambda e, g1=g1, pp=pp, xp=xp: e.tensor_tensor(out=g1[:, 0:n], in0=pp[:, 1, 0:n], in1=xp[:, 1, 0:n],
                                                                                       op=ALU.add), R=[pp, xp], W=[g1])
                        else:
                            k.I("pool", lambda e, g1=g1, xp=xp: e.tensor_tensor(out=g1[:, 0:n], in0=g1[:, 0:n], in1=xp[:, 1, 0:n],
                                                                                op=ALU.add), R=[g1, xp], W=[g1])
                        if gi == GRP - 1 or last:
                            assert gi >= 1
                            for (es, g) in ((es1, g1),):
                                if ngrp_done == 0:
                                    k.I("dve", lambda e, es=es, g=g: e.tensor_copy(out=es[:, 0:n], in_=g[:, 0:n]), R=[g], W=[es])
                                else:
                                    k.I("dve", lambda e, es=es, g=g: e.tensor_tensor(out=es[:, 0:n], in0=g[:, 0:n], in1=es[:, 0:n],
                                                                                     op=ALU.add), R=[g, es], W=[es])
                            ngrp_done += 1
                            ng += 1
                    Z0 = Zp
                    Z1 = Zq
                    k.I("dve", lambda e: e.reciprocal(out=r0[:, 0:n], in_=Z0[:, 0:n]), R=[Z0], W=[r0])
                    k.I("dve", lambda e: e.reciprocal(out=r1[:, 0:n], in_=Z1[:, 0:n]), R=[Z1], W=[r1])
                    k.I("dve", lambda e: e.tensor_tensor(out=o0[:, 0:n], in0=acc0[:, 0:n], in1=r0[:, 0:n], op=ALU.mult),
                        R=[acc0, r0], W=[o0])
                    k.I("dve", lambda e: e.tensor_tensor(out=o1[:, 0:n], in0=acc1[:, 0:n], in1=r1[:, 0:n], op=ALU.mult),
                        R=[acc1, r1], W=[o1])
                    k.I("dve", lambda e: e.scalar_tensor_tensor(out=o0[:, 0:n], in0=o1[:, 0:n], scalar=ls[:, 2:3], in1=o0[:, 0:n],
                                                                op0=ALU.mult, op1=ALU.add), R=[o1, ls, o0], W=[o0])
                    k.I("act", lambda e: e.activation(out=sq[:, 0:n], in_=o0[:, 0:n], func=AF.Square), R=[o0], W=[sq])
                    pss = self.psum[(ne % 2) * 2]
                    k.I("pe", lambda e, pss=pss: e.matmul(pss[:, 0:n], lhsT=self.ones[:], rhs=sq[:, 0:n], start=True, stop=True),
                        R=[self.ones, sq], W=[pss])
                    k.I("act", lambda e, pss=pss: e.activation(out=r0[:, 0:n], in_=pss[:, 0:n], func=AF.Sqrt, bias=self.epsb[:],
                                                               scale=1.0 / 128.0), R=[pss, self.epsb], W=[r0])
                    k.I("dve", lambda e: e.reciprocal(out=r0[:, 0:n], in_=r0[:, 0:n]), R=[r0], W=[r0])
                    k.I("dve", lambda e, o_=o_: e.scalar_tensor_tensor(out=o_[:, 0:n], in0=o0[:, 0:n], scalar=sgl[:, 0:1],
                                                                       in1=r0[:, 0:n], op0=ALU.mult, op1=ALU.mult),
                        R=[o0, sgl, r0], W=[o_])
                    k.store(self.scr["ST"][hd * 128:(hd + 1) * 128, t0:t0 + n], o_[:, 0:n], R=[o_])
            while bg:
                self.issue_chunk(bg.pop(0))
            k.barrier()

    def phase_lru1(self, li, j):
        k = self.k
        XT = self.scr["XT"].rearrange("(k p) t -> p k t", p=128)
        GT = self.scr["GATE"].rearrange("(k p) t -> p k t", p=128)
        RT = self.scr["R"].rearrange("(k p) t -> p k t", p=128)
        with contextlib.ExitStack() as st:
            win = k.sb(st, "win", [128, 8, 2 * D], BF16)
            self.wait_cvt("w_in", j)
            wv = self.scr["w_in"][j].rearrange("(k p) c -> p k c", p=128)
            for kc in range(KC):
                k.load(win[:, kc, :], wv[:, kc, :], W=[win], add=True)
            xt = [k.sb(st, "xt%d" % i, [128, 8, 512], F32) for i in range(2)]
            hb = [k.sb(st, "hb%d" % i, [128, 8, 512], BF16) for i in range(2)]
            gt = [k.sb(st, "gt%d" % i, [128, 8, 512], BF16) for i in range(2)]
            rt = [k.sb(st, "rt%d" % i, [128, 8, 512], F32) for i in range(2)]
            g1 = [k.sb(st, "g1_%d" % i, [128, 512], F32) for i in range(2)]
            g2 = [k.sb(st, "g2_%d" % i, [128, 512], F32) for i in range(2)]
            sq = k.sb(st, "sq", [128, 512], F32)
            rstd = k.sb(st, "rstd", [128, 512], F32)
            tmp = k.sb(st, "tmp", [128, 512], F32)
            for bi, (t0, n, isctx) in enumerate(self.blocks):
                x, h, g_, r_ = xt[bi % 2], hb[bi % 2], gt[bi % 2], rt[bi % 2]
                k.load(x[:, :, 0:n], XT[:, :, t0:t0 + n], W=[x])
                self.modulate((sq, rstd, tmp), x, n, li, self.A1, 0, isctx, out_bf=h, psb=self.psum[7])
                for c in range(16):
                    ps = self.psum[c % 4]
                    for kc in range(KC):
                        k.I("pe", lambda e, ps=ps, kc=kc, c=c: e.matmul(
                            ps[:, 0:n], lhsT=win[:, kc, c * 128:(c + 1) * 128], rhs=h[:, kc, 0:n],
                            start=(kc == 0), stop=(kc == KC - 1)), R=[win, h], W=[ps])
                    if c < 8:
                        a, b_ = g1[c % 2], g2[c % 2]
                        k.I("act", lambda e, ps=ps, a=a: e.activation(out=a[:, 0:n], in_=ps[:, 0:n], func=AF.Square), R=[ps], W=[a])
                        k.I("dve", lambda e, a=a: e.tensor_scalar(out=a[:, 0:n], in0=a[:, 0:n], scalar1=0.044715, scalar2=1.0,
                                                                  op0=ALU.mult, op1=ALU.add), R=[a], W=[a])
                        k.I("dve", lambda e, a=a, ps=ps: e.tensor_tensor(out=a[:, 0:n], in0=ps[:, 0:n], in1=a[:, 0:n], op=ALU.mult),
                            R=[ps, a], W=[a])
                        k.I("act", lambda e, a=a, b_=b_: e.activation(out=b_[:, 0:n], in_=a[:, 0:n], func=AF.Sigmoid,
                                                                      scale=1.5957691216057308), R=[a], W=[b_])
                        k.I("dve", lambda e, b_=b_, ps=ps, c=c: e.tensor_tensor(out=g_[:, c, 0:n], in0=ps[:, 0:n], in1=b_[:, 0:n],
                                                                                op=ALU.mult), R=[ps, b_], W=[g_])
                    else:
                        k.I("act", lambda e, ps=ps, c=c: e.copy(out=r_[:, c - 8, 0:n], in_=ps[:, 0:n]), R=[ps], W=[r_])
                k.store(GT[:, :, t0:t0 + n], g_[:, :, 0:n], R=[g_])
                k.store(RT[:, :, t0:t0 + n], r_[:, :, 0:n], R=[r_])
            k.barrier()

    def phase_lru2(self, li, j):
        k = self.k
        T = self.T
        segs = [(0, CTX), (CTX, T)]
        with contextlib.ExitStack() as st:
            Rb = k.sb(st, "Rb", [128, T], F32)
            U = k.sb(st, "U", [128, T], F32)
            A = k.sb(st, "A", [128, T], F32)
            B1 = k.sb(st, "B1", [128, T], F32)
            gw = k.sb(st, "gw", [128, 2, 2, 128], F32)
            gb = k.sb(st, "gb", [128, 4], F32)
            ap_ = k.sb(st, "ap", [128, 2], F32)
            sc8 = k.sb(st, "sc8", [128, 2], F32)
            cw = k.sb(st, "cw", [128, 4], F32)
            cb = k.sb(st, "cb", [128, 1], F32)
            rr = [k.sb(st, "rr%d" % i, [128, 512], F32) for i in range(2)]
            ii = [k.sb(st, "ii%d" % i, [128, 512], F32) for i in range(2)]
            s2 = [k.sb(st, "s2_%d" % i, [128, 512], F32) for i in range(2)]
            gl = [k.sb(st, "gl%d" % i, [128, 512], BF16) for i in range(2)]
            so = [k.sb(st, "so%d" % i, [128, 512], BF16) for i in range(2)]
            hs = [k.sb(st, "hs%d" % i, [128, 512], F32) for i in range(2)]
            nn = 0
            for kc in range(KC):
                sl = slice(kc * 128, (kc + 1) * 128)
                k.load(Rb[:], self.scr["R"][sl, :], W=[Rb])
                k.load(gw[:], self.inp["lru_gate_w"][j][:, :, kc].rearrange("d g c o -> c d g o"), W=[gw])
                k.load(gb[:], self.inp["lru_gate_b"][j][:, :, sl].rearrange("d g p -> p (d g)"), W=[gb],
                       allow_slow_non_contiguous=True)
                k.load(ap_[:], self.inp["lru_a_param"][j][:, sl].rearrange("d p -> p d"), W=[ap_], allow_slow_non_contiguous=True)
                k.load(cw[:], self.inp["lru_conv_w"][j][:, sl].rearrange("w p -> p w"), W=[cw], allow_slow_non_contiguous=True)
                k.load(cb[:], self.inp["lru_conv_b"][j][sl].rearrange("(p o) -> p o", o=1), W=[cb])
                k.I("act", lambda e: e.activation(out=sc8[:], in_=ap_[:], func=AF.Sigmoid), R=[ap_], W=[sc8])
                k.I("act", lambda e: e.activation(out=sc8[:], in_=sc8[:], func=AF.Ln), R=[sc8], W=[sc8])
                k.I("dve", lambda e: e.tensor_scalar_mul(out=sc8[:], in0=sc8[:], scalar1=8.0), R=[sc8], W=[sc8])
                k.I("dve", lambda e: e.tensor_scalar(out=U[:], in0=Rb[:], scalar1=cw[:, 2:3], scalar2=cb[:, 0:1],
                                                     op0=ALU.mult, op1=ALU.add), R=[Rb, cw, cb], W=[U])
                for (s_, e_) in segs:
                    for (tap, dlo, dhi, slo, shi) in ((0, s_ + 2, e_, s_, e_ - 2), (1, s_ + 1, e_, s_, e_ - 1),
                                                      (3, s_, e_ - 1, s_ + 1, e_)):
                        k.I("dve", lambda e, tap=tap, dlo=dlo, dhi=dhi, slo=slo, shi=shi: e.scalar_tensor_tensor(
                            out=U[:, dlo:dhi], in0=Rb[:, slo:shi], scalar=cw[:, tap:tap + 1], in1=U[:, dlo:dhi],
                            op0=ALU.mult, op1=ALU.add), R=[Rb, cw, U], W=[U])
                for d in range(2):
                    Bd = B1 if d == 0 else Rb
                    for (t0, n, isctx) in self.blocks:
                        pr, pi = self.psum[(2 * nn) % 4], self.psum[(2 * nn + 1) % 4]
                        r_, i_, q_ = rr[nn % 2], ii[nn % 2], s2[nn % 2]
                        nn += 1
                        k.I("pe", lambda e, pr=pr: e.matmul(pr[:, 0:n], lhsT=gw[:, d, 0, :], rhs=U[:, t0:t0 + n], start=True, stop=True),
                            R=[gw, U], W=[pr])
                        k.I("pe", lambda e, pi=pi: e.matmul(pi[:, 0:n], lhsT=gw[:, d, 1, :], rhs=U[:, t0:t0 + n], start=True, stop=True),
                            R=[gw, U], W=[pi])
                        k.I("act", lambda e, pr=pr, r_=r_: e.activation(out=r_[:, 0:n], in_=pr[:, 0:n], func=AF.Sigmoid,
                                                                        bias=gb[:, 2 * d:2 * d + 1]), R=[pr, gb], W=[r_])
                        k.I("act", lambda e, pi=pi, i_=i_: e.activation(out=i_[:, 0:n], in_=pi[:, 0:n], func=AF.Sigmoid,
                                                                        bias=gb[:, 2 * d + 1:2 * d + 2]), R=[pi, gb], W=[i_])
                        k.I("act", lambda e, r_=r_: e.activation(out=A[:, t0:t0 + n], in_=r_[:, 0:n], func=AF.Exp,
                                                                 scale=sc8[:, d:d + 1]), R=[r_, sc8], W=[A])
                        k.I("act", lambda e, q_=q_: e.activation(out=q_[:, 0:n], in_=A[:, t0:t0 + n], func=AF.Square), R=[A], W=[q_])
                        k.I("act", lambda e, q_=q_: e.activation(out=q_[:, 0:n], in_=q_[:, 0:n], func=AF.Sqrt, bias=self.ones[:, 0:1],
                                                                 scale=-1.0), R=[q_, self.ones], W=[q_])
                        k.I("dve", lambda e, i_=i_: e.tensor_tensor(out=i_[:, 0:n], in0=i_[:, 0:n], in1=U[:, t0:t0 + n], op=ALU.mult),
                            R=[i_, U], W=[i_])
                        k.I("dve", lambda e, i_=i_, q_=q_, Bd=Bd: e.tensor_tensor(out=Bd[:, t0:t0 + n], in0=i_[:, 0:n], in1=q_[:, 0:n],
                                                                                  op=ALU.mult), R=[i_, q_], W=[Bd])
                    if d == 0:
                        k.I("dve", lambda e, Bd=Bd: e.tensor_tensor_scan(out=Bd[:, 0:CTX], data0=A[:, 0:CTX], data1=Bd[:, 0:CTX],
                                                                         initial=0.0, op0=ALU.mult, op1=ALU.add), R=[A, Bd], W=[Bd])
                        k.I("dve", lambda e, Bd=Bd: e.tensor_tensor_scan(out=Bd[:, CTX:T], data0=A[:, CTX:T], data1=Bd[:, CTX:T],
                                                                         initial=Bd[:, CTX - 1:CTX], op0=ALU.mult, op1=ALU.add),
                            R=[A, Bd], W=[Bd])
                    else:
                        k.I("dve", lambda e, Bd=Bd: e.tensor_tensor_scan(
                            out=Bd[:, 0:CTX][:, ::-1], data0=A[:, 0:CTX][:, ::-1], data1=Bd[:, 0:CTX][:, ::-1],
                            initial=0.0, op0=ALU.mult, op1=ALU.add), R=[A, Bd], W=[Bd])
                        k.I("dve", lambda e, Bd=Bd: e.tensor_tensor_scan(
                            out=Bd[:, CTX:T][:, ::-1], data0=A[:, CTX:T][:, ::-1], data1=Bd[:, CTX:T][:, ::-1],
                            initial=Bd[:, 0:1], op0=ALU.mult, op1=ALU.add), R=[A, Bd], W=[Bd])
                for bi, (t0, n, isctx) in enumerate(self.blocks):
                    g_, s_o, h_ = gl[bi % 2], so[bi % 2], hs[bi % 2]
                    k.load(g_[:, 0:n], self.scr["GATE"][sl, t0:t0 + n], W=[g_])
                    k.I("dve", lambda e, h_=h_: e.tensor_tensor(out=h_[:, 0:n], in0=B1[:, t0:t0 + n], in1=Rb[:, t0:t0 + n], op=ALU.add),
                        R=[B1, Rb], W=[h_])
                    k.I("pool", lambda e, h_=h_, g_=g_, s_o=s_o: e.tensor_tensor(out=s_o[:, 0:n], in0=h_[:, 0:n], in1=g_[:, 0:n],
                                                                                op=ALU.mult), R=[h_, g_], W=[s_o])
                    k.store(self.scr["ST"][sl, t0:t0 + n], s_o[:, 0:n], R=[s_o])
            k.barrier()


def host_consts():
    ident = np.eye(128, dtype=np.float32)
    ones = np.ones((128, 128), dtype=np.float32)
    perm = np.zeros((128, 128), dtype=np.float32)
    ropec = np.zeros((128, 8), dtype=np.float32)
    for p in range(128):
        d = p % 64
        a = d // 32
        hf = (d % 32) // 16
        i = d % 16
        partner = p + 16 if hf == 0 else p - 16
        perm[partner, p] = 1.0
        invf = 1.0 / (10000.0 ** ((2.0 * i) / 32.0))
        sgn = -1.0 if hf == 0 else 1.0
        ropec[p, 0] = 1.0 if a == 0 else 0.0
        ropec[p, 1] = 1.0 if a == 1 else 0.0
        ropec[p, 2] = invf / (2.0 * math.pi)
        ropec[p, 3] = sgn * 2.0 * math.pi
        ropec[p, 4] = -sgn * math.pi
        ropec[p, 5] = 2.0 * math.pi
        ropec[p, 6] = -math.pi
    return {"ident": ident, "ones": ones, "perm": perm, "ropec": ropec}


def make_in_map(inputs, b, nlat=8192, shapes=None):
    m = {}

    def f(a):
        return np.ascontiguousarray(np.asarray(a, dtype=np.float32))
    m["x"] = f(inputs["x"][b][:nlat])
    m["c"] = f(inputs["c"][b])
    m["ctx"] = f(inputs["ctx"][b])
    for name in ("c_ctx", "mod_w", "mod_b", "norm_mix_g", "norm_ffn_g", "attn_w_qkv", "attn_w_o", "lru_w_in",
                 "lru_conv_w", "lru_conv_b", "lru_gate_w", "lru_gate_b", "lru_a_param", "lru_w_out",
                 "ffn_w_gate_up", "ffn_w_down", "moe_router_w", "moe_w_gate_up", "moe_w_down", "final_norm_g"):
        m[name] = f(inputs[name])
    m["attn_lambda"] = f(inputs["attn_lambda"]).reshape(2, 256)
    m["attn_subln_g"] = f(inputs["attn_subln_g"])
    m.update(host_consts())
    if shapes is not None:
        for kk in list(m):
            if shapes[kk] == [1, 1] and m[kk].shape != (1, 1):
                m[kk] = np.zeros((1, 1), np.float32)
    return m


def kernel(**inputs):
    prog = Prog()
    nc = prog.build()
    B = inputs["x"].shape[0]
    in_maps = [make_in_map(inputs, b) for b in range(B)]
    res = run_bass_kernel_spmd(nc, in_maps, core_ids=list(range(B)))
    return np.stack([np.asarray(res.results[b]["out"], dtype=np.float32) for b in range(B)], axis=0)
```
